# Optimizing a Trainium2 kernel written in Bass

```python
import math
import jax, jax.numpy as jnp
from jax import lax
import numpy as np

D_MODEL = 1024
BATCH = 4
SEQ = 8192
DEPTH = 2

HEAD_DIM = 64
N_HEADS = 16
D_MIX = N_HEADS * HEAD_DIM
N_HEADS_A = 8
N_KV_A = 2
WINDOW = 128
N_HEADS_B = 4
MOBA_BLOCK = 256
MOBA_TOPK = 3
N_HEADS_C = 4
C_KV_LATENT = 128
N_IDX_HEADS = 4
IDX_DIM = 64
DSA_TOPK = 256
D_FF = 2816
N_BUCKETS = 32
MAX_DISTANCE = 128
RMS_EPS = 1e-6
Q_BLOCK = 128

A_Q = N_HEADS_A * HEAD_DIM
A_KV = N_KV_A * HEAD_DIM
B_QKV = N_HEADS_B * HEAD_DIM
C_Q = N_HEADS_C * HEAD_DIM
C_QIDX = N_IDX_HEADS * IDX_DIM
COL_SIZES = (A_Q, A_KV, A_KV, B_QKV, B_QKV, B_QKV, C_Q, C_KV_LATENT, C_QIDX, IDX_DIM, N_IDX_HEADS)
D_IN = A_Q + 2 * A_KV + 3 * B_QKV + C_Q + C_KV_LATENT + C_QIDX + IDX_DIM + N_IDX_HEADS

kernel_name = "hymba_style_swa_moba_dsa_macaron"


def _rmsnorm(x, g):
    x32 = x.astype(jnp.float32)
    y = x32 * lax.rsqrt(jnp.mean(x32 * x32, axis=-1, keepdims=True) + RMS_EPS)
    return (y * g.astype(jnp.float32)).astype(x.dtype)


def _swiglu(x, w_gate, w_up, w_down):
    return (jax.nn.silu(x @ w_gate) * (x @ w_up)) @ w_down


def _t5_bucket(dist):
    n = jnp.maximum(dist, 0)
    max_exact = N_BUCKETS // 2
    nf = jnp.maximum(n, 1).astype(jnp.float32)
    large = max_exact + (jnp.log(nf / max_exact) / math.log(MAX_DISTANCE / max_exact)
                         * (N_BUCKETS - max_exact)).astype(jnp.int32)
    large = jnp.minimum(large, N_BUCKETS - 1)
    return jnp.where(n < max_exact, n, large)


def _split_cols(p):
    cuts = []
    acc = 0
    for sz in COL_SIZES[:-1]:
        acc += sz
        cuts.append(acc)
    return jnp.split(p, cuts, axis=-1)


def _sliding_window_attn(q, k, v, sinks, bias_tab):
    B, T = q.shape[0], q.shape[1]
    nq = T // Q_BLOCK
    G = N_HEADS_A // N_KV_A
    pad = ((0, 0), (WINDOW, 0), (0, 0), (0, 0))
    kp = jnp.pad(k, pad).reshape(B, nq + 1, Q_BLOCK, N_KV_A, HEAD_DIM)
    vp = jnp.pad(v, pad).reshape(B, nq + 1, Q_BLOCK, N_KV_A, HEAD_DIM)
    kb = jnp.concatenate([kp[:, :-1], kp[:, 1:]], axis=2)
    vb = jnp.concatenate([vp[:, :-1], vp[:, 1:]], axis=2)
    qb = q.reshape(B, nq, Q_BLOCK, N_KV_A, G, HEAD_DIM)
    s = jnp.einsum('bnqkgd,bnskd->bnkgqs', qb, kb).astype(jnp.float32) * (HEAD_DIM ** -0.5)
    qi = jnp.arange(Q_BLOCK)[:, None]
    si = jnp.arange(2 * Q_BLOCK)[None, :]
    dist = WINDOW + qi - si
    bias = bias_tab[:, _t5_bucket(dist)].astype(jnp.float32)
    bias = bias.reshape(N_KV_A, G, Q_BLOCK, 2 * Q_BLOCK)
    key_pos = jnp.arange(nq)[:, None] * Q_BLOCK - WINDOW + si
    valid = ((dist >= 0) & (dist < WINDOW))[None] & (key_pos >= 0)[:, None, :]
    s = jnp.where(valid[None, :, None, None], s + bias, -jnp.inf)
    sink = sinks.astype(jnp.float32).reshape(N_KV_A, G)[None, None, :, :, None, None]
    m = jnp.maximum(jnp.max(s, axis=-1, keepdims=True), sink)
    p = jnp.exp(s - m)
    denom = jnp.sum(p, axis=-1, keepdims=True) + jnp.exp(sink - m)
    o = jnp.einsum('bnkgqs,bnskd->bnqkgd', (p / denom).astype(v.dtype), vb)
    return o.reshape(B, T, N_HEADS_A * HEAD_DIM)


def _moba_attn(q, k, v, bias_tab):
    B, T = q.shape[0], q.shape[1]
    nb = -(-T // MOBA_BLOCK)
    Tp = nb * MOBA_BLOCK
    pad = ((0, 0), (0, Tp - T), (0, 0), (0, 0))
    kbh = jnp.pad(k, pad).reshape(B, nb, MOBA_BLOCK, N_HEADS_B, HEAD_DIM).transpose(0, 3, 1, 2, 4)
    vbh = jnp.pad(v, pad).reshape(B, nb, MOBA_BLOCK, N_HEADS_B, HEAD_DIM).transpose(0, 3, 1, 2, 4)
    n_sel = min(MOBA_TOPK, nb - 1)
    scale = HEAD_DIM ** -0.5
    h_idx = jnp.arange(N_HEADS_B)[None, :, None, None]
    b_idx = jnp.arange(B)[:, None, None, None]
    if n_sel > 0:
        k_mean = jnp.mean(kbh.astype(jnp.float32), axis=3)
        gate = jnp.einsum('bthd,bhnd->bhtn', q.astype(jnp.float32), k_mean)
        own_all = jnp.arange(T) // MOBA_BLOCK
        past = jnp.arange(nb)[None, :] < own_all[:, None]
        gate = jnp.where(past[None, None], gate, -jnp.inf)
        _, sel_idx = lax.top_k(gate, n_sel)

    def chunk(i):
        t0 = i * Q_BLOCK
        tq = t0 + jnp.arange(Q_BLOCK)
        qc = lax.dynamic_slice_in_dim(q, t0, Q_BLOCK, axis=1).transpose(0, 2, 1, 3)
        ob = t0 // MOBA_BLOCK
        k_own = lax.dynamic_index_in_dim(kbh, ob, axis=2, keepdims=False)
        v_own = lax.dynamic_index_in_dim(vbh, ob, axis=2, keepdims=False)
        d_own = tq[:, None] - (ob * MOBA_BLOCK + jnp.arange(MOBA_BLOCK))[None, :]
        s_own = (jnp.einsum('bhqd,bhsd->bhqs', qc, k_own).astype(jnp.float32) * scale
                 + bias_tab[:, _t5_bucket(d_own)].astype(jnp.float32)[None])
        s_own = jnp.where((d_own >= 0)[None, None], s_own, -jnp.inf)
        if n_sel == 0:
            p_own = jax.nn.softmax(s_own, axis=-1)
            o = jnp.einsum('bhqs,bhsd->bhqd', p_own.astype(v.dtype), v_own)
            return o.transpose(0, 2, 1, 3)
        idx_c = lax.dynamic_slice_in_dim(sel_idx, t0, Q_BLOCK, axis=2)
        k_sel = kbh[b_idx, h_idx, idx_c]
        v_sel = vbh[b_idx, h_idx, idx_c]
        sel_pos = idx_c[..., None] * MOBA_BLOCK + jnp.arange(MOBA_BLOCK)
        d_sel = tq[None, None, :, None, None] - sel_pos
        b_sel = bias_tab[h_idx[..., None], _t5_bucket(d_sel)].astype(jnp.float32)
        s_sel = jnp.einsum('bhqd,bhqjsd->bhqjs', qc, k_sel).astype(jnp.float32) * scale + b_sel
        ok = (idx_c < (tq // MOBA_BLOCK)[None, None, :, None])[..., None]
        s_sel = jnp.where(ok, s_sel, -jnp.inf).reshape(B, N_HEADS_B, Q_BLOCK, n_sel * MOBA_BLOCK)
        p = jax.nn.softmax(jnp.concatenate([s_sel, s_own], axis=-1), axis=-1).astype(v.dtype)
        p_sel = p[..., :n_sel * MOBA_BLOCK].reshape(B, N_HEADS_B, Q_BLOCK, n_sel, MOBA_BLOCK)
        p_own = p[..., n_sel * MOBA_BLOCK:]
        o = (jnp.einsum('bhqjs,bhqjsd->bhqd', p_sel, v_sel)
             + jnp.einsum('bhqs,bhsd->bhqd', p_own, v_own))
        return o.transpose(0, 2, 1, 3)

    out = lax.map(chunk, jnp.arange(T // Q_BLOCK))
    return out.transpose(1, 0, 2, 3, 4).reshape(B, T, N_HEADS_B * HEAD_DIM)


def _dsa_attn(q, k, v, q_idx, k_idx, w_idx, bias_tab):
    B, T = q.shape[0], q.shape[1]
    n_keep = min(DSA_TOPK, T // 4)
    key_pos = jnp.arange(T)
    b_idx = jnp.arange(B)[:, None, None]
    scale = HEAD_DIM ** -0.5

    def chunk(i):
        t0 = i * Q_BLOCK
        tq = t0 + jnp.arange(Q_BLOCK)
        qi = lax.dynamic_slice_in_dim(q_idx, t0, Q_BLOCK, axis=1)
        wi = lax.dynamic_slice_in_dim(w_idx, t0, Q_BLOCK, axis=1).astype(jnp.float32)
        dots = jnp.einsum('bqhd,bsd->bqhs', qi, k_idx).astype(jnp.float32) * (IDX_DIM ** -0.5)
        score = jnp.einsum('bqhs,bqh->bqs', jax.nn.relu(dots), wi)
        score = jnp.where((key_pos[None, :] <= tq[:, None])[None], score, -jnp.inf)
        _, idx = lax.top_k(score, n_keep)
        k_sel = k[b_idx, idx]
        v_sel = v[b_idx, idx]
        qc = lax.dynamic_slice_in_dim(q, t0, Q_BLOCK, axis=1)
        d = tq[None, :, None] - idx
        bias = bias_tab[:, _t5_bucket(d)].astype(jnp.float32).transpose(1, 0, 2, 3)
        s = jnp.einsum('bqhd,bqkd->bhqk', qc, k_sel).astype(jnp.float32) * scale + bias
        s = jnp.where((d >= 0)[:, None], s, -jnp.inf)
        p = jax.nn.softmax(s, axis=-1).astype(v.dtype)
        return jnp.einsum('bhqk,bqkd->bqhd', p, v_sel)

    out = lax.map(chunk, jnp.arange(T // Q_BLOCK))
    return out.transpose(1, 0, 2, 3, 4).reshape(B, T, N_HEADS_C * HEAD_DIM)


def setup_inputs(seed: int = 0) -> dict:
    key = jax.random.key(seed)
    ks = jax.random.split(key, 18)

    def nrm(k, shape, fan_in):
        return jax.random.normal(k, shape, jnp.float32) * (fan_in ** -0.5)

    def gain(k, shape):
        return 1.0 + 0.05 * jax.random.normal(k, shape, jnp.float32)

    return {
        "x": jax.random.normal(ks[0], (BATCH, SEQ, D_MODEL), jnp.float32),
        "rel_bias_table": 0.5 * jax.random.normal(ks[1], (N_BUCKETS, N_HEADS), jnp.float32),
        "ffn1_norm": gain(ks[2], (DEPTH, D_MODEL)),
        "ffn1_w_gate": nrm(ks[3], (DEPTH, D_MODEL, D_FF), D_MODEL),
        "ffn1_w_up": nrm(ks[4], (DEPTH, D_MODEL, D_FF), D_MODEL),
        "ffn1_w_down": nrm(ks[5], (DEPTH, D_FF, D_MODEL), D_FF),
        "mix_norm": gain(ks[6], (DEPTH, D_MODEL)),
        "w_in": nrm(ks[7], (DEPTH, D_MODEL, D_IN), D_MODEL),
        "attn_sinks": 0.5 * jax.random.normal(ks[8], (DEPTH, N_HEADS_A), jnp.float32),
        "kv_norm_c": gain(ks[9], (DEPTH, C_KV_LATENT)),
        "w_kv_up_c": nrm(ks[10], (DEPTH, C_KV_LATENT, 2 * HEAD_DIM), C_KV_LATENT),
        "w_out": nrm(ks[11], (DEPTH, D_MIX, D_MODEL), D_MIX),
        "ffn2_norm": gain(ks[12], (DEPTH, D_MODEL)),
        "ffn2_w_gate": nrm(ks[13], (DEPTH, D_MODEL, D_FF), D_MODEL),
        "ffn2_w_up": nrm(ks[14], (DEPTH, D_MODEL, D_FF), D_MODEL),
        "ffn2_w_down": nrm(ks[15], (DEPTH, D_FF, D_MODEL), D_FF),
        "final_norm": gain(ks[16], (D_MODEL,)),
    }


def reference(x, rel_bias_table, ffn1_norm, ffn1_w_gate, ffn1_w_up, ffn1_w_down, mix_norm, w_in,
              attn_sinks, kv_norm_c, w_kv_up_c, w_out, ffn2_norm, ffn2_w_gate, ffn2_w_up,
              ffn2_w_down, final_norm):
    B, T = x.shape[0], x.shape[1]
    tab = rel_bias_table.T
    tab_a = tab[:N_HEADS_A]
    tab_b = tab[N_HEADS_A:N_HEADS_A + N_HEADS_B]
    tab_c = tab[N_HEADS_A + N_HEADS_B:]
    for l in range(DEPTH):
        x = x + 0.5 * _swiglu(_rmsnorm(x, ffn1_norm[l]), ffn1_w_gate[l], ffn1_w_up[l], ffn1_w_down[l])
        h = _rmsnorm(x, mix_norm[l])
        (qa, ka, va, qb, kb, vb, qc, ckv, qidx, kidx, widx) = _split_cols(h @ w_in[l])
        out_a = _sliding_window_attn(qa.reshape(B, T, N_HEADS_A, HEAD_DIM),
                                     ka.reshape(B, T, N_KV_A, HEAD_DIM),
                                     va.reshape(B, T, N_KV_A, HEAD_DIM),
                                     attn_sinks[l], tab_a)
        out_b = _moba_attn(qb.reshape(B, T, N_HEADS_B, HEAD_DIM),
                           kb.reshape(B, T, N_HEADS_B, HEAD_DIM),
                           vb.reshape(B, T, N_HEADS_B, HEAD_DIM), tab_b)
        kv_c = _rmsnorm(ckv, kv_norm_c[l]) @ w_kv_up_c[l]
        out_c = _dsa_attn(qc.reshape(B, T, N_HEADS_C, HEAD_DIM),
                          kv_c[..., :HEAD_DIM], kv_c[..., HEAD_DIM:],
                          qidx.reshape(B, T, N_IDX_HEADS, IDX_DIM), kidx,
                          widx * (N_IDX_HEADS ** -0.5), tab_c)
        x = x + jnp.concatenate([out_a, out_b, out_c], axis=-1) @ w_out[l]
        x = x + 0.5 * _swiglu(_rmsnorm(x, ffn2_norm[l]), ffn2_w_gate[l], ffn2_w_up[l], ffn2_w_down[l])
    return _rmsnorm(x, final_norm)
```

```python
import math
import os
from contextlib import ExitStack

import numpy as np
import concourse.bass as bass
import concourse.mybir as mybir
from concourse.bass_utils import run_bass_kernel_spmd

F32 = mybir.dt.float32
BF16 = mybir.dt.bfloat16
U8 = mybir.dt.uint8
AF = mybir.ActivationFunctionType
ALU = mybir.AluOpType
AX = mybir.AxisListType

DT_SIZE = {F32: 4, BF16: 2, U8: 1}
ENGS = ["pe", "act", "dve", "pool", "sp"]
EPOCH = 20000
NSLOT = {"sp": 8, "act": 4, "pool": 8}

D = 1024
FF = 2816
NJ = FF // 128
DIN = 2244
BIG = 32768.0
EPS = 1e-6
NIT = 24
LO0 = -64.0
BRW = 128.0


class Op:
    __slots__ = ("eng", "fn", "dma", "deps", "signal", "sig", "slotprev", "cc", "tag")

    def __init__(self, eng, fn, dma):
        self.eng = eng
        self.fn = fn
        self.dma = dma
        self.deps = []
        self.signal = False
        self.sig = None
        self.slotprev = None
        self.cc = False


class Sched:
    def __init__(self, nc):
        self.nc = nc
        self.ops = {e: [] for e in ENGS}
        self.lastw = {}
        self.readers = {}
        self.pending = {e: [] for e in ENGS}
        self.dma_hist = {e: [] for e in ENGS}
        self.last_comp = {e: None for e in ENGS}
        self.tag = ""
        self.names = {}

    def add(self, eng, fn, reads=(), writes=(), dma=False, cc=False, bg=False):
        op = Op(eng, fn, dma)
        op.cc = cc
        op.tag = (self.tag, tuple(reads), tuple(writes))
        deps = []
        seen = set()

        def push(d):
            if d is None or id(d) in seen:
                return
            seen.add(id(d))
            if d.eng == "pe" and eng == "pe" and not d.dma and not dma:
                return
            deps.append(d)

        psum_reads = [k for k in reads if (k == "pb0" or (isinstance(k, tuple) and k[0] == "pb"))]
        if psum_reads:
            reads = [k for k in reads if k not in psum_reads]
            writes = list(writes) + psum_reads
        for k in reads:
            push(self.lastw.get(k))
        for k in writes:
            push(self.lastw.get(k))
            for r in self.readers.get(k, ()):
                push(r)
        for d in self.pending[eng]:
            push(d)
        self.pending[eng] = []
        op.deps = deps
        for d in deps:
            d.signal = True
        for k in writes:
            self.lastw[k] = op
            self.readers[k] = []
        for k in reads:
            lst = self.readers.setdefault(k, [])
            if not dma:
                lst[:] = [r for r in lst if not (r.eng == eng and not r.dma)]
            lst.append(op)
        self.ops[eng].append(op)
        if bg:
            pass
        elif dma or cc:
            h = self.dma_hist[eng]
            h.append(op)
            if len(h) > NSLOT[eng] + 2:
                h.pop(0)
        else:
            self.last_comp[eng] = op
        return op

    def barrier(self):
        tails = []
        for e in ENGS:
            if self.last_comp[e] is not None:
                tails.append(self.last_comp[e])
            tails.extend(self.dma_hist[e])
        for e in ENGS:
            self.pending[e] = list(tails)

    def emit(self, stack):
        nc = self.nc
        names = set()
        for e in ENGS:
            cnt = 0
            ndma = 0
            slot_last = {}
            ncc = 0
            for op in self.ops[e]:
                if op.dma:
                    R = NSLOT[e]
                    slot = ndma % R
                    val = 16 * (ndma // R + 1)
                    op.sig = ("d_%s_%d" % (e, slot), val, 16)
                    op.slotprev = slot_last.get(slot)
                    slot_last[slot] = op
                    ndma += 1
                elif op.cc:
                    ncc += 1
                    op.sig = ("cc_%s" % e, ncc, 1)
                elif op.signal:
                    ep = cnt // EPOCH
                    op.sig = ("c_%s_%d" % (e, ep), cnt - ep * EPOCH + 1, 1)
                    cnt += 1
                if op.sig is not None:
                    names.add(op.sig[0])
        sems = {}
        for name in sorted(names):
            sems[name] = stack.enter_context(nc.semaphore(name))
        block = stack.enter_context(nc.Block())
        stats = {"wait": 0, "ins": 0}

        def run(e):
            def body(engine):
                seenv = {}
                for op in self.ops[e]:
                    dl = list(op.deps)
                    if op.slotprev is not None:
                        dl.append(op.slotprev)
                    for d in dl:
                        name, val, _ = d.sig
                        if seenv.get(name, 0) >= val:
                            continue
                        engine.wait_ge(sems[name], val)
                        stats["wait"] += 1
                        seenv[name] = val
                    ins = op.fn(engine)
                    try:
                        self.names[str(ins.ins.name)] = op.tag
                    except Exception:
                        pass
                    stats["ins"] += 1
                    if op.sig is not None:
                        name, val, inc = op.sig
                        ins.then_inc(sems[name], inc)
            return body

        block.tensor(run("pe"))
        block.scalar(run("act"))
        block.vector(run("dve"))
        block.gpsimd(run("pool"))
        block.sync(run("sp"))
        self.stats = stats


class Arena:
    def __init__(self, nc, nbytes):
        self.h = nc.alloc_sbuf_tensor("arena", [128, nbytes], U8)
        self.ap = self.h.ap()
        self.nbytes = nbytes
        self.off = 0

    def alloc(self, name, parts, free, dt):
        if isinstance(free, int):
            free = [free]
        n = 1
        for f in free:
            n *= f
        nb = n * DT_SIZE[dt]
        off = (self.off + 63) // 64 * 64
        assert off + nb <= self.nbytes, "SBUF arena overflow %s: %d + %d > %d" % (name, off, nb, self.nbytes)
        self.off = off + nb
        ap = self.ap[0:parts, off:off + nb].bitcast(dt)
        if len(free) == 2:
            ap = ap.rearrange("p (a b) -> p a b", b=free[1])
        elif len(free) == 3:
            ap = ap.rearrange("p (a b c) -> p a b c", b=free[1], c=free[2])
        return ap

    def mark(self):
        return self.off

    def release(self, m):
        self.off = m


def bc_mid(a, n):
    return bass.AP(a.tensor, a.offset, [list(a.ap[0]), [0, n], list(a.ap[1])])


def bc_last(a, n):
    return bass.AP(a.tensor, a.offset, [list(a.ap[0]), list(a.ap[1]), [0, n]])


def bc_cols(a, n):
    return bass.AP(a.tensor, a.offset, [list(a.ap[0]), [0, n]])


def flat2(a):
    if len(a.shape) == 3:
        return a.rearrange("p a b -> p (a b)")
    return a


def _t5_bucket_np(n):
    n = np.maximum(n, 0)
    nf = np.maximum(n, 1).astype(np.float32)
    large = 16 + (np.log(nf / np.float32(16)) / np.float32(math.log(128 / 16)) * np.float32(16)).astype(np.int32)
    large = np.minimum(large, 31)
    return np.where(n < 16, n, large)


def host_consts(SEQ):
    oh = np.zeros((2, 33, 384), np.float32)
    for m in range(384):
        d = m - 128
        b = int(_t5_bucket_np(np.array([max(d, 0)]))[0])
        oh[0, 32 if d < 0 else b, m] = 1.0
        oh[1, 32 if (d < 0 or d >= 128) else b, m] = 1.0
    er = np.zeros((33, SEQ), np.float32)
    for n in range(min(32, SEQ // 256)):
        er[n, n * 256:(n + 1) * 256] = BIG
    er[32, :] = 1.0
    q = np.arange(128)[:, None]
    j = np.arange(128)[None, :]
    cm = np.where(j <= q, 0.0, -BIG).astype(np.float32)
    return oh, er, cm


def build_program(SEQ, DEPTH, debug=False, upto=None):
    TC = SEQ // 2
    NT = TC // 128
    NTT = TC // 512
    T2 = SEQ
    NC2 = T2 // 128
    NB = SEQ // 256
    NBH = NB // 2
    GW = max(NB, 8)

    nc = bass.Bass("TRN2", target_bir_lowering=False)
    L = DEPTH

    def din(name, shape, dt=F32):
        return nc.dram_tensor(name, list(shape), dt, kind="ExternalInput").ap()

    def dscr(name, shape, dt=BF16):
        return nc.dram_tensor(name, list(shape), dt, kind="Internal").ap()

    x_in = din("x", [TC, D])
    tab_d = din("tab", [32, 16])
    n1_d = din("n1", [L, D])
    w1g_d = din("w1g", [L, D, FF])
    w1u_d = din("w1u", [L, D, FF])
    w1d_d = din("w1d", [L, FF, D])
    nm_d = din("nm", [L, D])
    win_d = din("win", [L, D, DIN])
    sink_d = din("sinks", [L, 8])
    kvn_d = din("kvn", [L, 128])
    wup_d = din("wup", [L, 128, 128])
    wo_d = din("wo", [L, D, D])
    n2_d = din("n2", [L, D])
    w2g_d = din("w2g", [L, D, FF])
    w2u_d = din("w2u", [L, D, FF])
    w2d_d = din("w2d", [L, FF, D])
    nf_d = din("nf", [1, D])
    oh_d = din("oh", [2, 33, 384])
    er_d = din("er", [33, T2])
    cm_d = din("cm", [128, 128])
    pfx_d = din("pfx", [1, 2])
    out_d = nc.dram_tensor("out", [TC, D], F32, kind="ExternalOutput").ap()

    xres = dscr("xres", [TC, D], F32)
    qaT_d = dscr("qaT", [8, 64, TC])
    qbT_d = dscr("qbT", [4, 64, TC])
    qcT_d = dscr("qcT", [4, 64, TC])
    qiT_d = dscr("qiT", [4, 64, TC])
    wi_d = dscr("wi", [TC, 16], F32)
    KT_t = [dscr("KT%d" % t, [512, 512]) for t in range(NTT)]
    VT_t = [dscr("VT%d" % t, [512, 448]) for t in range(NTT)]
    gKT_t = [dscr("gKT%d" % t, [1024, 512]) for t in range(NTT)]
    gVT_t = [dscr("gVT%d" % t, [1024, 448]) for t in range(NTT)]
    catT_d = dscr("catT", [16, 64, TC])
    Zf_d = dscr("Zf", [16, 128, 384], F32)
    ZA_d = dscr("ZA", [8, 128, 384], F32)
    wbf = {}
    for l in range(L):
        for nm_, shp in (("w1g", [D, FF]), ("w1u", [D, FF]), ("w1d", [FF, D]), ("w2g", [D, FF]), ("w2u", [D, FF]),
                         ("w2d", [FF, D]), ("win", [D, DIN])):
            if l == 0 and nm_ in ("w1g", "w1u", "w1d"):
                continue
            wbf[(l, nm_)] = dscr("wbf_%s_%d" % (nm_, l), shp)
    dbg = {}
    if debug:
        dbg["xres"] = nc.dram_tensor("dbg_xres", [TC, D], F32, kind="ExternalOutput").ap()
        dbg["catT"] = nc.dram_tensor("dbg_catT", [16, 64, TC], BF16, kind="ExternalOutput").ap()
        dbg["qaT"] = nc.dram_tensor("dbg_qaT", [8, 64, TC], BF16, kind="ExternalOutput").ap()

    stack = ExitStack()
    S = Sched(nc)
    A = Arena(nc, 210000)
    pb = [nc.alloc_psum_tensor("pb%d" % i, [128, 512], F32).ap() for i in range(8)]
    pbh = [p.bitcast(BF16) for p in pb]

    ident = A.alloc("ident", 128, 128, BF16)
    ident4 = A.alloc("ident4", 128, [4, 128], BF16)
    ones64 = A.alloc("ones64", 128, 64, BF16)
    T0all = A.alloc("T0all", 128, [16, 128], BF16)
    T1all = A.alloc("T1all", 128, [16, 128], BF16)
    cmt = A.alloc("cmt", 128, 128, F32)
    pfxc = A.alloc("pfxc", 128, 2, F32)
    pfxrow = A.alloc("pfxrow", 128, GW, F32)
    stepc = A.alloc("stepc", 128, 2 * GW, F32)
    zero1 = A.alloc("zero1", 128, 1, F32)
    selT = A.alloc("selT", 65, 64, F32)
    tab31 = A.alloc("tab31", 128, 16, F32)
    b31s = A.alloc("b31s", 1, [8, 128], BF16)

    def phase0():
        m = A.mark()
        identf = A.alloc("identf", 128, 128, F32)
        tabf = A.alloc("tabf", 33, 16, F32)
        ohb = A.alloc("ohb", 33, [2, 384], BF16)
        lh = [A.alloc("lh%d" % i, 33, 128, BF16) for i in range(2)]
        zsb = [A.alloc("zsb%d" % i, 128, 384, F32) for i in range(2)]
        S.add("pool", lambda e: e.memset(identf, 0.0), writes=["identf"])
        S.add("pool", lambda e: e.affine_select(out=identf, in_=identf, pattern=[[-1, 128]],
                                                compare_op=ALU.not_equal, fill=1.0, base=0,
                                                channel_multiplier=1),
              reads=["identf"], writes=["identf"])
        S.add("dve", lambda e: e.tensor_copy(out=ident, in_=identf), reads=["identf"], writes=["ident"])
        S.add("dve", lambda e: e.tensor_copy(out=ident4, in_=bc_mid(identf, 4)), reads=["identf"], writes=["ident4"])
        S.add("dve", lambda e: e.memset(ones64, 1.0), writes=["ones64"])
        S.add("dve", lambda e: e.memset(selT[0:64, :], 0.0), writes=["selT"])
        S.add("dve", lambda e: e.memset(selT[64:65, :], 1.0), writes=["selT"])
        S.add("dve", lambda e: e.memset(zero1, 0.0), writes=["zero1"])
        S.add("sp", lambda e: e.dma_start(out=cmt, in_=cm_d), writes=["cmt"], dma=True)
        S.add("sp", lambda e: e.dma_start(out=pfxc, in_=pfx_d.to_broadcast([128, 2])), writes=["pfxc"], dma=True)
        S.add("sp", lambda e: e.dma_start(out=tab31, in_=tab_d[31:32, :].to_broadcast([128, 16])), writes=["tab31"], dma=True)
        for j in range(8):
            S.add("dve", lambda e, j=j: e.tensor_copy(out=b31s[0:1, j, :], in_=bc_cols(tab31[0:1, 8 + j:9 + j], 128)),
                  reads=["tab31"], writes=["b31s"])
        S.add("dve", lambda e: e.memset(pfxrow, 0.0), writes=["pfxrow"])
        S.add("dve", lambda e: e.tensor_scalar(out=pfxrow[:, 0:NBH], in0=pfxrow[:, 0:NBH], scalar1=pfxc[:, 0:1],
                                               scalar2=None, op0=ALU.add),
              reads=["pfxrow", "pfxc"], writes=["pfxrow"])
        S.add("dve", lambda e: e.memset(stepc[:, 0:GW], 0.0), writes=["stepc"])
        S.add("dve", lambda e: e.memset(stepc[:, GW:2 * GW], -BIG), writes=["stepc"])
        S.add("sp", lambda e: e.dma_start(out=tabf[0:32, :], in_=tab_d), writes=["tabf"], dma=True)
        S.add("dve", lambda e: e.memset(tabf[32:33, :], -BIG), writes=["tabf"])
        S.add("pool", lambda e: e.dma_start(out=ohb, in_=oh_d.rearrange("w r m -> r w m")), writes=["ohb"], dma=True)
        n = 0
        for h in range(16):
            for which in range(2):
                if which == 1 and h >= 8:
                    continue
                s = n % 2
                n += 1
                S.add("dve", lambda e, s=s, h=h: e.tensor_copy(out=lh[s], in_=bc_cols(tabf[0:33, h:h + 1], 128)),
                      reads=["tabf"], writes=[("lh", s)])
                S.add("pe", lambda e, s=s, which=which: e.matmul(pb[s][:, 0:384], lhsT=lh[s], rhs=ohb[:, which, :],
                                                                  start=True, stop=True),
                      reads=[("lh", s), "ohb"], writes=[("pb", s)])
                S.add("act", lambda e, s=s: e.activation(out=zsb[s], in_=pb[s][:, 0:384], func=AF.Copy),
                      reads=[("pb", s)], writes=[("zsb", s)])
                dst = (ZA_d if which == 1 else Zf_d)[h]
                zkey = ("Z", which, h)
                S.add("sp", lambda e, s=s, dst=dst: e.dma_start(out=dst, in_=zsb[s]),
                      reads=[("zsb", s)], writes=[zkey], dma=True)
        for h in range(16):
            zt = Zf_d.tensor
            base = h * 128 * 384
            S.add("pool", lambda e, h=h, base=base: e.dma_start(
                out=T0all[:, h, :], in_=bass.AP(Zf_d.tensor, base + 128, [[383, 128], [1, 128]])),
                reads=[("Z", 0, h)], writes=["T0all"], dma=True)
            if h < 8:
                S.add("pool", lambda e, h=h, base=base: e.dma_start(
                    out=T1all[:, h, :], in_=bass.AP(ZA_d.tensor, base + 256, [[383, 128], [1, 128]])),
                    reads=[("Z", 1, h)], writes=["T1all"], dma=True)
            else:
                S.add("pool", lambda e, h=h, base=base: e.dma_start(
                    out=T1all[:, h, :], in_=bass.AP(Zf_d.tensor, base + 256, [[383, 128], [1, 128]])),
                    reads=[("Z", 0, h)], writes=["T1all"], dma=True)
        S.barrier()
        A.release(m)

    def norm_sub(xs_ap, xkey, gtile, gkey, stat, skey, hb_ap, hkey, junk, width=D):
        if junk is None:
            jout, jkey = hb_ap, hkey
        else:
            jout, jkey = junk[:, 0:width], "junk"
        S.add("act", lambda e: e.activation(out=jout, in_=xs_ap, func=AF.Square, accum_out=stat[:, 0:1]),
              reads=[xkey], writes=[jkey, skey])
        S.add("act", lambda e: e.activation(out=stat[:, 1:2], in_=stat[:, 0:1], func=AF.Sqrt, scale=1.0 / width, bias=EPS),
              reads=[skey], writes=[skey])
        S.add("dve", lambda e: e.reciprocal(out=stat[:, 2:3], in_=stat[:, 1:2]), reads=[skey], writes=[skey])
        S.add("dve", lambda e: e.scalar_tensor_tensor(out=hb_ap, in0=xs_ap, scalar=stat[:, 2:3], in1=gtile,
                                                      op0=ALU.mult, op1=ALU.mult),
              reads=[xkey, skey, gkey], writes=[hkey])

    def precast(order):
        srcs = {"w1g": w1g_d, "w1u": w1u_d, "w1d": w1d_d, "w2g": w2g_d, "w2u": w2u_d, "w2d": w2d_d, "win": win_d}
        for (l, nm_) in order:
            dstt = wbf[(l, nm_)]
            rows = dstt.shape[0]
            nparts = 4
            step = rows // nparts
            for p in range(nparts):
                r0, r1 = p * step, (rows if p == nparts - 1 else (p + 1) * step)
                S.add("pool", lambda e, l=l, nm_=nm_, r0=r0, r1=r1, dstt=dstt: e.dma_start(out=dstt[r0:r1, :], in_=srcs[nm_][l, r0:r1, :]),
                      writes=[("wbf", l, nm_, p)], dma=True, bg=True)

    def ffn_phase(l, wg_d, wu_d, wd_d, norm_row, src, dst, pre=None):
        m = A.mark()
        wg = A.alloc("wg", 128, [8, FF], BF16)
        wu = A.alloc("wu", 128, [8, FF], BF16)
        wd = A.alloc("wd", 128, [NJ, D], BF16)
        gb = A.alloc("gb", 128, D, F32)
        NXS = 5
        xs = [A.alloc("xs%d" % s, 128, D, F32) for s in range(NXS)]
        hb1 = A.alloc("hb0", 128, D, BF16)
        hb = [hb1, hb1]
        hT = A.alloc("hT", 128, [8, 512], BF16)
        actT = A.alloc("actT", 128, [NJ, 512], BF16)
        sg = [A.alloc("sg%d" % s, 128, 512, F32) for s in range(2)]
        stat = A.alloc("stat", 128, [NXS, 4], F32)
        junk = None
        S.add("sp", lambda e: e.dma_start(out=gb, in_=norm_row.to_broadcast([128, D])), writes=["gb"], dma=True)
        if pre is None:
            for k in range(8):
                S.add("pool", lambda e, k=k: e.dma_start(out=wg[:, k, :], in_=wg_d[l, k * 128:(k + 1) * 128, :]),
                      writes=[("wg", k)], dma=True)
                S.add("pool", lambda e, k=k: e.dma_start(out=wu[:, k, :], in_=wu_d[l, k * 128:(k + 1) * 128, :]),
                      writes=[("wu", k)], dma=True)
            for j in range(NJ):
                S.add("pool", lambda e, j=j: e.dma_start(out=wd[:, j, :], in_=wd_d[l, j * 128:(j + 1) * 128, :]),
                      writes=[("wd", j)], dma=True)
            precast([(0, "win"), (0, "w2g"), (0, "w2u"), (0, "w2d")])
        else:
            gname, uname, dname = pre
            gb_, ub_, db_ = wbf[(l, gname)], wbf[(l, uname)], wbf[(l, dname)]
            allk = lambda nm_: [("wbf", l, nm_, p) for p in range(4)]
            nq = 0
            for k in range(8):
                S.add("sp" if nq % 2 == 0 else "act", lambda e, k=k: e.dma_start(out=wg[:, k, :], in_=gb_[k * 128:(k + 1) * 128, :]),
                      reads=allk(gname), writes=[("wg", k)], dma=True)
                nq += 1
                S.add("sp" if nq % 2 == 0 else "act", lambda e, k=k: e.dma_start(out=wu[:, k, :], in_=ub_[k * 128:(k + 1) * 128, :]),
                      reads=allk(uname), writes=[("wu", k)], dma=True)
                nq += 1
            for j in range(NJ):
                S.add("sp" if nq % 2 == 0 else "act", lambda e, j=j: e.dma_start(out=wd[:, j, :], in_=db_[j * 128:(j + 1) * 128, :]),
                      reads=allk(dname), writes=[("wd", j)], dma=True)
                nq += 1
        for tt in range(NTT):
            for sub in range(4):
                u = tt * 4 + sub
                sl = u % NXS
                S.add("sp", lambda e, u=u, sl=sl: e.dma_start(out=xs[sl], in_=src[u * 128:(u + 1) * 128, :]),
                      reads=[("xres", u)], writes=[("xs", sl)], dma=True)
                norm_sub(xs[sl], ("xs", sl), gb, "gb", stat[:, sl, :], ("stat", sl), hb[0], ("hb", 0), junk)
                for k in range(8):
                    S.add("pe", lambda e, u=u, k=k: e.transpose(out=pbh[0][:, k * 128:(k + 1) * 128],
                                                                in_=hb[0][:, k * 128:(k + 1) * 128], identity=ident),
                          reads=[("hb", 0), "ident"], writes=["pb0"])
                S.add("act", lambda e, sub=sub: e.activation(
                    out=hT[:, :, sub * 128:(sub + 1) * 128],
                    in_=pbh[0].rearrange("p (k t) -> p k t", t=128), func=AF.Copy),
                    reads=["pb0"], writes=[("hT", sub)])
            hkeys = [("hT", s) for s in range(4)]
            for j in range(NJ):
                pg = 1 + j % 2
                pu = 3 + j % 2
                for k in range(8):
                    S.add("pe", lambda e, j=j, k=k, pg=pg: e.matmul(pb[pg], lhsT=wg[:, k, j * 128:(j + 1) * 128],
                                                                    rhs=hT[:, k, :], start=(k == 0), stop=(k == 7)),
                          reads=[("wg", k)] + hkeys, writes=[("pb", pg)])
                for k in range(8):
                    S.add("pe", lambda e, j=j, k=k, pu=pu: e.matmul(pb[pu], lhsT=wu[:, k, j * 128:(j + 1) * 128],
                                                                    rhs=hT[:, k, :], start=(k == 0), stop=(k == 7)),
                          reads=[("wu", k)] + hkeys, writes=[("pb", pu)])
                S.add("act", lambda e, j=j, pg=pg: e.activation(out=sg[j % 2], in_=pb[pg], func=AF.Silu),
                      reads=[("pb", pg)], writes=[("sg", j % 2)])
                S.add("dve", lambda e, j=j, pu=pu: e.tensor_tensor(out=actT[:, j, :], in0=sg[j % 2], in1=pb[pu], op=ALU.mult),
                      reads=[("sg", j % 2), ("pb", pu)], writes=[("actT", j)])
            n = 0
            for sub in range(4):
                u = tt * 4 + sub
                sl = u % NXS
                for half in range(2):
                    py = 5 + n % 2
                    n += 1
                    for j in range(NJ):
                        S.add("pe", lambda e, j=j, sub=sub, half=half, py=py: e.matmul(
                            pb[py], lhsT=actT[:, j, sub * 128:(sub + 1) * 128],
                            rhs=wd[:, j, half * 512:(half + 1) * 512], start=(j == 0), stop=(j == NJ - 1)),
                            reads=[("actT", j), ("wd", j)], writes=[("pb", py)])
                    S.add("dve", lambda e, sl=sl, half=half, py=py: e.scalar_tensor_tensor(
                        out=xs[sl][:, half * 512:(half + 1) * 512], in0=pb[py], scalar=0.5,
                        in1=xs[sl][:, half * 512:(half + 1) * 512], op0=ALU.mult, op1=ALU.add),
                        reads=[("pb", py), ("xs", sl)], writes=[("xs", sl)])
                S.add("sp", lambda e, u=u, sl=sl: e.dma_start(out=dst[u * 128:(u + 1) * 128, :], in_=xs[sl]),
                      reads=[("xs", sl)], writes=[("xres", u)], dma=True)
        S.barrier()
        A.release(m)

    FM_COLS = [(0, 512), (512, 640), (768, 1024), (1024, 1280), (1536, 1792), (1920, 2176), (2176, 2240)]
    QSCALE = [True] * 8 + [False] * 2 + [True] * 4 + [False] * 4 + [True] * 4 + [True] * 4 + [False]

    def proj_phase(l):
        m = A.mark()
        wfm = A.alloc("wfm", 128, [8, 1728], BF16)
        wtm = A.alloc("wtm", 128, [8, 576], BF16)
        wupb = A.alloc("wupb", 128, 128, BF16)
        gb = A.alloc("gb", 128, D, F32)
        gkv = A.alloc("gkv", 128, 128, F32)
        xs = [A.alloc("xs%d" % s, 128, D, F32) for s in range(2)]
        hb = [A.alloc("hb%d" % s, 128, D, BF16) for s in range(2)]
        hT = A.alloc("hT", 128, [8, 512], BF16)
        hd = A.alloc("hd", 64, [28, 512], BF16)
        vtm = A.alloc("vtm", 128, [4, 448], BF16)
        ck = A.alloc("ck", 128, 128, F32)
        ckn = A.alloc("ckn", 128, 128, BF16)
        cknT = A.alloc("cknT", 128, 512, BF16)
        wis = A.alloc("wis", 128, [4, 16], F32)
        stat = A.alloc("stat", 128, [2, 4], F32)
        stat2 = A.alloc("stat2", 128, 4, F32)
        junk = A.alloc("junk", 128, D, BF16)
        S.add("sp", lambda e: e.dma_start(out=gb, in_=nm_d[l:l + 1, :].to_broadcast([128, D])), writes=["gb"], dma=True)
        S.add("sp", lambda e: e.dma_start(out=gkv, in_=kvn_d[l:l + 1, :].to_broadcast([128, 128])), writes=["gkv"], dma=True)
        S.add("pool", lambda e: e.dma_start(out=wupb, in_=wup_d[l]), writes=["wupb"], dma=True)
        winb = wbf[(l, "win")]
        wkeys = [("wbf", l, "win", p) for p in range(4)]
        nq = 0
        for k in range(8):
            c = 0
            for (a, b) in FM_COLS:
                S.add("sp" if nq % 2 == 0 else "act", lambda e, k=k, a=a, b=b, c=c: e.dma_start(
                    out=wfm[:, k, c:c + (b - a)], in_=winb[k * 128:(k + 1) * 128, a:b]),
                    reads=wkeys, writes=[("wfm", k)], dma=True)
                nq += 1
                c += b - a
            for (a, b, c) in [(640, 768, 0), (1280, 1536, 128), (1792, 1920, 384), (2180, 2244, 512)]:
                S.add("sp" if nq % 2 == 0 else "act", lambda e, k=k, a=a, b=b, c=c: e.dma_start(
                    out=wtm[:, k, c:c + (b - a)], in_=winb[k * 128:(k + 1) * 128, a:b]),
                    reads=wkeys, writes=[("wtm", k)], dma=True)
                nq += 1
        nev = 0
        PSTOP = int(os.environ.get("PROJ_STOP", "99"))
        for tt in range(NTT if PSTOP > 1 else 0):
            for sub in range(4):
                u = tt * 4 + sub
                sl = u % 2
                S.add("sp", lambda e, u=u, sl=sl: e.dma_start(out=xs[sl], in_=xres[u * 128:(u + 1) * 128, :]),
                      reads=[("xres", u)], writes=[("xs", sl)], dma=True)
                norm_sub(xs[sl], ("xs", sl), gb, "gb", stat[:, sl, :], ("stat", sl), hb[sl], ("hb", sl), junk)
                for k in range(8):
                    S.add("pe", lambda e, sl=sl, k=k: e.transpose(out=pbh[0][:, k * 128:(k + 1) * 128],
                                                                  in_=hb[sl][:, k * 128:(k + 1) * 128], identity=ident),
                          reads=[("hb", sl), "ident"], writes=["pb0"])
                S.add("act", lambda e, sub=sub: e.activation(
                    out=hT[:, :, sub * 128:(sub + 1) * 128],
                    in_=pbh[0].rearrange("p (k t) -> p k t", t=128), func=AF.Copy),
                    reads=["pb0"], writes=[("hT", sub)])
            hkeys = [("hT", s) for s in range(4)]
            for sub in range(4):
                u = tt * 4 + sub
                for k in range(8):
                    S.add("pe", lambda e, sub=sub, k=k: e.matmul(pb[4], lhsT=hT[:, k, sub * 128:(sub + 1) * 128],
                                                                 rhs=wtm[:, k, 0:512], start=(k == 0), stop=(k == 7)),
                          reads=[("wtm", k), ("hT", sub)], writes=[("pb", 4)])
                for k in range(8):
                    S.add("pe", lambda e, sub=sub, k=k: e.matmul(pb[5][:, 0:64], lhsT=hT[:, k, sub * 128:(sub + 1) * 128],
                                                                 rhs=wtm[:, k, 512:576], start=(k == 0), stop=(k == 7)),
                          reads=[("wtm", k), ("hT", sub)], writes=[("pb", 5)])
                S.add("act", lambda e, sub=sub: e.activation(out=vtm[:, sub, 0:384], in_=pb[4][:, 0:384], func=AF.Copy),
                      reads=[("pb", 4)], writes=[("vtm", sub)])
                S.add("act", lambda e: e.activation(out=ck, in_=pb[4][:, 384:512], func=AF.Copy), reads=[("pb", 4)], writes=["ck"])
                S.add("dve", lambda e, sub=sub: e.tensor_scalar(out=wis[:, sub, :], in0=pb[5][:, 48:64], scalar1=0.5,
                                                                scalar2=None, op0=ALU.mult),
                      reads=[("pb", 5)], writes=["wis"])
                norm_sub(ck, "ck", gkv, "gkv", stat2, "stat2", ckn, "ckn", junk, width=128)
                S.add("pe", lambda e: e.transpose(out=pbh[6][:, 0:128], in_=ckn, identity=ident),
                      reads=["ckn", "ident"], writes=[("pb", 6)])
                S.add("act", lambda e, sub=sub: e.activation(out=cknT[:, sub * 128:(sub + 1) * 128], in_=pbh[6][:, 0:128],
                                                             func=AF.Copy),
                      reads=[("pb", 6)], writes=[("cknT", sub)])
                S.add("pe", lambda e, sub=sub: e.matmul(pb[7][:, 0:64], lhsT=cknT[:, sub * 128:(sub + 1) * 128],
                                                        rhs=wupb[:, 64:128], start=True, stop=True),
                      reads=[("cknT", sub), "wupb"], writes=[("pb", 7)])
                S.add("dve", lambda e, sub=sub: e.tensor_copy(out=vtm[:, sub, 384:448], in_=pb[7][:, 0:64]),
                      reads=[("pb", 7)], writes=[("vtm", sub)])
            for f in range(27 if PSTOP > 2 else 0):
                pf = 1 + f % 3
                for k in range(8):
                    S.add("pe", lambda e, f=f, k=k, pf=pf: e.matmul(pb[pf][0:64, :], lhsT=wfm[:, k, f * 64:(f + 1) * 64],
                                                                    rhs=hT[:, k, :], start=(k == 0), stop=(k == 7)),
                          reads=[("wfm", k)] + hkeys, writes=[("pb", pf)])
                sc = 0.125 if QSCALE[f] else 1.0
                if nev % 2 == 0:
                    S.add("act", lambda e, f=f, pf=pf, sc=sc: e.activation(out=hd[:, f, :], in_=pb[pf][0:64, :],
                                                                           func=AF.Copy, scale=sc),
                          reads=[("pb", pf)], writes=[("hd", f)])
                else:
                    S.add("dve", lambda e, f=f, pf=pf, sc=sc: e.tensor_scalar(out=hd[:, f, :], in0=pb[pf][0:64, :],
                                                                              scalar1=sc, scalar2=None, op0=ALU.mult),
                          reads=[("pb", pf)], writes=[("hd", f)])
                nev += 1
            S.add("pe", lambda e: e.matmul(pb[1][0:64, :], lhsT=wupb[:, 0:64], rhs=cknT, start=True, stop=True),
                  reads=[("cknT", s) for s in range(4)] + ["wupb"], writes=[("pb", 1)])
            S.add("act", lambda e: e.activation(out=hd[:, 27, :], in_=pb[1][0:64, :], func=AF.Copy),
                  reads=[("pb", 1)], writes=[("hd", 27)])
            t0 = tt * 512
            if PSTOP <= 4:
                continue

            def fm_out(dst3, f0, nf, key):
                S.add("sp", lambda e: e.dma_start(out=dst3, in_=hd[:, f0:f0 + nf, :]),
                      reads=[("hd", f) for f in range(f0, f0 + nf)], writes=[key], dma=True)
            fm_out(qaT_d[:, :, t0:t0 + 512].rearrange("h d t -> d h t"), 0, 8, ("qaT", tt))
            fm_out(KT_t[tt][0:128, :].rearrange("(h d) t -> d h t", d=64), 8, 2, ("KTa", tt))
            fm_out(qbT_d[:, :, t0:t0 + 512].rearrange("h d t -> d h t"), 10, 4, ("qbT", tt))
            fm_out(KT_t[tt][128:384, :].rearrange("(h d) t -> d h t", d=64), 14, 4, ("KTb", tt))
            fm_out(qcT_d[:, :, t0:t0 + 512].rearrange("h d t -> d h t"), 18, 4, ("qcT", tt))
            fm_out(qiT_d[:, :, t0:t0 + 512].rearrange("h d t -> d h t"), 22, 4, ("qiT", tt))
            fm_out(KT_t[tt][448:512, :].rearrange("(h d) t -> d h t", d=64), 26, 1, ("KTi", tt))
            fm_out(KT_t[tt][384:448, :].rearrange("(h d) t -> d h t", d=64), 27, 1, ("KTc", tt))
            S.add("sp", lambda e, tt=tt: e.dma_start(out=VT_t[tt].rearrange("(s p) c -> p s c", p=128), in_=vtm),
                  reads=[("vtm", s) for s in range(4)], writes=[("VT", tt)], dma=True)
            S.add("sp", lambda e, t0=t0: e.dma_start(out=wi_d[t0:t0 + 512, :].rearrange("(s p) c -> p s c", p=128), in_=wis),
                  reads=["wis"], writes=[("wi", tt)], dma=True)
        S.barrier()
        A.release(m)

    def exchange():
        if os.environ.get("SKIP_XCHG"):
            return
        groups = [[2 * g, 2 * g + 1] for g in range(4)]
        for t in range(NTT):
            S.add("pool", lambda e, t=t: e.collective_compute("AllGather", ALU.bypass, replica_groups=groups,
                                                              ins=[KT_t[t]], outs=[gKT_t[t]]),
                  reads=["ccorder"], writes=[("gKT", t), "ccorder"], cc=True)
            S.add("pool", lambda e, t=t: e.collective_compute("AllGather", ALU.bypass, replica_groups=groups,
                                                              ins=[VT_t[t]], outs=[gVT_t[t]]),
                  reads=["ccorder"], writes=[("gVT", t), "ccorder"], cc=True)
        S.barrier()

    def finalize(pO, pD, okey, dkey, nh, dst3, dkey_out, extra=None, tmp=None):
        dn, oc, osb = tmp
        W = nh * 128
        S.add("act", lambda e: e.activation(out=osb[:, 0:W], in_=pO[0:65, 0:W], func=AF.Copy), reads=[okey], writes=["osb"])
        S.add("pe", lambda e: e.matmul(pD[0:64, 0:W], lhsT=selT, rhs=osb[:, 0:W], start=True, stop=True),
              reads=["selT", "osb"], writes=[dkey])
        if extra is not None:
            S.add("dve", lambda e: e.tensor_tensor(out=dn[:, 0:W], in0=pD[0:64, 0:W], in1=extra, op=ALU.add),
                  reads=[dkey, "exps"], writes=["dn"])
            S.add("dve", lambda e: e.reciprocal(out=dn[:, 0:W], in_=dn[:, 0:W]), reads=["dn"], writes=["dn"])
        else:
            S.add("dve", lambda e: e.reciprocal(out=dn[:, 0:W], in_=pD[0:64, 0:W]), reads=[dkey], writes=["dn"])
        S.add("dve", lambda e: e.tensor_tensor(out=oc[:, 0:W], in0=osb[0:64, 0:W], in1=dn[:, 0:W], op=ALU.mult),
              reads=["osb", "dn"], writes=["oc"])
        S.add("sp", lambda e: e.dma_start(out=dst3, in_=oc[:, 0:W].rearrange("p (h t) -> p h t", t=128)),
              reads=["oc"], writes=[dkey_out], dma=True)

    def swa_phase(l):
        m = A.mark()
        kaT = A.alloc("kaT", 64, [2, TC], BF16)
        vaO = A.alloc("vaO", 128, [NT, 2, 65], BF16)
        kaH = A.alloc("kaH", 65, [2, 128], BF16)
        vaH = A.alloc("vaH", 128, [2, 65], BF16)
        exps8 = A.alloc("exps8", 64, 8, F32)
        exps = A.alloc("exps", 64, [8, 128], F32)
        qa = [A.alloc("qa%d" % s, 65, [8, 128], BF16) for s in range(2)]
        PT = [A.alloc("PT%d" % s, 128, 512, BF16) for s in range(2)]
        dn = A.alloc("dn", 64, 512, F32)
        oc = A.alloc("oc", 64, 512, BF16)
        osb = A.alloc("osb", 65, 512, F32)
        S.add("dve", lambda e: e.memset(vaO[:, :, :, 64:65], 1.0), writes=["vaO"])
        S.add("dve", lambda e: e.memset(vaH[:, :, 64:65], 1.0), writes=["vaH"])
        for t in range(NTT):
            S.add("sp", lambda e, t=t: e.dma_start(out=kaT[:, :, t * 512:(t + 1) * 512],
                                                   in_=KT_t[t][0:128, :].rearrange("(g d) t -> d g t", d=64)),
                  reads=[("KTa", t)], writes=["kaT"], dma=True)
            for g in range(2):
                S.add("sp", lambda e, t=t, g=g: e.dma_start(out=vaO[:, 4 * t:4 * t + 4, g, 0:64],
                                                            in_=VT_t[t][:, g * 64:(g + 1) * 64].rearrange("(n p) d -> p n d", p=128)),
                      reads=[("VT", t)], writes=["vaO"], dma=True)
        S.add("sp", lambda e: e.dma_start(out=kaH[0:64, :, :],
                                          in_=gKT_t[NTT - 1][0:128, 384:512].rearrange("(g d) t -> d g t", d=64)),
              reads=[("gKT", NTT - 1)], writes=["kaH"], dma=True)
        S.add("dve", lambda e: e.memset(kaH[64:65, :, :], 1.0), writes=["kaH"])
        S.add("sp", lambda e: e.dma_start(out=vaH[:, :, 0:64], in_=gVT_t[NTT - 1][384:512, 0:128].rearrange("p (g d) -> p g d", d=64)),
              reads=[("gVT", NTT - 1)], writes=["vaH"], dma=True)
        S.add("sp", lambda e: e.dma_start(out=exps8, in_=sink_d[l:l + 1, :].to_broadcast([64, 8])), writes=["exps8"], dma=True)
        S.add("act", lambda e: e.activation(out=exps8, in_=exps8, func=AF.Exp), reads=["exps8"], writes=["exps8"])
        S.add("dve", lambda e: e.tensor_copy(out=exps, in_=bc_last(exps8, 128)), reads=["exps8"], writes=["exps"])
        for s in range(2):
            S.add("dve", lambda e, s=s: e.tensor_copy(out=flat2(qa[s][64:65, :, :]), in_=bc_cols(pfxc[64:65, 0:1], 1024)),
                  reads=["pfxc"], writes=[("qa", s)])
        for i in range(NT):
            s = i % 2
            S.add("sp", lambda e, i=i, s=s: e.dma_start(out=qa[s][0:64, :, :],
                                                        in_=qaT_d[:, :, i * 128:(i + 1) * 128].rearrange("h d t -> d h t")),
                  reads=[("qaT", i // 4)], writes=[("qa", s)], dma=True)
            for g in range(2):
                chunks = []
                if i == 0:
                    chunks.append((kaH[0:65, g, :], 65, vaH[:, g, :], T1all, ["kaH", "vaH"]))
                else:
                    chunks.append((kaT[:, g, (i - 1) * 128:i * 128], 64, vaO[:, i - 1, g, :], T1all,
                                   ["kaT", "vaO"]))
                chunks.append((kaT[:, g, i * 128:(i + 1) * 128], 64, vaO[:, i, g, :], T0all, ["kaT", "vaO"]))
                pO, pD = pb[4 + 2 * (g % 2)], pb[5 + 2 * (g % 2)]
                okey, dkey = ("pb", 4 + 2 * (g % 2)), ("pb", 5 + 2 * (g % 2))
                for ci, (kap, r, vap, Tt, kk) in enumerate(chunks):
                    ps = (2 * g + ci) % 4
                    S.add("pe", lambda e, kap=kap, r=r, ps=ps, s=s, g=g: e.matmul(
                        pb[ps], lhsT=kap, rhs=flat2(qa[s][0:r, 4 * g:4 * g + 4, :]), start=True, stop=False),
                        reads=kk + [("qa", s)], writes=[("pb", ps)])
                    S.add("pe", lambda e, Tt=Tt, ps=ps, g=g: e.matmul(
                        pb[ps], lhsT=ident, rhs=flat2(Tt[:, 4 * g:4 * g + 4, :]), start=False, stop=True),
                        reads=["ident", "T0all", "T1all"], writes=[("pb", ps)])
                    S.add("act", lambda e, ps=ps, ci=ci: e.activation(out=PT[ci], in_=pb[ps], func=AF.Exp),
                          reads=[("pb", ps)], writes=[("PT", ci)])
                for ci, (kap, r, vap, Tt, kk) in enumerate(chunks):
                    S.add("pe", lambda e, vap=vap, ci=ci, pO=pO: e.matmul(pO[0:65, :], lhsT=vap, rhs=PT[ci],
                                                                          start=(ci == 0), stop=(ci == 1)),
                          reads=kk + [("PT", ci)], writes=[okey])
                finalize(pO, pD, okey, dkey, 4,
                         catT_d[4 * g:4 * g + 4, :, i * 128:(i + 1) * 128].rearrange("h d t -> d h t"),
                         ("catT", i), extra=flat2(exps[:, 4 * g:4 * g + 4, :]), tmp=(dn, oc, osb))
        S.barrier()
        A.release(m)

    def moba_phase(l):
        m = A.mark()
        kb = A.alloc("kb", 97, [4, T2], BF16)
        vb = A.alloc("vb", 128, [NC2, 4, 65], BF16)
        kmf = A.alloc("kmf", 64, [4, GW], F32)
        kmT = A.alloc("kmT", 64, [4, GW], BF16)
        qb = [A.alloc("qb%d" % s, 97, [4, 128], BF16) for s in range(2)]
        bsel = [A.alloc("bsel%d" % s, 128, [4, 96], BF16) for s in range(2)]
        gt = A.alloc("gt", 128, [4, GW], F32)
        mx8 = A.alloc("mx8", 128, [4, 8], F32)
        thr = A.alloc("thr", 128, 4, F32)
        PT = [A.alloc("PT%d" % s, 128, 512, BF16) for s in range(4)]
        SBANK = [2, 3, 5, 7]
        dn = A.alloc("dn", 64, 512, F32)
        oc = A.alloc("oc", 64, 512, BF16)
        osb = A.alloc("osb", 65, 512, F32)
        S.add("dve", lambda e: e.memset(vb[:, :, :, 64:65], 1.0), writes=["vb"])
        for t in range(NTT):
            S.add("sp", lambda e, t=t: e.dma_start(out=kb[0:64, :, t * 512:(t + 1) * 512],
                                                   in_=gKT_t[t][128:384, :].rearrange("(h d) t -> d h t", d=64)),
                  reads=[("gKT", t)], writes=["kbk"], dma=True)
            S.add("sp", lambda e, t=t: e.dma_start(out=kb[0:64, :, TC + t * 512:TC + (t + 1) * 512],
                                                   in_=KT_t[t][128:384, :].rearrange("(h d) t -> d h t", d=64)),
                  reads=[("KTb", t)], writes=["kbk"], dma=True)
            for h in range(4):
                S.add("sp", lambda e, t=t, h=h: e.dma_start(
                    out=vb[:, 4 * t:4 * t + 4, h, 0:64],
                    in_=gVT_t[t][0:512, 128 + h * 64:128 + (h + 1) * 64].rearrange("(n p) d -> p n d", p=128)),
                    reads=[("gVT", t)], writes=["vb"], dma=True)
            for h in range(4):
                S.add("sp", lambda e, t=t, h=h: e.dma_start(
                    out=vb[:, NT + 4 * t:NT + 4 * t + 4, h, 0:64],
                    in_=VT_t[t][:, 128 + h * 64:128 + (h + 1) * 64].rearrange("(n p) d -> p n d", p=128)),
                    reads=[("VT", t)], writes=["vb"], dma=True)
        for h in range(4):
            S.add("pool", lambda e, h=h: e.dma_start(out=kb[64:97, h, :], in_=er_d), writes=["kbk"], dma=True)
        S.add("dve", lambda e: e.memset(kmf, 0.0), writes=["kmf"])
        for h in range(4):
            S.add("dve", lambda e, h=h: e.reduce_sum(out=kmf[:, h, 0:NB],
                                                     in_=kb[0:64, h, :].rearrange("p (n s) -> p n s", s=256), axis=AX.X),
                  reads=["kbk", "kmf"], writes=["kmf"])
        S.add("dve", lambda e: e.tensor_scalar(out=kmT, in0=kmf, scalar1=1.0 / 256, scalar2=None, op0=ALU.mult),
              reads=["kmf"], writes=["kmT"])
        for s in range(2):
            S.add("sp", lambda e, s=s: e.dma_start(out=qb[s][96:97, :, :], in_=b31s[0:1, 0:4, :]),
                  reads=["b31s"], writes=[("qb", s)], dma=True)
            S.add("dve", lambda e, s=s: e.memset(bsel[s][:, :, 0:64], 0.0), writes=[("bsel", s)])
            S.add("dve", lambda e, s=s: e.memset(bsel[s][:, :, 64:96], -1.0), writes=[("bsel", s)])
        for i in range(NT):
            s = i % 2
            nb = NBH + i // 2
            cq = NT + i
            ob0 = cq - (i % 2)
            S.add("sp", lambda e, i=i, s=s: e.dma_start(out=qb[s][0:64, :, :],
                                                        in_=qbT_d[:, :, i * 128:(i + 1) * 128].rearrange("h d t -> d h t")),
                  reads=[("qbT", i // 4)], writes=[("qb", s)], dma=True)
            for h in range(4):
                S.add("pe", lambda e, s=s, h=h: e.matmul(pb[0][:, h * GW:(h + 1) * GW], lhsT=qb[s][0:64, h, :],
                                                         rhs=kmT[:, h, :], start=(h == 0), stop=(h == 3)),
                      reads=[("qb", s), "kmT"], writes=[("pb", 0)])
            S.add("dve", lambda e: e.tensor_tensor(out=gt, in0=pb[0][:, 0:4 * GW].rearrange("p (h n) -> p h n", n=GW),
                                                   in1=bc_mid(pfxrow, 4), op=ALU.add),
                  reads=[("pb", 0), "pfxrow"], writes=["gt"])
            S.add("dve", lambda e, nb=nb: e.tensor_tensor(out=gt, in0=gt, in1=bc_mid(stepc[:, GW - nb:2 * GW - nb], 4), op=ALU.add),
                  reads=["gt", "stepc"], writes=["gt"])
            for h in range(4):
                S.add("dve", lambda e, h=h: e.max(out=mx8[:, h, :], in_=gt[:, h, :]), reads=["gt"], writes=["mx8"])
            S.add("dve", lambda e: e.tensor_scalar(out=thr, in0=mx8[:, :, 2], scalar1=-16000.0, scalar2=None, op0=ALU.max),
                  reads=["mx8"], writes=["thr"])
            for h in range(4):
                S.add("dve", lambda e, s=s, h=h, nb=nb: e.tensor_scalar(out=bsel[s][:, h, 64:64 + nb], in0=gt[:, h, 0:nb],
                                                                        scalar1=thr[:, h:h + 1], scalar2=-1.0,
                                                                        op0=ALU.is_ge, op1=ALU.add),
                      reads=["gt", "thr"], writes=[("bsel", s)])
            for h in range(4):
                S.add("pe", lambda e, s=s, h=h: e.transpose(out=pbh[1][0:96, h * 128:(h + 1) * 128], in_=bsel[s][:, h, :],
                                                            identity=ident),
                      reads=[("bsel", s), "ident"], writes=[("pb", 1)])
            S.add("act", lambda e, s=s: e.activation(out=flat2(qb[s][64:96, :, :]), in_=pbh[1][64:96, 0:512], func=AF.Copy),
                  reads=[("pb", 1)], writes=[("qb", s)])
            pO, pD = pb[4 + 2 * s], pb[1]
            okey, dkey = ("pb", 4 + 2 * s), ("pb", 1)
            for c in range(cq + 1):
                ps = SBANK[c % 4]
                Tt = None
                if c == cq:
                    r, Tt = 64, T0all
                elif c == cq - 1 and c >= ob0:
                    r, Tt = 64, T1all
                elif c == cq - 1:
                    r, Tt = 96, T1all
                else:
                    r = 97
                for h in range(4):
                    S.add("pe", lambda e, s=s, h=h, c=c, r=r, ps=ps, Tt=Tt: e.matmul(
                        pb[ps][:, h * 128:(h + 1) * 128], lhsT=kb[0:r, h, c * 128:(c + 1) * 128], rhs=qb[s][0:r, h, :],
                        start=(h == 0), stop=(Tt is None and h == 3)),
                        reads=["kbk", ("qb", s)], writes=[("pb", ps)])
                if Tt is not None:
                    S.add("pe", lambda e, ps=ps, Tt=Tt: e.matmul(pb[ps], lhsT=ident, rhs=flat2(Tt[:, 8:12, :]),
                                                                 start=False, stop=True),
                          reads=["ident", "T0all", "T1all"], writes=[("pb", ps)])
                S.add("act", lambda e, ps=ps, c=c: e.activation(out=PT[c % 4], in_=pb[ps], func=AF.Exp),
                      reads=[("pb", ps)], writes=[("PT", c % 4)])
                for h in range(4):
                    S.add("pe", lambda e, h=h, c=c, pO=pO, cq=cq: e.matmul(
                        pO[0:65, h * 128:(h + 1) * 128], lhsT=vb[:, c, h, :], rhs=PT[c % 4][:, h * 128:(h + 1) * 128],
                        start=(c == 0 and h == 0), stop=(c == cq and h == 3)),
                        reads=["vb", ("PT", c % 4)], writes=[okey])
            finalize(pO, pD, okey, dkey, 4, catT_d[8:12, :, i * 128:(i + 1) * 128].rearrange("h d t -> d h t"),
                     ("catT", i), tmp=(dn, oc, osb))
        S.barrier()
        A.release(m)

    def dsa_phase(l):
        m = A.mark()
        ki = A.alloc("ki", 64, T2, BF16)
        kc = A.alloc("kc", 65, T2, BF16)
        vc = A.alloc("vc", 128, [NC2, 65], BF16)
        Ib = A.alloc("Ib", 128, T2, F32)
        jk = A.alloc("jk", 128, T2, BF16)
        mb = [A.alloc("mb%d" % s, 128, T2, BF16) for s in range(2)]
        rl = [A.alloc("rl%d" % h, 128, 512, F32) for h in range(4)]
        qi = [A.alloc("qi%d" % s, 64, [4, 128], BF16) for s in range(2)]
        qc = [A.alloc("qc%d" % s, 65, [4, 128], BF16) for s in range(2)]
        wt = [A.alloc("wt%d" % s, 128, 16, F32) for s in range(2)]
        bs = A.alloc("bs", 128, 8, F32)
        PT = [A.alloc("PT%d" % s, 128, 512, BF16) for s in range(2)]
        dn = A.alloc("dn", 64, 512, F32)
        oc = A.alloc("oc", 64, 512, BF16)
        osb = A.alloc("osb", 65, 512, F32)
        lo, mid, cnt, dl = bs[:, 0:1], bs[:, 1:2], bs[:, 2:3], bs[:, 3:4]
        if l + 1 < L:
            precast([(l + 1, n_) for n_ in ("w1g", "w1u", "w1d", "win", "w2g", "w2u", "w2d")])
        S.add("dve", lambda e: e.memset(vc[:, :, 64:65], 1.0), writes=["vc"])
        for t in range(NTT):
            S.add("sp", lambda e, t=t: e.dma_start(out=ki[:, t * 512:(t + 1) * 512], in_=gKT_t[t][448:512, :]),
                  reads=[("gKT", t)], writes=["ki"], dma=True)
            S.add("sp", lambda e, t=t: e.dma_start(out=ki[:, TC + t * 512:TC + (t + 1) * 512], in_=KT_t[t][448:512, :]),
                  reads=[("KTi", t)], writes=["ki"], dma=True)
            S.add("sp", lambda e, t=t: e.dma_start(out=kc[0:64, t * 512:(t + 1) * 512], in_=gKT_t[t][384:448, :]),
                  reads=[("gKT", t)], writes=["kc"], dma=True)
            S.add("sp", lambda e, t=t: e.dma_start(out=kc[0:64, TC + t * 512:TC + (t + 1) * 512], in_=KT_t[t][384:448, :]),
                  reads=[("KTc", t)], writes=["kc"], dma=True)
            S.add("sp", lambda e, t=t: e.dma_start(out=vc[:, 4 * t:4 * t + 4, 0:64],
                                                   in_=gVT_t[t][0:512, 384:448].rearrange("(n p) c -> p n c", p=128)),
                  reads=[("gVT", t)], writes=["vc"], dma=True)
            S.add("sp", lambda e, t=t: e.dma_start(out=vc[:, NT + 4 * t:NT + 4 * t + 4, 0:64],
                                                   in_=VT_t[t][:, 384:448].rearrange("(n p) c -> p n c", p=128)),
                  reads=[("VT", t)], writes=["vc"], dma=True)
        S.add("dve", lambda e: e.memset(kc[64:65, :], 1.0), writes=["kc"])
        for s in range(2):
            S.add("sp", lambda e, s=s: e.dma_start(out=qc[s][64:65, :, :], in_=b31s[0:1, 4:8, :]),
                  reads=["b31s"], writes=[("qc", s)], dma=True)

        def stage_a(i):
            s = i % 2
            nkc = NT + i + 1
            nk = nkc * 128
            S.add("sp", lambda e: e.dma_start(out=qi[s], in_=qiT_d[:, :, i * 128:(i + 1) * 128].rearrange("h d t -> d h t")),
                  reads=[("qiT", i // 4)], writes=[("qi", s)], dma=True)
            S.add("sp", lambda e: e.dma_start(out=qc[s][0:64, :, :],
                                              in_=qcT_d[:, :, i * 128:(i + 1) * 128].rearrange("h d t -> d h t")),
                  reads=[("qcT", i // 4)], writes=[("qc", s)], dma=True)
            S.add("sp", lambda e: e.dma_start(out=wt[s], in_=wi_d[i * 128:(i + 1) * 128, :]),
                  reads=[("wi", i // 4)], writes=[("wt", s)], dma=True)
            nblk = (nk + 511) // 512
            for b in range(nblk):
                w = min(512, nk - b * 512)
                c0 = b * 512
                pfx_blk = (c0 + w) <= TC
                assert pfx_blk or c0 >= TC
                for h in range(4):
                    S.add("pe", lambda e, h=h, c0=c0, w=w: e.matmul(pb[h][:, 0:w], lhsT=qi[s][:, h, :], rhs=ki[:, c0:c0 + w],
                                                                    start=True, stop=True),
                          reads=[("qi", s), "ki"], writes=[("pb", h)])
                    S.add("act", lambda e, h=h, w=w: e.activation(out=rl[h][:, 0:w], in_=pb[h][:, 0:w], func=AF.Relu),
                          reads=[("pb", h)], writes=[("rl", h)])
                sc2 = pfxc[:, 0:1] if pfx_blk else zero1[:, 0:1]
                S.add("dve", lambda e, c0=c0, w=w, sc2=sc2: e.tensor_scalar(
                    out=Ib[:, c0:c0 + w], in0=rl[0][:, 0:w], scalar1=wt[s][:, 12:13], scalar2=sc2, op0=ALU.mult, op1=ALU.add),
                    reads=[("rl", 0), ("wt", s), "pfxc", "zero1"], writes=["Ib"])
                for h in range(1, 4):
                    S.add("dve", lambda e, h=h, c0=c0, w=w: e.scalar_tensor_tensor(
                        out=Ib[:, c0:c0 + w], in0=rl[h][:, 0:w], scalar=wt[s][:, 12 + h:13 + h], in1=Ib[:, c0:c0 + w],
                        op0=ALU.mult, op1=ALU.add),
                        reads=[("rl", h), ("wt", s), "Ib"], writes=["Ib"])
            S.add("dve", lambda e: e.tensor_tensor(out=Ib[:, nk - 128:nk], in0=Ib[:, nk - 128:nk], in1=cmt, op=ALU.add),
                  reads=["Ib", "cmt"], writes=["Ib"])
            S.add("dve", lambda e: e.memset(mid, LO0 + BRW / 2), writes=["mid"])
            for it in range(NIT):
                step = BRW / (2 ** (it + 1))
                S.add("dve", lambda e: e.tensor_scalar(out=jk[:, 0:nk], in0=Ib[:, 0:nk], scalar1=mid, scalar2=None,
                                                       op0=ALU.is_ge, op1=ALU.add, accum_out=cnt),
                      reads=["Ib", "mid"], writes=["jk", "cnt"])
                S.add("dve", lambda e: e.tensor_scalar(out=dl, in0=cnt, scalar1=256.0, scalar2=-0.5,
                                                       op0=ALU.is_ge, op1=ALU.add),
                      reads=["cnt"], writes=["dl"])
                if it + 1 < NIT:
                    S.add("dve", lambda e, step=step: e.scalar_tensor_tensor(out=mid, in0=dl, scalar=step, in1=mid,
                                                                             op0=ALU.mult, op1=ALU.add),
                          reads=["dl", "mid"], writes=["mid"])
                else:
                    S.add("dve", lambda e: e.tensor_scalar(out=dl, in0=dl, scalar1=-0.5, scalar2=step,
                                                           op0=ALU.add, op1=ALU.mult),
                          reads=["dl"], writes=["dl"])
                    S.add("dve", lambda e: e.tensor_tensor(out=lo, in0=mid, in1=dl, op=ALU.add), reads=["mid", "dl"], writes=["lo"])
            S.add("dve", lambda e: e.tensor_scalar(out=mb[s][:, 0:nk], in0=Ib[:, 0:nk], scalar1=lo, scalar2=-BIG,
                                                   op0=ALU.is_lt, op1=ALU.mult),
                  reads=["Ib", "lo"], writes=[("mb", s)])

        def stage_b(i):
            s = i % 2
            nkc = NT + i + 1
            pO, pD = pb[6], pb[7]
            okey, dkey = ("pb", 6), ("pb", 7)
            for c in range(nkc):
                ps = 4 + c % 2
                Tt = None
                if c == nkc - 1:
                    r, Tt = 64, T0all
                elif c == nkc - 2:
                    r, Tt = 64, T1all
                else:
                    r = 65
                S.add("pe", lambda e, c=c, r=r, ps=ps: e.matmul(pb[ps], lhsT=kc[0:r, c * 128:(c + 1) * 128],
                                                                rhs=flat2(qc[s][0:r, :, :]), start=True, stop=False),
                      reads=["kc", ("qc", s)], writes=[("pb", ps)])
                S.add("pe", lambda e, c=c, ps=ps, Tt=Tt: e.matmul(pb[ps], lhsT=mb[s][:, c * 128:(c + 1) * 128], rhs=flat2(ident4),
                                                                  start=False, stop=(Tt is None)),
                      reads=[("mb", s), "ident4"], writes=[("pb", ps)])
                if Tt is not None:
                    S.add("pe", lambda e, ps=ps, Tt=Tt: e.matmul(pb[ps], lhsT=ident, rhs=flat2(Tt[:, 12:16, :]),
                                                                 start=False, stop=True),
                          reads=["ident", "T0all", "T1all"], writes=[("pb", ps)])
                S.add("act", lambda e, ps=ps, c=c: e.activation(out=PT[c % 2], in_=pb[ps], func=AF.Exp),
                      reads=[("pb", ps)], writes=[("PT", c % 2)])
                S.add("pe", lambda e, c=c: e.matmul(pO[0:65, :], lhsT=vc[:, c, :], rhs=PT[c % 2], start=(c == 0), stop=(c == nkc - 1)),
                      reads=["vc", ("PT", c % 2)], writes=[okey])
            finalize(pO, pD, okey, dkey, 4, catT_d[12:16, :, i * 128:(i + 1) * 128].rearrange("h d t -> d h t"),
                     ("catT", i), tmp=(dn, oc, osb))

        stage_a(0)
        for i in range(NT):
            if i + 1 < NT:
                stage_a(i + 1)
            stage_b(i)
        S.barrier()
        A.release(m)

    def wout_phase(l):
        m = A.mark()
        wo = A.alloc("wo", 64, [16, D], BF16)
        xs = [A.alloc("xs%d" % s, 128, D, F32) for s in range(3)]
        ct = [A.alloc("ct%d" % s, 64, [16, 128], BF16) for s in range(2)]
        S.add("pool", lambda e: e.dma_start(out=wo[:, 0:8, :], in_=wo_d[l, 0:512, :].rearrange("(h d) m -> d h m", d=64)),
              writes=["wo"], dma=True)
        S.add("pool", lambda e: e.dma_start(out=wo[:, 8:16, :], in_=wo_d[l, 512:1024, :].rearrange("(h d) m -> d h m", d=64)),
              writes=["wo"], dma=True)
        for u in range(NT):
            sl = u % 3
            s = u % 2
            S.add("sp", lambda e, u=u, sl=sl: e.dma_start(out=xs[sl], in_=xres[u * 128:(u + 1) * 128, :]),
                  reads=[("xres", u)], writes=[("xs", sl)], dma=True)
            S.add("sp", lambda e, u=u, s=s: e.dma_start(out=ct[s], in_=catT_d[:, :, u * 128:(u + 1) * 128].rearrange("h d t -> d h t")),
                  reads=[("catT", u)], writes=[("ct", s)], dma=True)
            for half in range(2):
                py = (2 * u + half) % 4
                for h in range(16):
                    S.add("pe", lambda e, s=s, h=h, half=half, py=py: e.matmul(
                        pb[py], lhsT=ct[s][:, h, :], rhs=wo[:, h, half * 512:(half + 1) * 512], start=(h == 0), stop=(h == 15)),
                        reads=[("ct", s), "wo"], writes=[("pb", py)])
                S.add("dve", lambda e, sl=sl, half=half, py=py: e.tensor_tensor(
                    out=xs[sl][:, half * 512:(half + 1) * 512], in0=pb[py], in1=xs[sl][:, half * 512:(half + 1) * 512], op=ALU.add),
                    reads=[("pb", py), ("xs", sl)], writes=[("xs", sl)])
            S.add("sp", lambda e, u=u, sl=sl: e.dma_start(out=xres[u * 128:(u + 1) * 128, :], in_=xs[sl]),
                  reads=[("xs", sl)], writes=[("xres", u)], dma=True)
        S.barrier()
        A.release(m)

    def final_phase():
        m = A.mark()
        gb = A.alloc("gb", 128, D, F32)
        xs = [A.alloc("xs%d" % s, 128, D, F32) for s in range(3)]
        ob = [A.alloc("ob%d" % s, 128, D, F32) for s in range(2)]
        stat = A.alloc("stat", 128, [3, 4], F32)
        junk = A.alloc("junk", 128, D, BF16)
        S.add("sp", lambda e: e.dma_start(out=gb, in_=nf_d.to_broadcast([128, D])), writes=["gb"], dma=True)
        for u in range(NT):
            sl = u % 3
            S.add("sp", lambda e, u=u, sl=sl: e.dma_start(out=xs[sl], in_=xres[u * 128:(u + 1) * 128, :]),
                  reads=[("xres", u)], writes=[("xs", sl)], dma=True)
            norm_sub(xs[sl], ("xs", sl), gb, "gb", stat[:, sl, :], ("stat", sl), ob[u % 2], ("ob", u % 2), junk)
            S.add("sp", lambda e, u=u: e.dma_start(out=out_d[u * 128:(u + 1) * 128, :], in_=ob[u % 2]),
                  reads=[("ob", u % 2)], writes=[("out", u)], dma=True)
        S.barrier()
        A.release(m)

    def dump(name, src):
        S.add("sp", lambda e: e.dma_start(out=dbg[name], in_=src), writes=[("dbg", name)], dma=True)
        S.barrier()

    plist = [("phase0", phase0)]
    for l in range(L):
        plist.append(("ffn1_%d" % l, lambda l=l: ffn_phase(l, w1g_d, w1u_d, w1d_d, n1_d[l:l + 1, :], x_in if l == 0 else xres, xres,
                                                          pre=(None if l == 0 else ("w1g", "w1u", "w1d")))))
        plist.append(("proj_%d" % l, lambda l=l: proj_phase(l)))
        plist.append(("xchg_%d" % l, exchange))
        plist.append(("swa_%d" % l, lambda l=l: swa_phase(l)))
        plist.append(("moba_%d" % l, lambda l=l: moba_phase(l)))
        plist.append(("dsa_%d" % l, lambda l=l: dsa_phase(l)))
        plist.append(("wout_%d" % l, lambda l=l: wout_phase(l)))
        plist.append(("ffn2_%d" % l, lambda l=l: ffn_phase(l, w2g_d, w2u_d, w2d_d, n2_d[l:l + 1, :], xres, xres,
                                                          pre=("w2g", "w2u", "w2d"))))
    plist.append(("final", final_phase))
    for name, fn in plist:
        S.tag = name
        fn()
        if upto is not None and name == upto:
            break
    if debug:
        S.tag = "dump"
        dump("xres", xres)
        dump("catT", catT_d)
        dump("qaT", qaT_d)
    S.add("act", lambda e: e.activation(out=zero1, in_=zero1, func=AF.Copy), reads=["zero1"], writes=["zero1"])
    S.add("dve", lambda e: e.memset(zero1, 0.0), writes=["zero1"])
    S.add("pool", lambda e: e.memset(zero1, 0.0), writes=["zero1"])
    S.add("sp", lambda e: e.dma_start(out=pfxc, in_=pfx_d.to_broadcast([128, 2])), writes=["pfxc"], dma=True)
    S.emit(stack)
    stack.close()
    return nc, S


_CACHE = {}


def make_in_maps(inputs, SEQ, DEPTH):
    f = lambda a: np.ascontiguousarray(np.asarray(a, dtype=np.float32))
    x = f(inputs["x"])
    B = x.shape[0]
    TC = SEQ // 2
    oh, er, cm = host_consts(SEQ)
    shared = {
        "tab": f(inputs["rel_bias_table"]),
        "n1": f(inputs["ffn1_norm"]), "w1g": f(inputs["ffn1_w_gate"]), "w1u": f(inputs["ffn1_w_up"]),
        "w1d": f(inputs["ffn1_w_down"]), "nm": f(inputs["mix_norm"]), "win": f(inputs["w_in"]),
        "sinks": f(inputs["attn_sinks"]), "kvn": f(inputs["kv_norm_c"]), "wup": f(inputs["w_kv_up_c"]),
        "wo": f(inputs["w_out"]), "n2": f(inputs["ffn2_norm"]), "w2g": f(inputs["ffn2_w_gate"]),
        "w2u": f(inputs["ffn2_w_up"]), "w2d": f(inputs["ffn2_w_down"]),
        "nf": f(inputs["final_norm"]).reshape(1, -1),
        "oh": oh, "er": er, "cm": cm,
    }
    maps = []
    for c in range(2 * B):
        b, half = c // 2, c % 2
        mp = dict(shared)
        mp["x"] = np.ascontiguousarray(x[b, half * TC:(half + 1) * TC, :])
        mp["pfx"] = np.array([[-BIG if half == 0 else 0.0, 0.0]], np.float32)
        maps.append(mp)
    return maps


def kernel(**inputs):
    x = np.asarray(inputs["x"])
    B, SEQ, _ = x.shape
    DEPTH = np.asarray(inputs["ffn1_norm"]).shape[0]
    assert B == 4
    key = (SEQ, DEPTH)
    if key not in _CACHE:
        _CACHE[key] = build_program(SEQ, DEPTH)[0]
    nc = _CACHE[key]
    maps = make_in_maps(inputs, SEQ, DEPTH)
    res = run_bass_kernel_spmd(nc, maps, core_ids=list(range(8)))
    TC = SEQ // 2
    out = np.empty((B, SEQ, D), np.float32)
    for c in range(8):
        b, half = c // 2, c % 2
        out[b, half * TC:(half + 1) * TC, :] = np.asarray(res.results[c]["out"], dtype=np.float32)
    return out
```

```python
import math
import os
from contextlib import ExitStack

import numpy as np
import concourse.bass as bass
import concourse.mybir as mybir
from concourse.bass_utils import run_bass_kernel_spmd

F32 = mybir.dt.float32
BF16 = mybir.dt.bfloat16
U8 = mybir.dt.uint8
AF = mybir.ActivationFunctionType
ALU = mybir.AluOpType
AX = mybir.AxisListType

DT_SIZE = {F32: 4, BF16: 2, U8: 1}
ENGS = ["pe", "act", "dve", "pool", "sp"]
EPOCH = 20000
NSLOT = {"sp": 8, "act": 4, "pool": 8}

D = 1024
FF = 2816
NJ = FF // 128
DIN = 2244
BIG = 32768.0
EPS = 1e-6
NIT = 24
LO0 = -64.0
BRW = 128.0


class Op:
    __slots__ = ("eng", "fn", "dma", "deps", "signal", "sig", "slotprev", "cc", "tag")

    def __init__(self, eng, fn, dma):
        self.eng = eng
        self.fn = fn
        self.dma = dma
        self.deps = []
        self.signal = False
        self.sig = None
        self.slotprev = None
        self.cc = False


class Sched:
    def __init__(self, nc):
        self.nc = nc
        self.ops = {e: [] for e in ENGS}
        self.lastw = {}
        self.readers = {}
        self.pending = {e: [] for e in ENGS}
        self.dma_hist = {e: [] for e in ENGS}
        self.last_comp = {e: None for e in ENGS}
        self.tag = ""
        self.names = {}

    def add(self, eng, fn, reads=(), writes=(), dma=False, cc=False, bg=False):
        op = Op(eng, fn, dma)
        op.cc = cc
        op.tag = (self.tag, tuple(reads), tuple(writes))
        deps = []
        seen = set()

        def push(d):
            if d is None or id(d) in seen:
                return
            seen.add(id(d))
            if d.eng == "pe" and eng == "pe" and not d.dma and not dma:
                return
            deps.append(d)

        psum_reads = [k for k in reads if (k == "pb0" or (isinstance(k, tuple) and k[0] == "pb"))]
        if psum_reads:
            reads = [k for k in reads if k not in psum_reads]
            writes = list(writes) + psum_reads
        for k in reads:
            push(self.lastw.get(k))
        for k in writes:
            push(self.lastw.get(k))
            for r in self.readers.get(k, ()):
                push(r)
        for d in self.pending[eng]:
            push(d)
        self.pending[eng] = []
        op.deps = deps
        for d in deps:
            d.signal = True
        for k in writes:
            self.lastw[k] = op
            self.readers[k] = []
        for k in reads:
            lst = self.readers.setdefault(k, [])
            if not dma:
                lst[:] = [r for r in lst if not (r.eng == eng and not r.dma)]
            lst.append(op)
        self.ops[eng].append(op)
        if bg:
            pass
        elif dma or cc:
            h = self.dma_hist[eng]
            h.append(op)
            if len(h) > NSLOT[eng] + 2:
                h.pop(0)
        else:
            self.last_comp[eng] = op
        return op

    def barrier(self):
        tails = []
        for e in ENGS:
            if self.last_comp[e] is not None:
                tails.append(self.last_comp[e])
            tails.extend(self.dma_hist[e])
        for e in ENGS:
            self.pending[e] = list(tails)

    def emit(self, stack):
        nc = self.nc
        names = set()
        for e in ENGS:
            cnt = 0
            ndma = 0
            slot_last = {}
            ncc = 0
            for op in self.ops[e]:
                if op.dma:
                    R = NSLOT[e]
                    slot = ndma % R
                    val = 16 * (ndma // R + 1)
                    op.sig = ("d_%s_%d" % (e, slot), val, 16)
                    op.slotprev = slot_last.get(slot)
                    slot_last[slot] = op
                    ndma += 1
                elif op.cc:
                    ncc += 1
                    op.sig = ("cc_%s" % e, ncc, 1)
                elif op.signal:
                    ep = cnt // EPOCH
                    op.sig = ("c_%s_%d" % (e, ep), cnt - ep * EPOCH + 1, 1)
                    cnt += 1
                if op.sig is not None:
                    names.add(op.sig[0])
        sems = {}
        for name in sorted(names):
            sems[name] = stack.enter_context(nc.semaphore(name))
        block = stack.enter_context(nc.Block())
        stats = {"wait": 0, "ins": 0}

        def run(e):
            def body(engine):
                seenv = {}
                for op in self.ops[e]:
                    dl = list(op.deps)
                    if op.slotprev is not None:
                        dl.append(op.slotprev)
                    for d in dl:
                        name, val, _ = d.sig
                        if seenv.get(name, 0) >= val:
                            continue
                        engine.wait_ge(sems[name], val)
                        stats["wait"] += 1
                        seenv[name] = val
                    ins = op.fn(engine)
                    try:
                        self.names[str(ins.ins.name)] = op.tag
                    except Exception:
                        pass
                    stats["ins"] += 1
                    if op.sig is not None:
                        name, val, inc = op.sig
                        ins.then_inc(sems[name], inc)
            return body

        block.tensor(run("pe"))
        block.scalar(run("act"))
        block.vector(run("dve"))
        block.gpsimd(run("pool"))
        block.sync(run("sp"))
        self.stats = stats


class Arena:
    def __init__(self, nc, nbytes):
        self.h = nc.alloc_sbuf_tensor("arena", [128, nbytes], U8)
        self.ap = self.h.ap()
        self.nbytes = nbytes
        self.off = 0

    def alloc(self, name, parts, free, dt):
        if isinstance(free, int):
            free = [free]
        n = 1
        for f in free:
            n *= f
        nb = n * DT_SIZE[dt]
        off = (self.off + 63) // 64 * 64
        assert off + nb <= self.nbytes, "SBUF arena overflow %s: %d + %d > %d" % (name, off, nb, self.nbytes)
        self.off = off + nb
        ap = self.ap[0:parts, off:off + nb].bitcast(dt)
        if len(free) == 2:
            ap = ap.rearrange("p (a b) -> p a b", b=free[1])
        elif len(free) == 3:
            ap = ap.rearrange("p (a b c) -> p a b c", b=free[1], c=free[2])
        return ap

    def mark(self):
        return self.off

    def release(self, m):
        self.off = m


def bc_mid(a, n):
    return bass.AP(a.tensor, a.offset, [list(a.ap[0]), [0, n], list(a.ap[1])])


def bc_last(a, n):
    return bass.AP(a.tensor, a.offset, [list(a.ap[0]), list(a.ap[1]), [0, n]])


def bc_cols(a, n):
    return bass.AP(a.tensor, a.offset, [list(a.ap[0]), [0, n]])


def flat2(a):
    if len(a.shape) == 3:
        return a.rearrange("p a b -> p (a b)")
    return a


def _t5_bucket_np(n):
    n = np.maximum(n, 0)
    nf = np.maximum(n, 1).astype(np.float32)
    large = 16 + (np.log(nf / np.float32(16)) / np.float32(math.log(128 / 16)) * np.float32(16)).astype(np.int32)
    large = np.minimum(large, 31)
    return np.where(n < 16, n, large)


def host_consts(SEQ):
    oh = np.zeros((2, 33, 384), np.float32)
    for m in range(384):
        d = m - 128
        b = int(_t5_bucket_np(np.array([max(d, 0)]))[0])
        oh[0, 32 if d < 0 else b, m] = 1.0
        oh[1, 32 if (d < 0 or d >= 128) else b, m] = 1.0
    er = np.zeros((33, SEQ), np.float32)
    for n in range(min(32, SEQ // 256)):
        er[n, n * 256:(n + 1) * 256] = BIG
    er[32, :] = 1.0
    q = np.arange(128)[:, None]
    j = np.arange(128)[None, :]
    cm = np.where(j <= q, 0.0, -BIG).astype(np.float32)
    return oh, er, cm


def build_program(SEQ, DEPTH, debug=False, upto=None):
    TC = SEQ // 2
    NT = TC // 128
    NTT = TC // 512
    T2 = SEQ
    NC2 = T2 // 128
    NB = SEQ // 256
    NBH = NB // 2
    GW = max(NB, 8)

    nc = bass.Bass("TRN2", target_bir_lowering=False)
    L = DEPTH

    def din(name, shape, dt=F32):
        return nc.dram_tensor(name, list(shape), dt, kind="ExternalInput").ap()

    def dscr(name, shape, dt=BF16):
        return nc.dram_tensor(name, list(shape), dt, kind="Internal").ap()

    x_in = din("x", [TC, D])
    tab_d = din("tab", [32, 16])
    n1_d = din("n1", [L, D])
    w1g_d = din("w1g", [L, D, FF])
    w1u_d = din("w1u", [L, D, FF])
    w1d_d = din("w1d", [L, FF, D])
    nm_d = din("nm", [L, D])
    win_d = din("win", [L, D, DIN])
    sink_d = din("sinks", [L, 8])
    kvn_d = din("kvn", [L, 128])
    wup_d = din("wup", [L, 128, 128])
    wo_d = din("wo", [L, D, D])
    n2_d = din("n2", [L, D])
    w2g_d = din("w2g", [L, D, FF])
    w2u_d = din("w2u", [L, D, FF])
    w2d_d = din("w2d", [L, FF, D])
    nf_d = din("nf", [1, D])
    oh_d = din("oh", [2, 33, 384])
    er_d = din("er", [33, T2])
    cm_d = din("cm", [128, 128])
    pfx_d = din("pfx", [1, 2])
    out_d = nc.dram_tensor("out", [TC, D], F32, kind="ExternalOutput").ap()

    xres = dscr("xres", [TC, D], F32)
    qaT_d = dscr("qaT", [8, 64, TC])
    qbT_d = dscr("qbT", [4, 64, TC])
    qcT_d = dscr("qcT", [4, 64, TC])
    qiT_d = dscr("qiT", [4, 64, TC])
    wi_d = dscr("wi", [TC, 16], F32)
    KT_t = [dscr("KT%d" % t, [512, 512]) for t in range(NTT)]
    VT_t = [dscr("VT%d" % t, [512, 448]) for t in range(NTT)]
    gKT_t = [dscr("gKT%d" % t, [1024, 512]) for t in range(NTT)]
    gVT_t = [dscr("gVT%d" % t, [1024, 448]) for t in range(NTT)]
    catT_d = dscr("catT", [16, 64, TC])
    Zf_d = dscr("Zf", [16, 128, 384], F32)
    ZA_d = dscr("ZA", [8, 128, 384], F32)
    wbf = {}
    for l in range(L):
        for nm_, shp in (("w1g", [D, FF]), ("w1u", [D, FF]), ("w1d", [FF, D]), ("w2g", [D, FF]), ("w2u", [D, FF]),
                         ("w2d", [FF, D]), ("win", [D, DIN])):
            if l == 0 and nm_ in ("w1g", "w1u", "w1d"):
                continue
            wbf[(l, nm_)] = dscr("wbf_%s_%d" % (nm_, l), shp)
    dbg = {}
    if debug:
        dbg["xres"] = nc.dram_tensor("dbg_xres", [TC, D], F32, kind="ExternalOutput").ap()
        dbg["catT"] = nc.dram_tensor("dbg_catT", [16, 64, TC], BF16, kind="ExternalOutput").ap()
        dbg["qaT"] = nc.dram_tensor("dbg_qaT", [8, 64, TC], BF16, kind="ExternalOutput").ap()

    stack = ExitStack()
    S = Sched(nc)
    A = Arena(nc, 210000)
    pb = [nc.alloc_psum_tensor("pb%d" % i, [128, 512], F32).ap() for i in range(8)]
    pbh = [p.bitcast(BF16) for p in pb]

    ident = A.alloc("ident", 128, 128, BF16)
    ident4 = A.alloc("ident4", 128, [4, 128], BF16)
    ones64 = A.alloc("ones64", 128, 64, BF16)
    T0all = A.alloc("T0all", 128, [16, 128], BF16)
    T1all = A.alloc("T1all", 128, [16, 128], BF16)
    cmt = A.alloc("cmt", 128, 128, F32)
    pfxc = A.alloc("pfxc", 128, 2, F32)
    pfxrow = A.alloc("pfxrow", 128, GW, F32)
    stepc = A.alloc("stepc", 128, 2 * GW, F32)
    zero1 = A.alloc("zero1", 128, 1, F32)
    selT = A.alloc("selT", 65, 64, F32)
    tab31 = A.alloc("tab31", 128, 16, F32)
    b31s = A.alloc("b31s", 1, [8, 128], BF16)

    def phase0():
        m = A.mark()
        identf = A.alloc("identf", 128, 128, F32)
        tabf = A.alloc("tabf", 33, 16, F32)
        ohb = A.alloc("ohb", 33, [2, 384], BF16)
        lh = [A.alloc("lh%d" % i, 33, 128, BF16) for i in range(2)]
        zsb = [A.alloc("zsb%d" % i, 128, 384, F32) for i in range(2)]
        S.add("pool", lambda e: e.memset(identf, 0.0), writes=["identf"])
        S.add("pool", lambda e: e.affine_select(out=identf, in_=identf, pattern=[[-1, 128]],
                                                compare_op=ALU.not_equal, fill=1.0, base=0,
                                                channel_multiplier=1),
              reads=["identf"], writes=["identf"])
        S.add("dve", lambda e: e.tensor_copy(out=ident, in_=identf), reads=["identf"], writes=["ident"])
        S.add("dve", lambda e: e.tensor_copy(out=ident4, in_=bc_mid(identf, 4)), reads=["identf"], writes=["ident4"])
        S.add("dve", lambda e: e.memset(ones64, 1.0), writes=["ones64"])
        S.add("dve", lambda e: e.memset(selT[0:64, :], 0.0), writes=["selT"])
        S.add("dve", lambda e: e.memset(selT[64:65, :], 1.0), writes=["selT"])
        S.add("dve", lambda e: e.memset(zero1, 0.0), writes=["zero1"])
        S.add("sp", lambda e: e.dma_start(out=cmt, in_=cm_d), writes=["cmt"], dma=True)
        S.add("sp", lambda e: e.dma_start(out=pfxc, in_=pfx_d.to_broadcast([128, 2])), writes=["pfxc"], dma=True)
        S.add("sp", lambda e: e.dma_start(out=tab31, in_=tab_d[31:32, :].to_broadcast([128, 16])), writes=["tab31"], dma=True)
        for j in range(8):
            S.add("dve", lambda e, j=j: e.tensor_copy(out=b31s[0:1, j, :], in_=bc_cols(tab31[0:1, 8 + j:9 + j], 128)),
                  reads=["tab31"], writes=["b31s"])
        S.add("dve", lambda e: e.memset(pfxrow, 0.0), writes=["pfxrow"])
        S.add("dve", lambda e: e.tensor_scalar(out=pfxrow[:, 0:NBH], in0=pfxrow[:, 0:NBH], scalar1=pfxc[:, 0:1],
                                               scalar2=None, op0=ALU.add),
              reads=["pfxrow", "pfxc"], writes=["pfxrow"])
        S.add("dve", lambda e: e.memset(stepc[:, 0:GW], 0.0), writes=["stepc"])
        S.add("dve", lambda e: e.memset(stepc[:, GW:2 * GW], -BIG), writes=["stepc"])
        S.add("sp", lambda e: e.dma_start(out=tabf[0:32, :], in_=tab_d), writes=["tabf"], dma=True)
        S.add("dve", lambda e: e.memset(tabf[32:33, :], -BIG), writes=["tabf"])
        S.add("pool", lambda e: e.dma_start(out=ohb, in_=oh_d.rearrange("w r m -> r w m")), writes=["ohb"], dma=True)
        n = 0
        for h in range(16):
            for which in range(2):
                if which == 1 and h >= 8:
                    continue
                s = n % 2
                n += 1
                S.add("dve", lambda e, s=s, h=h: e.tensor_copy(out=lh[s], in_=bc_cols(tabf[0:33, h:h + 1], 128)),
                      reads=["tabf"], writes=[("lh", s)])
                S.add("pe", lambda e, s=s, which=which: e.matmul(pb[s][:, 0:384], lhsT=lh[s], rhs=ohb[:, which, :],
                                                                  start=True, stop=True),
                      reads=[("lh", s), "ohb"], writes=[("pb", s)])
                S.add("act", lambda e, s=s: e.activation(out=zsb[s], in_=pb[s][:, 0:384], func=AF.Copy),
                      reads=[("pb", s)], writes=[("zsb", s)])
                dst = (ZA_d if which == 1 else Zf_d)[h]
                zkey = ("Z", which, h)
                S.add("sp", lambda e, s=s, dst=dst: e.dma_start(out=dst, in_=zsb[s]),
                      reads=[("zsb", s)], writes=[zkey], dma=True)
        for h in range(16):
            zt = Zf_d.tensor
            base = h * 128 * 384
            S.add("pool", lambda e, h=h, base=base: e.dma_start(
                out=T0all[:, h, :], in_=bass.AP(Zf_d.tensor, base + 128, [[383, 128], [1, 128]])),
                reads=[("Z", 0, h)], writes=["T0all"], dma=True)
            if h < 8:
                S.add("pool", lambda e, h=h, base=base: e.dma_start(
                    out=T1all[:, h, :], in_=bass.AP(ZA_d.tensor, base + 256, [[383, 128], [1, 128]])),
                    reads=[("Z", 1, h)], writes=["T1all"], dma=True)
            else:
                S.add("pool", lambda e, h=h, base=base: e.dma_start(
                    out=T1all[:, h, :], in_=bass.AP(Zf_d.tensor, base + 256, [[383, 128], [1, 128]])),
                    reads=[("Z", 0, h)], writes=["T1all"], dma=True)
        S.barrier()
        A.release(m)

    def norm_sub(xs_ap, xkey, gtile, gkey, stat, skey, hb_ap, hkey, junk, width=D):
        if junk is None:
            jout, jkey = hb_ap, hkey
        else:
            jout, jkey = junk[:, 0:width], "junk"
        S.add("act", lambda e: e.activation(out=jout, in_=xs_ap, func=AF.Square, accum_out=stat[:, 0:1]),
              reads=[xkey], writes=[jkey, skey])
        S.add("act", lambda e: e.activation(out=stat[:, 1:2], in_=stat[:, 0:1], func=AF.Sqrt, scale=1.0 / width, bias=EPS),
              reads=[skey], writes=[skey])
        S.add("dve", lambda e: e.reciprocal(out=stat[:, 2:3], in_=stat[:, 1:2]), reads=[skey], writes=[skey])
        S.add("dve", lambda e: e.scalar_tensor_tensor(out=hb_ap, in0=xs_ap, scalar=stat[:, 2:3], in1=gtile,
                                                      op0=ALU.mult, op1=ALU.mult),
              reads=[xkey, skey, gkey], writes=[hkey])

    def precast(order):
        srcs = {"w1g": w1g_d, "w1u": w1u_d, "w1d": w1d_d, "w2g": w2g_d, "w2u": w2u_d, "w2d": w2d_d, "win": win_d}
        for (l, nm_) in order:
            dstt = wbf[(l, nm_)]
            rows = dstt.shape[0]
            nparts = 4
            step = rows // nparts
            for p in range(nparts):
                r0, r1 = p * step, (rows if p == nparts - 1 else (p + 1) * step)
                S.add("pool", lambda e, l=l, nm_=nm_, r0=r0, r1=r1, dstt=dstt: e.dma_start(out=dstt[r0:r1, :], in_=srcs[nm_][l, r0:r1, :]),
                      writes=[("wbf", l, nm_, p)], dma=True, bg=True)

    def ffn_phase(l, wg_d, wu_d, wd_d, norm_row, src, dst, pre=None):
        m = A.mark()
        wg = A.alloc("wg", 128, [8, FF], BF16)
        wu = A.alloc("wu", 128, [8, FF], BF16)
        wd = A.alloc("wd", 128, [NJ, D], BF16)
        gb = A.alloc("gb", 128, D, F32)
        NXS = 5
        xs = [A.alloc("xs%d" % s, 128, D, F32) for s in range(NXS)]
        hb1 = A.alloc("hb0", 128, D, BF16)
        hb = [hb1, hb1]
        hT = A.alloc("hT", 128, [8, 512], BF16)
        actT = A.alloc("actT", 128, [NJ, 512], BF16)
        sg = [A.alloc("sg%d" % s, 128, 512, F32) for s in range(2)]
        stat = A.alloc("stat", 128, [NXS, 4], F32)
        junk = None
        S.add("sp", lambda e: e.dma_start(out=gb, in_=norm_row.to_broadcast([128, D])), writes=["gb"], dma=True)
        if pre is None:
            for k in range(8):
                S.add("pool", lambda e, k=k: e.dma_start(out=wg[:, k, :], in_=wg_d[l, k * 128:(k + 1) * 128, :]),
                      writes=[("wg", k)], dma=True)
                S.add("pool", lambda e, k=k: e.dma_start(out=wu[:, k, :], in_=wu_d[l, k * 128:(k + 1) * 128, :]),
                      writes=[("wu", k)], dma=True)
            for j in range(NJ):
                S.add("pool", lambda e, j=j: e.dma_start(out=wd[:, j, :], in_=wd_d[l, j * 128:(j + 1) * 128, :]),
                      writes=[("wd", j)], dma=True)
            precast([(0, "win"), (0, "w2g"), (0, "w2u"), (0, "w2d")])
        else:
            gname, uname, dname = pre
            gb_, ub_, db_ = wbf[(l, gname)], wbf[(l, uname)], wbf[(l, dname)]
            allk = lambda nm_: [("wbf", l, nm_, p) for p in range(4)]
            nq = 0
            for k in range(8):
                S.add("sp" if nq % 2 == 0 else "act", lambda e, k=k: e.dma_start(out=wg[:, k, :], in_=gb_[k * 128:(k + 1) * 128, :]),
                      reads=allk(gname), writes=[("wg", k)], dma=True)
                nq += 1
                S.add("sp" if nq % 2 == 0 else "act", lambda e, k=k: e.dma_start(out=wu[:, k, :], in_=ub_[k * 128:(k + 1) * 128, :]),
                      reads=allk(uname), writes=[("wu", k)], dma=True)
                nq += 1
            for j in range(NJ):
                S.add("sp" if nq % 2 == 0 else "act", lambda e, j=j: e.dma_start(out=wd[:, j, :], in_=db_[j * 128:(j + 1) * 128, :]),
                      reads=allk(dname), writes=[("wd", j)], dma=True)
                nq += 1
        for tt in range(NTT):
            for sub in range(4):
                u = tt * 4 + sub
                sl = u % NXS
                S.add("sp", lambda e, u=u, sl=sl: e.dma_start(out=xs[sl], in_=src[u * 128:(u + 1) * 128, :]),
                      reads=[("xres", u)], writes=[("xs", sl)], dma=True)
                norm_sub(xs[sl], ("xs", sl), gb, "gb", stat[:, sl, :], ("stat", sl), hb[0], ("hb", 0), junk)
                for k in range(8):
                    S.add("pe", lambda e, u=u, k=k: e.transpose(out=pbh[0][:, k * 128:(k + 1) * 128],
                                                                in_=hb[0][:, k * 128:(k + 1) * 128], identity=ident),
                          reads=[("hb", 0), "ident"], writes=["pb0"])
                S.add("act", lambda e, sub=sub: e.activation(
                    out=hT[:, :, sub * 128:(sub + 1) * 128],
                    in_=pbh[0].rearrange("p (k t) -> p k t", t=128), func=AF.Copy),
                    reads=["pb0"], writes=[("hT", sub)])
            hkeys = [("hT", s) for s in range(4)]
            for j in range(NJ):
                pg = 1 + j % 2
                pu = 3 + j % 2
                for k in range(8):
                    S.add("pe", lambda e, j=j, k=k, pg=pg: e.matmul(pb[pg], lhsT=wg[:, k, j * 128:(j + 1) * 128],
                                                                    rhs=hT[:, k, :], start=(k == 0), stop=(k == 7)),
                          reads=[("wg", k)] + hkeys, writes=[("pb", pg)])
                for k in range(8):
                    S.add("pe", lambda e, j=j, k=k, pu=pu: e.matmul(pb[pu], lhsT=wu[:, k, j * 128:(j + 1) * 128],
                                                                    rhs=hT[:, k, :], start=(k == 0), stop=(k == 7)),
                          reads=[("wu", k)] + hkeys, writes=[("pb", pu)])
                S.add("act", lambda e, j=j, pg=pg: e.activation(out=sg[j % 2], in_=pb[pg], func=AF.Silu),
                      reads=[("pb", pg)], writes=[("sg", j % 2)])
                S.add("dve", lambda e, j=j, pu=pu: e.tensor_tensor(out=actT[:, j, :], in0=sg[j % 2], in1=pb[pu], op=ALU.mult),
                      reads=[("sg", j % 2), ("pb", pu)], writes=[("actT", j)])
            n = 0
            for sub in range(4):
                u = tt * 4 + sub
                sl = u % NXS
                for half in range(2):
                    py = 5 + n % 2
                    n += 1
                    for j in range(NJ):
                        S.add("pe", lambda e, j=j, sub=sub, half=half, py=py: e.matmul(
                            pb[py], lhsT=actT[:, j, sub * 128:(sub + 1) * 128],
                            rhs=wd[:, j, half * 512:(half + 1) * 512], start=(j == 0), stop=(j == NJ - 1)),
                            reads=[("actT", j), ("wd", j)], writes=[("pb", py)])
                    S.add("dve", lambda e, sl=sl, half=half, py=py: e.scalar_tensor_tensor(
                        out=xs[sl][:, half * 512:(half + 1) * 512], in0=pb[py], scalar=0.5,
                        in1=xs[sl][:, half * 512:(half + 1) * 512], op0=ALU.mult, op1=ALU.add),
                        reads=[("pb", py), ("xs", sl)], writes=[("xs", sl)])
                S.add("sp", lambda e, u=u, sl=sl: e.dma_start(out=dst[u * 128:(u + 1) * 128, :], in_=xs[sl]),
                      reads=[("xs", sl)], writes=[("xres", u)], dma=True)
        S.barrier()
        A.release(m)

    FM_COLS = [(0, 512), (512, 640), (768, 1024), (1024, 1280), (1536, 1792), (1920, 2176), (2176, 2240)]
    QSCALE = [True] * 8 + [False] * 2 + [True] * 4 + [False] * 4 + [True] * 4 + [True] * 4 + [False]

    def proj_phase(l):
        m = A.mark()
        wfm = A.alloc("wfm", 128, [8, 1728], BF16)
        wtm = A.alloc("wtm", 128, [8, 576], BF16)
        wupb = A.alloc("wupb", 128, 128, BF16)
        gb = A.alloc("gb", 128, D, F32)
        gkv = A.alloc("gkv", 128, 128, F32)
        xs = [A.alloc("xs%d" % s, 128, D, F32) for s in range(2)]
        hb = [A.alloc("hb%d" % s, 128, D, BF16) for s in range(2)]
        hT = A.alloc("hT", 128, [8, 512], BF16)
        hd = A.alloc("hd", 64, [28, 512], BF16)
        vtm = A.alloc("vtm", 128, [4, 448], BF16)
        ck = A.alloc("ck", 128, 128, F32)
        ckn = A.alloc("ckn", 128, 128, BF16)
        cknT = A.alloc("cknT", 128, 512, BF16)
        wis = A.alloc("wis", 128, [4, 16], F32)
        stat = A.alloc("stat", 128, [2, 4], F32)
        stat2 = A.alloc("stat2", 128, 4, F32)
        junk = A.alloc("junk", 128, D, BF16)
        S.add("sp", lambda e: e.dma_start(out=gb, in_=nm_d[l:l + 1, :].to_broadcast([128, D])), writes=["gb"], dma=True)
        S.add("sp", lambda e: e.dma_start(out=gkv, in_=kvn_d[l:l + 1, :].to_broadcast([128, 128])), writes=["gkv"], dma=True)
        S.add("pool", lambda e: e.dma_start(out=wupb, in_=wup_d[l]), writes=["wupb"], dma=True)
        winb = wbf[(l, "win")]
        wkeys = [("wbf", l, "win", p) for p in range(4)]
        nq = 0
        for k in range(8):
            c = 0
            for (a, b) in FM_COLS:
                S.add("sp" if nq % 2 == 0 else "act", lambda e, k=k, a=a, b=b, c=c: e.dma_start(
                    out=wfm[:, k, c:c + (b - a)], in_=winb[k * 128:(k + 1) * 128, a:b]),
                    reads=wkeys, writes=[("wfm", k)], dma=True)
                nq += 1
                c += b - a
            for (a, b, c) in [(640, 768, 0), (1280, 1536, 128), (1792, 1920, 384), (2180, 2244, 512)]:
                S.add("sp" if nq % 2 == 0 else "act", lambda e, k=k, a=a, b=b, c=c: e.dma_start(
                    out=wtm[:, k, c:c + (b - a)], in_=winb[k * 128:(k + 1) * 128, a:b]),
                    reads=wkeys, writes=[("wtm", k)], dma=True)
                nq += 1
        nev = 0
        PSTOP = int(os.environ.get("PROJ_STOP", "99"))
        for tt in range(NTT if PSTOP > 1 else 0):
            for sub in range(4):
                u = tt * 4 + sub
                sl = u % 2
                S.add("sp", lambda e, u=u, sl=sl: e.dma_start(out=xs[sl], in_=xres[u * 128:(u + 1) * 128, :]),
                      reads=[("xres", u)], writes=[("xs", sl)], dma=True)
                norm_sub(xs[sl], ("xs", sl), gb, "gb", stat[:, sl, :], ("stat", sl), hb[sl], ("hb", sl), junk)
                for k in range(8):
                    S.add("pe", lambda e, sl=sl, k=k: e.transpose(out=pbh[0][:, k * 128:(k + 1) * 128],
                                                                  in_=hb[sl][:, k * 128:(k + 1) * 128], identity=ident),
                          reads=[("hb", sl), "ident"], writes=["pb0"])
                S.add("act", lambda e, sub=sub: e.activation(
                    out=hT[:, :, sub * 128:(sub + 1) * 128],
                    in_=pbh[0].rearrange("p (k t) -> p k t", t=128), func=AF.Copy),
                    reads=["pb0"], writes=[("hT", sub)])
            hkeys = [("hT", s) for s in range(4)]
            for sub in range(4):
                u = tt * 4 + sub
                for k in range(8):
                    S.add("pe", lambda e, sub=sub, k=k: e.matmul(pb[4], lhsT=hT[:, k, sub * 128:(sub + 1) * 128],
                                                                 rhs=wtm[:, k, 0:512], start=(k == 0), stop=(k == 7)),
                          reads=[("wtm", k), ("hT", sub)], writes=[("pb", 4)])
                for k in range(8):
                    S.add("pe", lambda e, sub=sub, k=k: e.matmul(pb[5][:, 0:64], lhsT=hT[:, k, sub * 128:(sub + 1) * 128],
                                                                 rhs=wtm[:, k, 512:576], start=(k == 0), stop=(k == 7)),
                          reads=[("wtm", k), ("hT", sub)], writes=[("pb", 5)])
                S.add("act", lambda e, sub=sub: e.activation(out=vtm[:, sub, 0:384], in_=pb[4][:, 0:384], func=AF.Copy),
                      reads=[("pb", 4)], writes=[("vtm", sub)])
                S.add("act", lambda e: e.activation(out=ck, in_=pb[4][:, 384:512], func=AF.Copy), reads=[("pb", 4)], writes=["ck"])
                S.add("dve", lambda e, sub=sub: e.tensor_scalar(out=wis[:, sub, :], in0=pb[5][:, 48:64], scalar1=0.5,
                                                                scalar2=None, op0=ALU.mult),
                      reads=[("pb", 5)], writes=["wis"])
                norm_sub(ck, "ck", gkv, "gkv", stat2, "stat2", ckn, "ckn", junk, width=128)
                S.add("pe", lambda e: e.transpose(out=pbh[6][:, 0:128], in_=ckn, identity=ident),
                      reads=["ckn", "ident"], writes=[("pb", 6)])
                S.add("act", lambda e, sub=sub: e.activation(out=cknT[:, sub * 128:(sub + 1) * 128], in_=pbh[6][:, 0:128],
                                                             func=AF.Copy),
                      reads=[("pb", 6)], writes=[("cknT", sub)])
                S.add("pe", lambda e, sub=sub: e.matmul(pb[7][:, 0:64], lhsT=cknT[:, sub * 128:(sub + 1) * 128],
                                                        rhs=wupb[:, 64:128], start=True, stop=True),
                      reads=[("cknT", sub), "wupb"], writes=[("pb", 7)])
                S.add("dve", lambda e, sub=sub: e.tensor_copy(out=vtm[:, sub, 384:448], in_=pb[7][:, 0:64]),
                      reads=[("pb", 7)], writes=[("vtm", sub)])
            for f in range(27 if PSTOP > 2 else 0):
                pf = 1 + f % 3
                for k in range(8):
                    S.add("pe", lambda e, f=f, k=k, pf=pf: e.matmul(pb[pf][0:64, :], lhsT=wfm[:, k, f * 64:(f + 1) * 64],
                                                                    rhs=hT[:, k, :], start=(k == 0), stop=(k == 7)),
                          reads=[("wfm", k)] + hkeys, writes=[("pb", pf)])
                sc = 0.125 if QSCALE[f] else 1.0
                if nev % 2 == 0:
                    S.add("act", lambda e, f=f, pf=pf, sc=sc: e.activation(out=hd[:, f, :], in_=pb[pf][0:64, :],
                                                                           func=AF.Copy, scale=sc),
                          reads=[("pb", pf)], writes=[("hd", f)])
                else:
                    S.add("dve", lambda e, f=f, pf=pf, sc=sc: e.tensor_scalar(out=hd[:, f, :], in0=pb[pf][0:64, :],
                                                                              scalar1=sc, scalar2=None, op0=ALU.mult),
                          reads=[("pb", pf)], writes=[("hd", f)])
                nev += 1
            S.add("pe", lambda e: e.matmul(pb[1][0:64, :], lhsT=wupb[:, 0:64], rhs=cknT, start=True, stop=True),
                  reads=[("cknT", s) for s in range(4)] + ["wupb"], writes=[("pb", 1)])
            S.add("act", lambda e: e.activation(out=hd[:, 27, :], in_=pb[1][0:64, :], func=AF.Copy),
                  reads=[("pb", 1)], writes=[("hd", 27)])
            t0 = tt * 512
            if PSTOP <= 4:
                continue

            def fm_out(dst3, f0, nf, key):
                S.add("sp", lambda e: e.dma_start(out=dst3, in_=hd[:, f0:f0 + nf, :]),
                      reads=[("hd", f) for f in range(f0, f0 + nf)], writes=[key], dma=True)
            fm_out(qaT_d[:, :, t0:t0 + 512].rearrange("h d t -> d h t"), 0, 8, ("qaT", tt))
            fm_out(KT_t[tt][0:128, :].rearrange("(h d) t -> d h t", d=64), 8, 2, ("KTa", tt))
            fm_out(qbT_d[:, :, t0:t0 + 512].rearrange("h d t -> d h t"), 10, 4, ("qbT", tt))
            fm_out(KT_t[tt][128:384, :].rearrange("(h d) t -> d h t", d=64), 14, 4, ("KTb", tt))
            fm_out(qcT_d[:, :, t0:t0 + 512].rearrange("h d t -> d h t"), 18, 4, ("qcT", tt))
            fm_out(qiT_d[:, :, t0:t0 + 512].rearrange("h d t -> d h t"), 22, 4, ("qiT", tt))
            fm_out(KT_t[tt][448:512, :].rearrange("(h d) t -> d h t", d=64), 26, 1, ("KTi", tt))
            fm_out(KT_t[tt][384:448, :].rearrange("(h d) t -> d h t", d=64), 27, 1, ("KTc", tt))
            S.add("sp", lambda e, tt=tt: e.dma_start(out=VT_t[tt].rearrange("(s p) c -> p s c", p=128), in_=vtm),
                  reads=[("vtm", s) for s in range(4)], writes=[("VT", tt)], dma=True)
            S.add("sp", lambda e, t0=t0: e.dma_start(out=wi_d[t0:t0 + 512, :].rearrange("(s p) c -> p s c", p=128), in_=wis),
                  reads=["wis"], writes=[("wi", tt)], dma=True)
        S.barrier()
        A.release(m)

    def exchange():
        if os.environ.get("SKIP_XCHG"):
            return
        groups = [[2 * g, 2 * g + 1] for g in range(4)]
        for t in range(NTT):
            S.add("pool", lambda e, t=t: e.collective_compute("AllGather", ALU.bypass, replica_groups=groups,
                                                              ins=[KT_t[t]], outs=[gKT_t[t]]),
                  reads=["ccorder"], writes=[("gKT", t), "ccorder"], cc=True)
            S.add("pool", lambda e, t=t: e.collective_compute("AllGather", ALU.bypass, replica_groups=groups,
                                                              ins=[VT_t[t]], outs=[gVT_t[t]]),
                  reads=["ccorder"], writes=[("gVT", t), "ccorder"], cc=True)
        S.barrier()

    def finalize(pO, pD, okey, dkey, nh, dst3, dkey_out, extra=None, tmp=None):
        dn, oc, osb = tmp
        W = nh * 128
        S.add("act", lambda e: e.activation(out=osb[:, 0:W], in_=pO[0:65, 0:W], func=AF.Copy), reads=[okey], writes=["osb"])
        S.add("pe", lambda e: e.matmul(pD[0:64, 0:W], lhsT=selT, rhs=osb[:, 0:W], start=True, stop=True),
              reads=["selT", "osb"], writes=[dkey])
        if extra is not None:
            S.add("dve", lambda e: e.tensor_tensor(out=dn[:, 0:W], in0=pD[0:64, 0:W], in1=extra, op=ALU.add),
                  reads=[dkey, "exps"], writes=["dn"])
            S.add("dve", lambda e: e.reciprocal(out=dn[:, 0:W], in_=dn[:, 0:W]), reads=["dn"], writes=["dn"])
        else:
            S.add("dve", lambda e: e.reciprocal(out=dn[:, 0:W], in_=pD[0:64, 0:W]), reads=[dkey], writes=["dn"])
        S.add("dve", lambda e: e.tensor_tensor(out=oc[:, 0:W], in0=osb[0:64, 0:W], in1=dn[:, 0:W], op=ALU.mult),
              reads=["osb", "dn"], writes=["oc"])
        S.add("sp", lambda e: e.dma_start(out=dst3, in_=oc[:, 0:W].rearrange("p (h t) -> p h t", t=128)),
              reads=["oc"], writes=[dkey_out], dma=True)

    def swa_phase(l):
        m = A.mark()
        kaT = A.alloc("kaT", 64, [2, TC], BF16)
        vaO = A.alloc("vaO", 128, [NT, 2, 65], BF16)
        kaH = A.alloc("kaH", 65, [2, 128], BF16)
        vaH = A.alloc("vaH", 128, [2, 65], BF16)
        exps8 = A.alloc("exps8", 64, 8, F32)
        exps = A.alloc("exps", 64, [8, 128], F32)
        qa = [A.alloc("qa%d" % s, 65, [8, 128], BF16) for s in range(2)]
        PT = [A.alloc("PT%d" % s, 128, 512, BF16) for s in range(2)]
        dn = A.alloc("dn", 64, 512, F32)
        oc = A.alloc("oc", 64, 512, BF16)
        osb = A.alloc("osb", 65, 512, F32)
        S.add("dve", lambda e: e.memset(vaO[:, :, :, 64:65], 1.0), writes=["vaO"])
        S.add("dve", lambda e: e.memset(vaH[:, :, 64:65], 1.0), writes=["vaH"])
        for t in range(NTT):
            S.add("sp", lambda e, t=t: e.dma_start(out=kaT[:, :, t * 512:(t + 1) * 512],
                                                   in_=KT_t[t][0:128, :].rearrange("(g d) t -> d g t", d=64)),
                  reads=[("KTa", t)], writes=["kaT"], dma=True)
            for g in range(2):
                S.add("sp", lambda e, t=t, g=g: e.dma_start(out=vaO[:, 4 * t:4 * t + 4, g, 0:64],
                                                            in_=VT_t[t][:, g * 64:(g + 1) * 64].rearrange("(n p) d -> p n d", p=128)),
                      reads=[("VT", t)], writes=["vaO"], dma=True)
        S.add("sp", lambda e: e.dma_start(out=kaH[0:64, :, :],
                                          in_=gKT_t[NTT - 1][0:128, 384:512].rearrange("(g d) t -> d g t", d=64)),
              reads=[("gKT", NTT - 1)], writes=["kaH"], dma=True)
        S.add("dve", lambda e: e.memset(kaH[64:65, :, :], 1.0), writes=["kaH"])
        S.add("sp", lambda e: e.dma_start(out=vaH[:, :, 0:64], in_=gVT_t[NTT - 1][384:512, 0:128].rearrange("p (g d) -> p g d", d=64)),
              reads=[("gVT", NTT - 1)], writes=["vaH"], dma=True)
        S.add("sp", lambda e: e.dma_start(out=exps8, in_=sink_d[l:l + 1, :].to_broadcast([64, 8])), writes=["exps8"], dma=True)
        S.add("act", lambda e: e.activation(out=exps8, in_=exps8, func=AF.Exp), reads=["exps8"], writes=["exps8"])
        S.add("dve", lambda e: e.tensor_copy(out=exps, in_=bc_last(exps8, 128)), reads=["exps8"], writes=["exps"])
        for s in range(2):
            S.add("dve", lambda e, s=s: e.tensor_copy(out=flat2(qa[s][64:65, :, :]), in_=bc_cols(pfxc[64:65, 0:1], 1024)),
                  reads=["pfxc"], writes=[("qa", s)])
        for i in range(NT):
            s = i % 2
            S.add("sp", lambda e, i=i, s=s: e.dma_start(out=qa[s][0:64, :, :],
                                                        in_=qaT_d[:, :, i * 128:(i + 1) * 128].rearrange("h d t -> d h t")),
                  reads=[("qaT", i // 4)], writes=[("qa", s)], dma=True)
            for g in range(2):
                chunks = []
                if i == 0:
                    chunks.append((kaH[0:65, g, :], 65, vaH[:, g, :], T1all, ["kaH", "vaH"]))
                else:
                    chunks.append((kaT[:, g, (i - 1) * 128:i * 128], 64, vaO[:, i - 1, g, :], T1all,
                                   ["kaT", "vaO"]))
                chunks.append((kaT[:, g, i * 128:(i + 1) * 128], 64, vaO[:, i, g, :], T0all, ["kaT", "vaO"]))
                pO, pD = pb[4 + 2 * (g % 2)], pb[5 + 2 * (g % 2)]
                okey, dkey = ("pb", 4 + 2 * (g % 2)), ("pb", 5 + 2 * (g % 2))
                for ci, (kap, r, vap, Tt, kk) in enumerate(chunks):
                    ps = (2 * g + ci) % 4
                    S.add("pe", lambda e, kap=kap, r=r, ps=ps, s=s, g=g: e.matmul(
                        pb[ps], lhsT=kap, rhs=flat2(qa[s][0:r, 4 * g:4 * g + 4, :]), start=True, stop=False),
                        reads=kk + [("qa", s)], writes=[("pb", ps)])
                    S.add("pe", lambda e, Tt=Tt, ps=ps, g=g: e.matmul(
                        pb[ps], lhsT=ident, rhs=flat2(Tt[:, 4 * g:4 * g + 4, :]), start=False, stop=True),
                        reads=["ident", "T0all", "T1all"], writes=[("pb", ps)])
                    S.add("act", lambda e, ps=ps, ci=ci: e.activation(out=PT[ci], in_=pb[ps], func=AF.Exp),
                          reads=[("pb", ps)], writes=[("PT", ci)])
                for ci, (kap, r, vap, Tt, kk) in enumerate(chunks):
                    S.add("pe", lambda e, vap=vap, ci=ci, pO=pO: e.matmul(pO[0:65, :], lhsT=vap, rhs=PT[ci],
                                                                          start=(ci == 0), stop=(ci == 1)),
                          reads=kk + [("PT", ci)], writes=[okey])
                finalize(pO, pD, okey, dkey, 4,
                         catT_d[4 * g:4 * g + 4, :, i * 128:(i + 1) * 128].rearrange("h d t -> d h t"),
                         ("catT", i), extra=flat2(exps[:, 4 * g:4 * g + 4, :]), tmp=(dn, oc, osb))
        S.barrier()
        A.release(m)

    def moba_phase(l):
        m = A.mark()
        kb = A.alloc("kb", 97, [4, T2], BF16)
        vb = A.alloc("vb", 128, [NC2, 4, 65], BF16)
        kmf = A.alloc("kmf", 64, [4, GW], F32)
        kmT = A.alloc("kmT", 64, [4, GW], BF16)
        qb = [A.alloc("qb%d" % s, 97, [4, 128], BF16) for s in range(2)]
        bsel = [A.alloc("bsel%d" % s, 128, [4, 96], BF16) for s in range(2)]
        gt = A.alloc("gt", 128, [4, GW], F32)
        mx8 = A.alloc("mx8", 128, [4, 8], F32)
        thr = A.alloc("thr", 128, 4, F32)
        PT = [A.alloc("PT%d" % s, 128, 512, BF16) for s in range(4)]
        SBANK = [2, 3, 5, 7]
        dn = A.alloc("dn", 64, 512, F32)
        oc = A.alloc("oc", 64, 512, BF16)
        osb = A.alloc("osb", 65, 512, F32)
        S.add("dve", lambda e: e.memset(vb[:, :, :, 64:65], 1.0), writes=["vb"])
        for t in range(NTT):
            S.add("sp", lambda e, t=t: e.dma_start(out=kb[0:64, :, t * 512:(t + 1) * 512],
                                                   in_=gKT_t[t][128:384, :].rearrange("(h d) t -> d h t", d=64)),
                  reads=[("gKT", t)], writes=["kbk"], dma=True)
            S.add("sp", lambda e, t=t: e.dma_start(out=kb[0:64, :, TC + t * 512:TC + (t + 1) * 512],
                                                   in_=KT_t[t][128:384, :].rearrange("(h d) t -> d h t", d=64)),
                  reads=[("KTb", t)], writes=["kbk"], dma=True)
            for h in range(4):
                S.add("sp", lambda e, t=t, h=h: e.dma_start(
                    out=vb[:, 4 * t:4 * t + 4, h, 0:64],
                    in_=gVT_t[t][0:512, 128 + h * 64:128 + (h + 1) * 64].rearrange("(n p) d -> p n d", p=128)),
                    reads=[("gVT", t)], writes=["vb"], dma=True)
            for h in range(4):
                S.add("sp", lambda e, t=t, h=h: e.dma_start(
                    out=vb[:, NT + 4 * t:NT + 4 * t + 4, h, 0:64],
                    in_=VT_t[t][:, 128 + h * 64:128 + (h + 1) * 64].rearrange("(n p) d -> p n d", p=128)),
                    reads=[("VT", t)], writes=["vb"], dma=True)
        for h in range(4):
            S.add("pool", lambda e, h=h: e.dma_start(out=kb[64:97, h, :], in_=er_d), writes=["kbk"], dma=True)
        S.add("dve", lambda e: e.memset(kmf, 0.0), writes=["kmf"])
        for h in range(4):
            S.add("dve", lambda e, h=h: e.reduce_sum(out=kmf[:, h, 0:NB],
                                                     in_=kb[0:64, h, :].rearrange("p (n s) -> p n s", s=256), axis=AX.X),
                  reads=["kbk", "kmf"], writes=["kmf"])
        S.add("dve", lambda e: e.tensor_scalar(out=kmT, in0=kmf, scalar1=1.0 / 256, scalar2=None, op0=ALU.mult),
              reads=["kmf"], writes=["kmT"])
        for s in range(2):
            S.add("sp", lambda e, s=s: e.dma_start(out=qb[s][96:97, :, :], in_=b31s[0:1, 0:4, :]),
                  reads=["b31s"], writes=[("qb", s)], dma=True)
            S.add("dve", lambda e, s=s: e.memset(bsel[s][:, :, 0:64], 0.0), writes=[("bsel", s)])
            S.add("dve", lambda e, s=s: e.memset(bsel[s][:, :, 64:96], -1.0), writes=[("bsel", s)])
        for i in range(NT):
            s = i % 2
            nb = NBH + i // 2
            cq = NT + i
            ob0 = cq - (i % 2)
            S.add("sp", lambda e, i=i, s=s: e.dma_start(out=qb[s][0:64, :, :],
                                                        in_=qbT_d[:, :, i * 128:(i + 1) * 128].rearrange("h d t -> d h t")),
                  reads=[("qbT", i // 4)], writes=[("qb", s)], dma=True)
            for h in range(4):
                S.add("pe", lambda e, s=s, h=h: e.matmul(pb[0][:, h * GW:(h + 1) * GW], lhsT=qb[s][0:64, h, :],
                                                         rhs=kmT[:, h, :], start=(h == 0), stop=(h == 3)),
                      reads=[("qb", s), "kmT"], writes=[("pb", 0)])
            S.add("dve", lambda e: e.tensor_tensor(out=gt, in0=pb[0][:, 0:4 * GW].rearrange("p (h n) -> p h n", n=GW),
                                                   in1=bc_mid(pfxrow, 4), op=ALU.add),
                  reads=[("pb", 0), "pfxrow"], writes=["gt"])
            S.add("dve", lambda e, nb=nb: e.tensor_tensor(out=gt, in0=gt, in1=bc_mid(stepc[:, GW - nb:2 * GW - nb], 4), op=ALU.add),
                  reads=["gt", "stepc"], writes=["gt"])
            for h in range(4):
                S.add("dve", lambda e, h=h: e.max(out=mx8[:, h, :], in_=gt[:, h, :]), reads=["gt"], writes=["mx8"])
            S.add("dve", lambda e: e.tensor_scalar(out=thr, in0=mx8[:, :, 2], scalar1=-16000.0, scalar2=None, op0=ALU.max),
                  reads=["mx8"], writes=["thr"])
            for h in range(4):
                S.add("dve", lambda e, s=s, h=h, nb=nb: e.tensor_scalar(out=bsel[s][:, h, 64:64 + nb], in0=gt[:, h, 0:nb],
                                                                        scalar1=thr[:, h:h + 1], scalar2=-1.0,
                                                                        op0=ALU.is_ge, op1=ALU.add),
                      reads=["gt", "thr"], writes=[("bsel", s)])
            for h in range(4):
                S.add("pe", lambda e, s=s, h=h: e.transpose(out=pbh[1][0:96, h * 128:(h + 1) * 128], in_=bsel[s][:, h, :],
                                                            identity=ident),
                      reads=[("bsel", s), "ident"], writes=[("pb", 1)])
            S.add("act", lambda e, s=s: e.activation(out=flat2(qb[s][64:96, :, :]), in_=pbh[1][64:96, 0:512], func=AF.Copy),
                  reads=[("pb", 1)], writes=[("qb", s)])
            pO, pD = pb[4 + 2 * s], pb[1]
            okey, dkey = ("pb", 4 + 2 * s), ("pb", 1)
            def qk(c, i=i, s=s, cq=cq, ob0=ob0):
                ps = SBANK[c % 4]
                Tt = None
                if c == cq:
                    r, Tt = 64, T0all
                elif c == cq - 1 and c >= ob0:
                    r, Tt = 64, T1all
                elif c == cq - 1:
                    r, Tt = 96, T1all
                else:
                    r = 97
                for h in range(4):
                    S.add("pe", lambda e, h=h, r=r, ps=ps, Tt=Tt: e.matmul(
                        pb[ps][:, h * 128:(h + 1) * 128], lhsT=kb[0:r, h, c * 128:(c + 1) * 128], rhs=qb[s][0:r, h, :],
                        start=(h == 0), stop=(Tt is None and h == 3)),
                        reads=["kbk", ("qb", s)], writes=[("pb", ps)])
                if Tt is not None:
                    S.add("pe", lambda e, ps=ps, Tt=Tt: e.matmul(pb[ps], lhsT=ident, rhs=flat2(Tt[:, 8:12, :]),
                                                                 start=False, stop=True),
                          reads=["ident", "T0all", "T1all"], writes=[("pb", ps)])
                S.add("act", lambda e, ps=ps: e.activation(out=PT[c % 4], in_=pb[ps], func=AF.Exp),
                      reads=[("pb", ps)], writes=[("PT", c % 4)])

            def pv(c, pO=pO, cq=cq, okey=okey):
                for h in range(4):
                    S.add("pe", lambda e, h=h: e.matmul(
                        pO[0:65, h * 128:(h + 1) * 128], lhsT=vb[:, c, h, :], rhs=PT[c % 4][:, h * 128:(h + 1) * 128],
                        start=(c == 0 and h == 0), stop=(c == cq and h == 3)),
                        reads=["vb", ("PT", c % 4)], writes=[okey])

            qk(0)
            if cq >= 1:
                qk(1)
            for c in range(cq + 1):
                if c + 2 <= cq:
                    qk(c + 2)
                pv(c)
            finalize(pO, pD, okey, dkey, 4, catT_d[8:12, :, i * 128:(i + 1) * 128].rearrange("h d t -> d h t"),
                     ("catT", i), tmp=(dn, oc, osb))
        S.barrier()
        A.release(m)

    def dsa_phase(l):
        m = A.mark()
        ki = A.alloc("ki", 64, T2, BF16)
        kc = A.alloc("kc", 65, T2, BF16)
        vc = A.alloc("vc", 128, [NC2, 65], BF16)
        Ib = A.alloc("Ib", 128, T2, F32)
        jk = A.alloc("jk", 128, T2, BF16)
        mb = [A.alloc("mb%d" % s, 128, T2, BF16) for s in range(2)]
        rl = [A.alloc("rl%d" % h, 128, 512, F32) for h in range(4)]
        qi = [A.alloc("qi%d" % s, 64, [4, 128], BF16) for s in range(2)]
        qc = [A.alloc("qc%d" % s, 65, [4, 128], BF16) for s in range(2)]
        wt = [A.alloc("wt%d" % s, 128, 16, F32) for s in range(2)]
        bs = A.alloc("bs", 128, 8, F32)
        PT = [A.alloc("PT%d" % s, 128, 512, BF16) for s in range(2)]
        dn = A.alloc("dn", 64, 512, F32)
        oc = A.alloc("oc", 64, 512, BF16)
        osb = A.alloc("osb", 65, 512, F32)
        lo, mid, cnt, dl = bs[:, 0:1], bs[:, 1:2], bs[:, 2:3], bs[:, 3:4]
        if l + 1 < L:
            precast([(l + 1, n_) for n_ in ("w1g", "w1u", "w1d", "win", "w2g", "w2u", "w2d")])
        S.add("dve", lambda e: e.memset(vc[:, :, 64:65], 1.0), writes=["vc"])
        for t in range(NTT):
            S.add("sp", lambda e, t=t: e.dma_start(out=ki[:, t * 512:(t + 1) * 512], in_=gKT_t[t][448:512, :]),
                  reads=[("gKT", t)], writes=["ki"], dma=True)
            S.add("sp", lambda e, t=t: e.dma_start(out=ki[:, TC + t * 512:TC + (t + 1) * 512], in_=KT_t[t][448:512, :]),
                  reads=[("KTi", t)], writes=["ki"], dma=True)
            S.add("sp", lambda e, t=t: e.dma_start(out=kc[0:64, t * 512:(t + 1) * 512], in_=gKT_t[t][384:448, :]),
                  reads=[("gKT", t)], writes=["kc"], dma=True)
            S.add("sp", lambda e, t=t: e.dma_start(out=kc[0:64, TC + t * 512:TC + (t + 1) * 512], in_=KT_t[t][384:448, :]),
                  reads=[("KTc", t)], writes=["kc"], dma=True)
            S.add("sp", lambda e, t=t: e.dma_start(out=vc[:, 4 * t:4 * t + 4, 0:64],
                                                   in_=gVT_t[t][0:512, 384:448].rearrange("(n p) c -> p n c", p=128)),
                  reads=[("gVT", t)], writes=["vc"], dma=True)
            S.add("sp", lambda e, t=t: e.dma_start(out=vc[:, NT + 4 * t:NT + 4 * t + 4, 0:64],
                                                   in_=VT_t[t][:, 384:448].rearrange("(n p) c -> p n c", p=128)),
                  reads=[("VT", t)], writes=["vc"], dma=True)
        S.add("dve", lambda e: e.memset(kc[64:65, :], 1.0), writes=["kc"])
        for s in range(2):
            S.add("sp", lambda e, s=s: e.dma_start(out=qc[s][64:65, :, :], in_=b31s[0:1, 4:8, :]),
                  reads=["b31s"], writes=[("qc", s)], dma=True)

        def stage_a(i):
            s = i % 2
            nkc = NT + i + 1
            nk = nkc * 128
            S.add("sp", lambda e: e.dma_start(out=qi[s], in_=qiT_d[:, :, i * 128:(i + 1) * 128].rearrange("h d t -> d h t")),
                  reads=[("qiT", i // 4)], writes=[("qi", s)], dma=True)
            S.add("sp", lambda e: e.dma_start(out=qc[s][0:64, :, :],
                                              in_=qcT_d[:, :, i * 128:(i + 1) * 128].rearrange("h d t -> d h t")),
                  reads=[("qcT", i // 4)], writes=[("qc", s)], dma=True)
            S.add("sp", lambda e: e.dma_start(out=wt[s], in_=wi_d[i * 128:(i + 1) * 128, :]),
                  reads=[("wi", i // 4)], writes=[("wt", s)], dma=True)
            nblk = (nk + 511) // 512
            for b in range(nblk):
                w = min(512, nk - b * 512)
                c0 = b * 512
                pfx_blk = (c0 + w) <= TC
                assert pfx_blk or c0 >= TC
                for h in range(4):
                    S.add("pe", lambda e, h=h, c0=c0, w=w: e.matmul(pb[h][:, 0:w], lhsT=qi[s][:, h, :], rhs=ki[:, c0:c0 + w],
                                                                    start=True, stop=True),
                          reads=[("qi", s), "ki"], writes=[("pb", h)])
                    S.add("act", lambda e, h=h, w=w: e.activation(out=rl[h][:, 0:w], in_=pb[h][:, 0:w], func=AF.Relu),
                          reads=[("pb", h)], writes=[("rl", h)])
                sc2 = pfxc[:, 0:1] if pfx_blk else zero1[:, 0:1]
                S.add("dve", lambda e, c0=c0, w=w, sc2=sc2: e.tensor_scalar(
                    out=Ib[:, c0:c0 + w], in0=rl[0][:, 0:w], scalar1=wt[s][:, 12:13], scalar2=sc2, op0=ALU.mult, op1=ALU.add),
                    reads=[("rl", 0), ("wt", s), "pfxc", "zero1"], writes=["Ib"])
                for h in range(1, 4):
                    S.add("dve", lambda e, h=h, c0=c0, w=w: e.scalar_tensor_tensor(
                        out=Ib[:, c0:c0 + w], in0=rl[h][:, 0:w], scalar=wt[s][:, 12 + h:13 + h], in1=Ib[:, c0:c0 + w],
                        op0=ALU.mult, op1=ALU.add),
                        reads=[("rl", h), ("wt", s), "Ib"], writes=["Ib"])
            S.add("dve", lambda e: e.tensor_tensor(out=Ib[:, nk - 128:nk], in0=Ib[:, nk - 128:nk], in1=cmt, op=ALU.add),
                  reads=["Ib", "cmt"], writes=["Ib"])
            S.add("dve", lambda e: e.memset(mid, LO0 + BRW / 2), writes=["mid"])
            for it in range(NIT):
                step = BRW / (2 ** (it + 1))
                S.add("dve", lambda e: e.tensor_scalar(out=jk[:, 0:nk], in0=Ib[:, 0:nk], scalar1=mid, scalar2=None,
                                                       op0=ALU.is_ge, op1=ALU.add, accum_out=cnt),
                      reads=["Ib", "mid"], writes=["jk", "cnt"])
                S.add("dve", lambda e: e.tensor_scalar(out=dl, in0=cnt, scalar1=256.0, scalar2=-0.5,
                                                       op0=ALU.is_ge, op1=ALU.add),
                      reads=["cnt"], writes=["dl"])
                if it + 1 < NIT:
                    S.add("dve", lambda e, step=step: e.scalar_tensor_tensor(out=mid, in0=dl, scalar=step, in1=mid,
                                                                             op0=ALU.mult, op1=ALU.add),
                          reads=["dl", "mid"], writes=["mid"])
                else:
                    S.add("dve", lambda e: e.tensor_scalar(out=dl, in0=dl, scalar1=-0.5, scalar2=step,
                                                           op0=ALU.add, op1=ALU.mult),
                          reads=["dl"], writes=["dl"])
                    S.add("dve", lambda e: e.tensor_tensor(out=lo, in0=mid, in1=dl, op=ALU.add), reads=["mid", "dl"], writes=["lo"])
            S.add("dve", lambda e: e.tensor_scalar(out=mb[s][:, 0:nk], in0=Ib[:, 0:nk], scalar1=lo, scalar2=-BIG,
                                                   op0=ALU.is_lt, op1=ALU.mult),
                  reads=["Ib", "lo"], writes=[("mb", s)])

        def stage_b(i):
            s = i % 2
            nkc = NT + i + 1
            pO, pD = pb[6], pb[7]
            okey, dkey = ("pb", 6), ("pb", 7)
            def qk(c):
                ps = 4 + c % 2
                Tt = None
                if c == nkc - 1:
                    r, Tt = 64, T0all
                elif c == nkc - 2:
                    r, Tt = 64, T1all
                else:
                    r = 65
                S.add("pe", lambda e, r=r, ps=ps: e.matmul(pb[ps], lhsT=kc[0:r, c * 128:(c + 1) * 128],
                                                           rhs=flat2(qc[s][0:r, :, :]), start=True, stop=False),
                      reads=["kc", ("qc", s)], writes=[("pb", ps)])
                S.add("pe", lambda e, ps=ps, Tt=Tt: e.matmul(pb[ps], lhsT=mb[s][:, c * 128:(c + 1) * 128], rhs=flat2(ident4),
                                                             start=False, stop=(Tt is None)),
                      reads=[("mb", s), "ident4"], writes=[("pb", ps)])
                if Tt is not None:
                    S.add("pe", lambda e, ps=ps, Tt=Tt: e.matmul(pb[ps], lhsT=ident, rhs=flat2(Tt[:, 12:16, :]),
                                                                 start=False, stop=True),
                          reads=["ident", "T0all", "T1all"], writes=[("pb", ps)])
                S.add("act", lambda e, ps=ps: e.activation(out=PT[c % 2], in_=pb[ps], func=AF.Exp),
                      reads=[("pb", ps)], writes=[("PT", c % 2)])

            def pv(c):
                S.add("pe", lambda e: e.matmul(pO[0:65, :], lhsT=vc[:, c, :], rhs=PT[c % 2], start=(c == 0), stop=(c == nkc - 1)),
                      reads=["vc", ("PT", c % 2)], writes=[okey])

            qk(0)
            for c in range(nkc):
                if c + 1 < nkc:
                    qk(c + 1)
                pv(c)
            finalize(pO, pD, okey, dkey, 4, catT_d[12:16, :, i * 128:(i + 1) * 128].rearrange("h d t -> d h t"),
                     ("catT", i), tmp=(dn, oc, osb))

        stage_a(0)
        for i in range(NT):
            if i + 1 < NT:
                stage_a(i + 1)
            stage_b(i)
        S.barrier()
        A.release(m)

    def wout_phase(l):
        m = A.mark()
        wo = A.alloc("wo", 64, [16, D], BF16)
        xs = [A.alloc("xs%d" % s, 128, D, F32) for s in range(3)]
        ct = [A.alloc("ct%d" % s, 64, [16, 128], BF16) for s in range(2)]
        S.add("pool", lambda e: e.dma_start(out=wo[:, 0:8, :], in_=wo_d[l, 0:512, :].rearrange("(h d) m -> d h m", d=64)),
              writes=["wo"], dma=True)
        S.add("pool", lambda e: e.dma_start(out=wo[:, 8:16, :], in_=wo_d[l, 512:1024, :].rearrange("(h d) m -> d h m", d=64)),
              writes=["wo"], dma=True)
        for u in range(NT):
            sl = u % 3
            s = u % 2
            S.add("sp", lambda e, u=u, sl=sl: e.dma_start(out=xs[sl], in_=xres[u * 128:(u + 1) * 128, :]),
                  reads=[("xres", u)], writes=[("xs", sl)], dma=True)
            S.add("sp", lambda e, u=u, s=s: e.dma_start(out=ct[s], in_=catT_d[:, :, u * 128:(u + 1) * 128].rearrange("h d t -> d h t")),
                  reads=[("catT", u)], writes=[("ct", s)], dma=True)
            for half in range(2):
                py = (2 * u + half) % 4
                for h in range(16):
                    S.add("pe", lambda e, s=s, h=h, half=half, py=py: e.matmul(
                        pb[py], lhsT=ct[s][:, h, :], rhs=wo[:, h, half * 512:(half + 1) * 512], start=(h == 0), stop=(h == 15)),
                        reads=[("ct", s), "wo"], writes=[("pb", py)])
                S.add("dve", lambda e, sl=sl, half=half, py=py: e.tensor_tensor(
                    out=xs[sl][:, half * 512:(half + 1) * 512], in0=pb[py], in1=xs[sl][:, half * 512:(half + 1) * 512], op=ALU.add),
                    reads=[("pb", py), ("xs", sl)], writes=[("xs", sl)])
            S.add("sp", lambda e, u=u, sl=sl: e.dma_start(out=xres[u * 128:(u + 1) * 128, :], in_=xs[sl]),
                  reads=[("xs", sl)], writes=[("xres", u)], dma=True)
        S.barrier()
        A.release(m)

    def final_phase():
        m = A.mark()
        gb = A.alloc("gb", 128, D, F32)
        xs = [A.alloc("xs%d" % s, 128, D, F32) for s in range(3)]
        ob = [A.alloc("ob%d" % s, 128, D, F32) for s in range(2)]
        stat = A.alloc("stat", 128, [3, 4], F32)
        junk = A.alloc("junk", 128, D, BF16)
        S.add("sp", lambda e: e.dma_start(out=gb, in_=nf_d.to_broadcast([128, D])), writes=["gb"], dma=True)
        for u in range(NT):
            sl = u % 3
            S.add("sp", lambda e, u=u, sl=sl: e.dma_start(out=xs[sl], in_=xres[u * 128:(u + 1) * 128, :]),
                  reads=[("xres", u)], writes=[("xs", sl)], dma=True)
            norm_sub(xs[sl], ("xs", sl), gb, "gb", stat[:, sl, :], ("stat", sl), ob[u % 2], ("ob", u % 2), junk)
            S.add("sp", lambda e, u=u: e.dma_start(out=out_d[u * 128:(u + 1) * 128, :], in_=ob[u % 2]),
                  reads=[("ob", u % 2)], writes=[("out", u)], dma=True)
        S.barrier()
        A.release(m)

    def dump(name, src):
        S.add("sp", lambda e: e.dma_start(out=dbg[name], in_=src), writes=[("dbg", name)], dma=True)
        S.barrier()

    plist = [("phase0", phase0)]
    for l in range(L):
        plist.append(("ffn1_%d" % l, lambda l=l: ffn_phase(l, w1g_d, w1u_d, w1d_d, n1_d[l:l + 1, :], x_in if l == 0 else xres, xres,
                                                          pre=(None if l == 0 else ("w1g", "w1u", "w1d")))))
        plist.append(("proj_%d" % l, lambda l=l: proj_phase(l)))
        plist.append(("xchg_%d" % l, exchange))
        plist.append(("swa_%d" % l, lambda l=l: swa_phase(l)))
        plist.append(("moba_%d" % l, lambda l=l: moba_phase(l)))
        plist.append(("dsa_%d" % l, lambda l=l: dsa_phase(l)))
        plist.append(("wout_%d" % l, lambda l=l: wout_phase(l)))
        plist.append(("ffn2_%d" % l, lambda l=l: ffn_phase(l, w2g_d, w2u_d, w2d_d, n2_d[l:l + 1, :], xres, xres,
                                                          pre=("w2g", "w2u", "w2d"))))
    plist.append(("final", final_phase))
    for name, fn in plist:
        S.tag = name
        fn()
        if upto is not None and name == upto:
            break
    if debug:
        S.tag = "dump"
        dump("xres", xres)
        dump("catT", catT_d)
        dump("qaT", qaT_d)
    S.add("act", lambda e: e.activation(out=zero1, in_=zero1, func=AF.Copy), reads=["zero1"], writes=["zero1"])
    S.add("dve", lambda e: e.memset(zero1, 0.0), writes=["zero1"])
    S.add("pool", lambda e: e.memset(zero1, 0.0), writes=["zero1"])
    S.add("sp", lambda e: e.dma_start(out=pfxc, in_=pfx_d.to_broadcast([128, 2])), writes=["pfxc"], dma=True)
    S.emit(stack)
    stack.close()
    return nc, S


_CACHE = {}


def make_in_maps(inputs, SEQ, DEPTH):
    f = lambda a: np.ascontiguousarray(np.asarray(a, dtype=np.float32))
    x = f(inputs["x"])
    B = x.shape[0]
    TC = SEQ // 2
    oh, er, cm = host_consts(SEQ)
    shared = {
        "tab": f(inputs["rel_bias_table"]),
        "n1": f(inputs["ffn1_norm"]), "w1g": f(inputs["ffn1_w_gate"]), "w1u": f(inputs["ffn1_w_up"]),
        "w1d": f(inputs["ffn1_w_down"]), "nm": f(inputs["mix_norm"]), "win": f(inputs["w_in"]),
        "sinks": f(inputs["attn_sinks"]), "kvn": f(inputs["kv_norm_c"]), "wup": f(inputs["w_kv_up_c"]),
        "wo": f(inputs["w_out"]), "n2": f(inputs["ffn2_norm"]), "w2g": f(inputs["ffn2_w_gate"]),
        "w2u": f(inputs["ffn2_w_up"]), "w2d": f(inputs["ffn2_w_down"]),
        "nf": f(inputs["final_norm"]).reshape(1, -1),
        "oh": oh, "er": er, "cm": cm,
    }
    maps = []
    for c in range(2 * B):
        b, half = c // 2, c % 2
        mp = dict(shared)
        mp["x"] = np.ascontiguousarray(x[b, half * TC:(half + 1) * TC, :])
        mp["pfx"] = np.array([[-BIG if half == 0 else 0.0, 0.0]], np.float32)
        maps.append(mp)
    return maps


def kernel(**inputs):
    x = np.asarray(inputs["x"])
    B, SEQ, _ = x.shape
    DEPTH = np.asarray(inputs["ffn1_norm"]).shape[0]
    assert B == 4
    key = (SEQ, DEPTH)
    if key not in _CACHE:
        _CACHE[key] = build_program(SEQ, DEPTH)[0]
    nc = _CACHE[key]
    maps = make_in_maps(inputs, SEQ, DEPTH)
    res = run_bass_kernel_spmd(nc, maps, core_ids=list(range(8)))
    TC = SEQ // 2
    out = np.empty((B, SEQ, D), np.float32)
    for c in range(8):
        b, half = c // 2, c % 2
        out[b, half * TC:(half + 1) * TC, :] = np.asarray(res.results[c]["out"], dtype=np.float32)
    return out
```

```python
import math
import os
from contextlib import ExitStack

import numpy as np
import concourse.bass as bass
import concourse.mybir as mybir
from concourse.bass_utils import run_bass_kernel_spmd

F32 = mybir.dt.float32
BF16 = mybir.dt.bfloat16
U8 = mybir.dt.uint8
AF = mybir.ActivationFunctionType
ALU = mybir.AluOpType
AX = mybir.AxisListType

DT_SIZE = {F32: 4, BF16: 2, U8: 1}
ENGS = ["pe", "act", "dve", "pool", "sp"]
EPOCH = 20000
NSLOT = {"sp": 8, "act": 4, "pool": 8}

D = 1024
FF = 2816
NJ = FF // 128
DIN = 2244
BIG = 32768.0
EPS = 1e-6
NIT = 24
LO0 = -64.0 - 1.0 / 3.0
BRW = 128.0
ACT_FRAC = 0.35


class Op:
    __slots__ = ("eng", "fn", "dma", "deps", "signal", "sig", "slotprev", "cc", "tag")

    def __init__(self, eng, fn, dma):
        self.eng = eng
        self.fn = fn
        self.dma = dma
        self.deps = []
        self.signal = False
        self.sig = None
        self.slotprev = None
        self.cc = False


class Sched:
    def __init__(self, nc):
        self.nc = nc
        self.ops = {e: [] for e in ENGS}
        self.lastw = {}
        self.readers = {}
        self.pending = {e: [] for e in ENGS}
        self.dma_hist = {e: [] for e in ENGS}
        self.last_comp = {e: None for e in ENGS}
        self.tag = ""
        self.names = {}

    def add(self, eng, fn, reads=(), writes=(), dma=False, cc=False, bg=False):
        op = Op(eng, fn, dma)
        op.cc = cc
        op.tag = (self.tag, tuple(reads), tuple(writes))
        deps = []
        seen = set()

        def push(d):
            if d is None or id(d) in seen:
                return
            seen.add(id(d))
            if d.eng == "pe" and eng == "pe" and not d.dma and not dma:
                return
            deps.append(d)

        psum_reads = [k for k in reads if (k == "pb0" or (isinstance(k, tuple) and k[0] == "pb"))]
        if psum_reads:
            reads = [k for k in reads if k not in psum_reads]
            writes = list(writes) + psum_reads
        for k in reads:
            push(self.lastw.get(k))
        for k in writes:
            push(self.lastw.get(k))
            for r in self.readers.get(k, ()):
                push(r)
        for d in self.pending[eng]:
            push(d)
        self.pending[eng] = []
        op.deps = deps
        for d in deps:
            d.signal = True
        for k in writes:
            self.lastw[k] = op
            self.readers[k] = []
        for k in reads:
            lst = self.readers.setdefault(k, [])
            if not dma:
                lst[:] = [r for r in lst if not (r.eng == eng and not r.dma)]
            lst.append(op)
        self.ops[eng].append(op)
        if bg:
            pass
        elif dma or cc:
            h = self.dma_hist[eng]
            h.append(op)
            if len(h) > NSLOT[eng] + 2:
                h.pop(0)
        else:
            self.last_comp[eng] = op
        return op

    def barrier(self):
        tails = []
        for e in ENGS:
            if self.last_comp[e] is not None:
                tails.append(self.last_comp[e])
            tails.extend(self.dma_hist[e])
        for e in ENGS:
            self.pending[e] = list(tails)

    def emit(self, stack):
        nc = self.nc
        names = set()
        for e in ENGS:
            cnt = 0
            ndma = 0
            slot_last = {}
            ncc = 0
            for op in self.ops[e]:
                if op.dma:
                    R = NSLOT[e]
                    slot = ndma % R
                    val = 16 * (ndma // R + 1)
                    op.sig = ("d_%s_%d" % (e, slot), val, 16)
                    op.slotprev = slot_last.get(slot)
                    slot_last[slot] = op
                    ndma += 1
                elif op.cc:
                    ncc += 1
                    op.sig = ("cc_%s" % e, ncc, 1)
                elif op.signal:
                    ep = cnt // EPOCH
                    op.sig = ("c_%s_%d" % (e, ep), cnt - ep * EPOCH + 1, 1)
                    cnt += 1
                if op.sig is not None:
                    names.add(op.sig[0])
        sems = {}
        for name in sorted(names):
            sems[name] = stack.enter_context(nc.semaphore(name))
        block = stack.enter_context(nc.Block())
        stats = {"wait": 0, "ins": 0}

        def run(e):
            def body(engine):
                seenv = {}
                for op in self.ops[e]:
                    dl = list(op.deps)
                    if op.slotprev is not None:
                        dl.append(op.slotprev)
                    for d in dl:
                        name, val, _ = d.sig
                        if seenv.get(name, 0) >= val:
                            continue
                        engine.wait_ge(sems[name], val)
                        stats["wait"] += 1
                        seenv[name] = val
                    ins = op.fn(engine)
                    try:
                        self.names[str(ins.ins.name)] = op.tag
                    except Exception:
                        pass
                    stats["ins"] += 1
                    if op.sig is not None:
                        name, val, inc = op.sig
                        ins.then_inc(sems[name], inc)
            return body

        block.tensor(run("pe"))
        block.scalar(run("act"))
        block.vector(run("dve"))
        block.gpsimd(run("pool"))
        block.sync(run("sp"))
        self.stats = stats


class Arena:
    def __init__(self, nc, nbytes):
        self.h = nc.alloc_sbuf_tensor("arena", [128, nbytes], U8)
        self.ap = self.h.ap()
        self.nbytes = nbytes
        self.off = 0

    def alloc(self, name, parts, free, dt):
        if isinstance(free, int):
            free = [free]
        n = 1
        for f in free:
            n *= f
        nb = n * DT_SIZE[dt]
        off = (self.off + 63) // 64 * 64
        assert off + nb <= self.nbytes, "SBUF arena overflow %s: %d + %d > %d" % (name, off, nb, self.nbytes)
        self.off = off + nb
        ap = self.ap[0:parts, off:off + nb].bitcast(dt)
        if len(free) == 2:
            ap = ap.rearrange("p (a b) -> p a b", b=free[1])
        elif len(free) == 3:
            ap = ap.rearrange("p (a b c) -> p a b c", b=free[1], c=free[2])
        return ap

    def mark(self):
        return self.off

    def release(self, m):
        self.off = m


def bc_mid(a, n):
    return bass.AP(a.tensor, a.offset, [list(a.ap[0]), [0, n], list(a.ap[1])])


def bc_last(a, n):
    return bass.AP(a.tensor, a.offset, [list(a.ap[0]), list(a.ap[1]), [0, n]])


def bc_cols(a, n):
    return bass.AP(a.tensor, a.offset, [list(a.ap[0]), [0, n]])


def flat2(a):
    if len(a.shape) == 3:
        return a.rearrange("p a b -> p (a b)")
    return a


def _t5_bucket_np(n):
    n = np.maximum(n, 0)
    nf = np.maximum(n, 1).astype(np.float32)
    large = 16 + (np.log(nf / np.float32(16)) / np.float32(math.log(128 / 16)) * np.float32(16)).astype(np.int32)
    large = np.minimum(large, 31)
    return np.where(n < 16, n, large)


def host_consts(SEQ):
    oh = np.zeros((2, 33, 384), np.float32)
    for m in range(384):
        d = m - 128
        b = int(_t5_bucket_np(np.array([max(d, 0)]))[0])
        oh[0, 32 if d < 0 else b, m] = 1.0
        oh[1, 32 if (d < 0 or d >= 128) else b, m] = 1.0
    er = np.zeros((33, SEQ), np.float32)
    for n in range(min(32, SEQ // 256)):
        er[n, n * 256:(n + 1) * 256] = BIG
    er[32, :] = 1.0
    q = np.arange(128)[:, None]
    j = np.arange(128)[None, :]
    cm = np.where(j <= q, 0.0, -BIG).astype(np.float32)
    return oh, er, cm


def build_program(SEQ, DEPTH, debug=False, upto=None):
    TC = SEQ // 2
    NT = TC // 128
    NTT = TC // 512
    T2 = SEQ
    NC2 = T2 // 128
    NB = SEQ // 256
    NBH = NB // 2
    GW = max(NB, 8)

    nc = bass.Bass("TRN2", target_bir_lowering=False)
    L = DEPTH

    def din(name, shape, dt=F32):
        return nc.dram_tensor(name, list(shape), dt, kind="ExternalInput").ap()

    def dscr(name, shape, dt=BF16):
        return nc.dram_tensor(name, list(shape), dt, kind="Internal").ap()

    x_in = din("x", [TC, D])
    tab_d = din("tab", [32, 16])
    n1_d = din("n1", [L, D])
    w1g_d = din("w1g", [L, D, FF])
    w1u_d = din("w1u", [L, D, FF])
    w1d_d = din("w1d", [L, FF, D])
    nm_d = din("nm", [L, D])
    win_d = din("win", [L, D, DIN])
    sink_d = din("sinks", [L, 8])
    kvn_d = din("kvn", [L, 128])
    wup_d = din("wup", [L, 128, 128])
    wo_d = din("wo", [L, D, D])
    n2_d = din("n2", [L, D])
    w2g_d = din("w2g", [L, D, FF])
    w2u_d = din("w2u", [L, D, FF])
    w2d_d = din("w2d", [L, FF, D])
    nf_d = din("nf", [1, D])
    oh_d = din("oh", [2, 33, 384])
    er_d = din("er", [33, T2])
    cm_d = din("cm", [128, 128])
    pfx_d = din("pfx", [1, 2])
    out_d = nc.dram_tensor("out", [TC, D], F32, kind="ExternalOutput").ap()

    xres = dscr("xres", [TC, D], F32)
    qaT_d = dscr("qaT", [8, 64, TC])
    qbT_d = dscr("qbT", [4, 64, TC])
    qcT_d = dscr("qcT", [4, 64, TC])
    qiT_d = dscr("qiT", [4, 64, TC])
    wi_d = dscr("wi", [TC, 16], F32)
    KT_t = [dscr("KT%d" % t, [512, 512]) for t in range(NTT)]
    VT_t = [dscr("VT%d" % t, [512, 448]) for t in range(NTT)]
    gKT_t = [dscr("gKT%d" % t, [1024, 512]) for t in range(NTT)]
    gVT_t = [dscr("gVT%d" % t, [1024, 448]) for t in range(NTT)]
    catT_d = dscr("catT", [16, 64, TC])
    Zf_d = dscr("Zf", [16, 128, 384], F32)
    ZA_d = dscr("ZA", [8, 128, 384], F32)
    wbf = {}
    for l in range(L):
        for nm_, shp in (("w1g", [D, FF]), ("w1u", [D, FF]), ("w1d", [FF, D]), ("w2g", [D, FF]), ("w2u", [D, FF]),
                         ("w2d", [FF, D]), ("win", [D, DIN])):
            if l == 0 and nm_ in ("w1g", "w1u", "w1d"):
                continue
            wbf[(l, nm_)] = dscr("wbf_%s_%d" % (nm_, l), shp)
    dbg = {}
    if debug:
        dbg["xres"] = nc.dram_tensor("dbg_xres", [TC, D], F32, kind="ExternalOutput").ap()
        dbg["catT"] = nc.dram_tensor("dbg_catT", [16, 64, TC], BF16, kind="ExternalOutput").ap()
        dbg["qaT"] = nc.dram_tensor("dbg_qaT", [8, 64, TC], BF16, kind="ExternalOutput").ap()

    stack = ExitStack()
    S = Sched(nc)
    A = Arena(nc, 210000)
    pb = [nc.alloc_psum_tensor("pb%d" % i, [128, 512], F32).ap() for i in range(8)]
    pbh = [p.bitcast(BF16) for p in pb]

    ident = A.alloc("ident", 128, 128, BF16)
    ident4 = A.alloc("ident4", 128, [4, 128], BF16)
    ones64 = A.alloc("ones64", 128, 64, BF16)
    T0all = A.alloc("T0all", 128, [16, 128], BF16)
    T1all = A.alloc("T1all", 128, [16, 128], BF16)
    cmt = A.alloc("cmt", 128, 128, F32)
    pfxc = A.alloc("pfxc", 128, 2, F32)
    pfxrow = A.alloc("pfxrow", 128, GW, F32)
    stepc = A.alloc("stepc", 128, 2 * GW, F32)
    zero1 = A.alloc("zero1", 128, 1, F32)
    selT = A.alloc("selT", 65, 64, F32)
    tab31 = A.alloc("tab31", 128, 16, F32)
    b31s = A.alloc("b31s", 1, [8, 128], BF16)

    def phase0():
        m = A.mark()
        identf = A.alloc("identf", 128, 128, F32)
        tabf = A.alloc("tabf", 33, 16, F32)
        ohb = A.alloc("ohb", 33, [2, 384], BF16)
        lh = [A.alloc("lh%d" % i, 33, 128, BF16) for i in range(2)]
        zsb = [A.alloc("zsb%d" % i, 128, 384, F32) for i in range(2)]
        S.add("pool", lambda e: e.memset(identf, 0.0), writes=["identf"])
        S.add("pool", lambda e: e.affine_select(out=identf, in_=identf, pattern=[[-1, 128]],
                                                compare_op=ALU.not_equal, fill=1.0, base=0,
                                                channel_multiplier=1),
              reads=["identf"], writes=["identf"])
        S.add("dve", lambda e: e.tensor_copy(out=ident, in_=identf), reads=["identf"], writes=["ident"])
        S.add("dve", lambda e: e.tensor_copy(out=ident4, in_=bc_mid(identf, 4)), reads=["identf"], writes=["ident4"])
        S.add("dve", lambda e: e.memset(ones64, 1.0), writes=["ones64"])
        S.add("dve", lambda e: e.memset(selT[0:64, :], 0.0), writes=["selT"])
        S.add("dve", lambda e: e.memset(selT[64:65, :], 1.0), writes=["selT"])
        S.add("dve", lambda e: e.memset(zero1, 0.0), writes=["zero1"])
        S.add("sp", lambda e: e.dma_start(out=cmt, in_=cm_d), writes=["cmt"], dma=True)
        S.add("sp", lambda e: e.dma_start(out=pfxc, in_=pfx_d.to_broadcast([128, 2])), writes=["pfxc"], dma=True)
        S.add("sp", lambda e: e.dma_start(out=tab31, in_=tab_d[31:32, :].to_broadcast([128, 16])), writes=["tab31"], dma=True)
        for j in range(8):
            S.add("dve", lambda e, j=j: e.tensor_copy(out=b31s[0:1, j, :], in_=bc_cols(tab31[0:1, 8 + j:9 + j], 128)),
                  reads=["tab31"], writes=["b31s"])
        S.add("dve", lambda e: e.memset(pfxrow, 0.0), writes=["pfxrow"])
        S.add("dve", lambda e: e.tensor_scalar(out=pfxrow[:, 0:NBH], in0=pfxrow[:, 0:NBH], scalar1=pfxc[:, 0:1],
                                               scalar2=None, op0=ALU.add),
              reads=["pfxrow", "pfxc"], writes=["pfxrow"])
        S.add("dve", lambda e: e.memset(stepc[:, 0:GW], 0.0), writes=["stepc"])
        S.add("dve", lambda e: e.memset(stepc[:, GW:2 * GW], -BIG), writes=["stepc"])
        S.add("sp", lambda e: e.dma_start(out=tabf[0:32, :], in_=tab_d), writes=["tabf"], dma=True)
        S.add("dve", lambda e: e.memset(tabf[32:33, :], -BIG), writes=["tabf"])
        S.add("pool", lambda e: e.dma_start(out=ohb, in_=oh_d.rearrange("w r m -> r w m")), writes=["ohb"], dma=True)
        n = 0
        for h in range(16):
            for which in range(2):
                if which == 1 and h >= 8:
                    continue
                s = n % 2
                n += 1
                S.add("dve", lambda e, s=s, h=h: e.tensor_copy(out=lh[s], in_=bc_cols(tabf[0:33, h:h + 1], 128)),
                      reads=["tabf"], writes=[("lh", s)])
                S.add("pe", lambda e, s=s, which=which: e.matmul(pb[s][:, 0:384], lhsT=lh[s], rhs=ohb[:, which, :],
                                                                  start=True, stop=True),
                      reads=[("lh", s), "ohb"], writes=[("pb", s)])
                S.add("act", lambda e, s=s: e.activation(out=zsb[s], in_=pb[s][:, 0:384], func=AF.Copy),
                      reads=[("pb", s)], writes=[("zsb", s)])
                dst = (ZA_d if which == 1 else Zf_d)[h]
                zkey = ("Z", which, h)
                S.add("sp", lambda e, s=s, dst=dst: e.dma_start(out=dst, in_=zsb[s]),
                      reads=[("zsb", s)], writes=[zkey], dma=True)
        for h in range(16):
            zt = Zf_d.tensor
            base = h * 128 * 384
            S.add("pool", lambda e, h=h, base=base: e.dma_start(
                out=T0all[:, h, :], in_=bass.AP(Zf_d.tensor, base + 128, [[383, 128], [1, 128]])),
                reads=[("Z", 0, h)], writes=["T0all"], dma=True)
            if h < 8:
                S.add("pool", lambda e, h=h, base=base: e.dma_start(
                    out=T1all[:, h, :], in_=bass.AP(ZA_d.tensor, base + 256, [[383, 128], [1, 128]])),
                    reads=[("Z", 1, h)], writes=["T1all"], dma=True)
            else:
                S.add("pool", lambda e, h=h, base=base: e.dma_start(
                    out=T1all[:, h, :], in_=bass.AP(Zf_d.tensor, base + 256, [[383, 128], [1, 128]])),
                    reads=[("Z", 0, h)], writes=["T1all"], dma=True)
        S.barrier()
        A.release(m)

    def norm_sub(xs_ap, xkey, gtile, gkey, stat, skey, hb_ap, hkey, junk, width=D):
        if junk is None:
            jout, jkey = hb_ap, hkey
        else:
            jout, jkey = junk[:, 0:width], "junk"
        S.add("act", lambda e: e.activation(out=jout, in_=xs_ap, func=AF.Square, accum_out=stat[:, 0:1]),
              reads=[xkey], writes=[jkey, skey])
        S.add("act", lambda e: e.activation(out=stat[:, 1:2], in_=stat[:, 0:1], func=AF.Sqrt, scale=1.0 / width, bias=EPS),
              reads=[skey], writes=[skey])
        S.add("dve", lambda e: e.reciprocal(out=stat[:, 2:3], in_=stat[:, 1:2]), reads=[skey], writes=[skey])
        S.add("dve", lambda e: e.scalar_tensor_tensor(out=hb_ap, in0=xs_ap, scalar=stat[:, 2:3], in1=gtile,
                                                      op0=ALU.mult, op1=ALU.mult),
              reads=[xkey, skey, gkey], writes=[hkey])

    def precast(order):
        srcs = {"w1g": w1g_d, "w1u": w1u_d, "w1d": w1d_d, "w2g": w2g_d, "w2u": w2u_d, "w2d": w2d_d, "win": win_d}
        for (l, nm_) in order:
            dstt = wbf[(l, nm_)]
            rows = dstt.shape[0]
            nparts = 4
            step = rows // nparts
            for p in range(nparts):
                r0, r1 = p * step, (rows if p == nparts - 1 else (p + 1) * step)
                S.add("pool", lambda e, l=l, nm_=nm_, r0=r0, r1=r1, dstt=dstt: e.dma_start(out=dstt[r0:r1, :], in_=srcs[nm_][l, r0:r1, :]),
                      writes=[("wbf", l, nm_, p)], dma=True, bg=True)

    def ffn_phase(l, wg_d, wu_d, wd_d, norm_row, src, dst, pre=None):
        m = A.mark()
        wg = A.alloc("wg", 128, [8, FF], BF16)
        wu = A.alloc("wu", 128, [8, FF], BF16)
        wd = A.alloc("wd", 128, [NJ, D], BF16)
        gb = A.alloc("gb", 128, D, F32)
        NXS = 5
        xs = [A.alloc("xs%d" % s, 128, D, F32) for s in range(NXS)]
        hb1 = A.alloc("hb0", 128, D, BF16)
        hb = [hb1, hb1]
        hT = A.alloc("hT", 128, [8, 512], BF16)
        actT = A.alloc("actT", 128, [NJ, 512], BF16)
        sg = [A.alloc("sg%d" % s, 128, 512, F32) for s in range(2)]
        stat = A.alloc("stat", 128, [NXS, 4], F32)
        junk = None
        S.add("sp", lambda e: e.dma_start(out=gb, in_=norm_row.to_broadcast([128, D])), writes=["gb"], dma=True)
        if pre is None:
            for k in range(8):
                S.add("pool", lambda e, k=k: e.dma_start(out=wg[:, k, :], in_=wg_d[l, k * 128:(k + 1) * 128, :]),
                      writes=[("wg", k)], dma=True)
                S.add("pool", lambda e, k=k: e.dma_start(out=wu[:, k, :], in_=wu_d[l, k * 128:(k + 1) * 128, :]),
                      writes=[("wu", k)], dma=True)
            for j in range(NJ):
                S.add("pool", lambda e, j=j: e.dma_start(out=wd[:, j, :], in_=wd_d[l, j * 128:(j + 1) * 128, :]),
                      writes=[("wd", j)], dma=True)
            precast([(0, "win"), (0, "w2g"), (0, "w2u"), (0, "w2d")])
        else:
            gname, uname, dname = pre
            gb_, ub_, db_ = wbf[(l, gname)], wbf[(l, uname)], wbf[(l, dname)]
            allk = lambda nm_: [("wbf", l, nm_, p) for p in range(4)]
            nq = 0
            for k in range(8):
                S.add("sp" if nq % 2 == 0 else "act", lambda e, k=k: e.dma_start(out=wg[:, k, :], in_=gb_[k * 128:(k + 1) * 128, :]),
                      reads=allk(gname), writes=[("wg", k)], dma=True)
                nq += 1
                S.add("sp" if nq % 2 == 0 else "act", lambda e, k=k: e.dma_start(out=wu[:, k, :], in_=ub_[k * 128:(k + 1) * 128, :]),
                      reads=allk(uname), writes=[("wu", k)], dma=True)
                nq += 1
            for j in range(NJ):
                S.add("sp" if nq % 2 == 0 else "act", lambda e, j=j: e.dma_start(out=wd[:, j, :], in_=db_[j * 128:(j + 1) * 128, :]),
                      reads=allk(dname), writes=[("wd", j)], dma=True)
                nq += 1
        for tt in range(NTT):
            for sub in range(4):
                u = tt * 4 + sub
                sl = u % NXS
                S.add("sp", lambda e, u=u, sl=sl: e.dma_start(out=xs[sl], in_=src[u * 128:(u + 1) * 128, :]),
                      reads=[("xres", u)], writes=[("xs", sl)], dma=True)
                norm_sub(xs[sl], ("xs", sl), gb, "gb", stat[:, sl, :], ("stat", sl), hb[0], ("hb", 0), junk)
                for k in range(8):
                    S.add("pe", lambda e, u=u, k=k: e.transpose(out=pbh[0][:, k * 128:(k + 1) * 128],
                                                                in_=hb[0][:, k * 128:(k + 1) * 128], identity=ident),
                          reads=[("hb", 0), "ident"], writes=["pb0"])
                S.add("act", lambda e, sub=sub: e.activation(
                    out=hT[:, :, sub * 128:(sub + 1) * 128],
                    in_=pbh[0].rearrange("p (k t) -> p k t", t=128), func=AF.Copy),
                    reads=["pb0"], writes=[("hT", sub)])
            hkeys = [("hT", s) for s in range(4)]
            for j in range(NJ):
                pg = 1 + j % 2
                pu = 3 + j % 2
                for k in range(8):
                    S.add("pe", lambda e, j=j, k=k, pg=pg: e.matmul(pb[pg], lhsT=wg[:, k, j * 128:(j + 1) * 128],
                                                                    rhs=hT[:, k, :], start=(k == 0), stop=(k == 7)),
                          reads=[("wg", k)] + hkeys, writes=[("pb", pg)])
                for k in range(8):
                    S.add("pe", lambda e, j=j, k=k, pu=pu: e.matmul(pb[pu], lhsT=wu[:, k, j * 128:(j + 1) * 128],
                                                                    rhs=hT[:, k, :], start=(k == 0), stop=(k == 7)),
                          reads=[("wu", k)] + hkeys, writes=[("pb", pu)])
                S.add("act", lambda e, j=j, pg=pg: e.activation(out=sg[j % 2], in_=pb[pg], func=AF.Silu),
                      reads=[("pb", pg)], writes=[("sg", j % 2)])
                S.add("dve", lambda e, j=j, pu=pu: e.tensor_tensor(out=actT[:, j, :], in0=sg[j % 2], in1=pb[pu], op=ALU.mult),
                      reads=[("sg", j % 2), ("pb", pu)], writes=[("actT", j)])
            n = 0
            for sub in range(4):
                u = tt * 4 + sub
                sl = u % NXS
                for half in range(2):
                    py = 5 + n % 2
                    n += 1
                    for j in range(NJ):
                        S.add("pe", lambda e, j=j, sub=sub, half=half, py=py: e.matmul(
                            pb[py], lhsT=actT[:, j, sub * 128:(sub + 1) * 128],
                            rhs=wd[:, j, half * 512:(half + 1) * 512], start=(j == 0), stop=(j == NJ - 1)),
                            reads=[("actT", j), ("wd", j)], writes=[("pb", py)])
                    S.add("dve", lambda e, sl=sl, half=half, py=py: e.scalar_tensor_tensor(
                        out=xs[sl][:, half * 512:(half + 1) * 512], in0=pb[py], scalar=0.5,
                        in1=xs[sl][:, half * 512:(half + 1) * 512], op0=ALU.mult, op1=ALU.add),
                        reads=[("pb", py), ("xs", sl)], writes=[("xs", sl)])
                S.add("sp", lambda e, u=u, sl=sl: e.dma_start(out=dst[u * 128:(u + 1) * 128, :], in_=xs[sl]),
                      reads=[("xs", sl)], writes=[("xres", u)], dma=True)
        S.barrier()
        A.release(m)

    FM_COLS = [(0, 512), (512, 640), (768, 1024), (1024, 1280), (1536, 1792), (1920, 2176), (2176, 2240)]
    QSCALE = [True] * 8 + [False] * 2 + [True] * 4 + [False] * 4 + [True] * 4 + [True] * 4 + [False]

    def proj_phase(l):
        m = A.mark()
        wfm = A.alloc("wfm", 128, [8, 1728], BF16)
        wtm = A.alloc("wtm", 128, [8, 576], BF16)
        wupb = A.alloc("wupb", 128, 128, BF16)
        gb = A.alloc("gb", 128, D, F32)
        gkv = A.alloc("gkv", 128, 128, F32)
        xs = [A.alloc("xs%d" % s, 128, D, F32) for s in range(2)]
        hb = [A.alloc("hb%d" % s, 128, D, BF16) for s in range(2)]
        hT = A.alloc("hT", 128, [8, 512], BF16)
        hd = A.alloc("hd", 64, [28, 512], BF16)
        vtm = A.alloc("vtm", 128, [4, 448], BF16)
        ck = A.alloc("ck", 128, 128, F32)
        ckn = A.alloc("ckn", 128, 128, BF16)
        cknT = A.alloc("cknT", 128, 512, BF16)
        wis = A.alloc("wis", 128, [4, 16], F32)
        stat = A.alloc("stat", 128, [2, 4], F32)
        stat2 = A.alloc("stat2", 128, 4, F32)
        junk = A.alloc("junk", 128, D, BF16)
        S.add("sp", lambda e: e.dma_start(out=gb, in_=nm_d[l:l + 1, :].to_broadcast([128, D])), writes=["gb"], dma=True)
        S.add("sp", lambda e: e.dma_start(out=gkv, in_=kvn_d[l:l + 1, :].to_broadcast([128, 128])), writes=["gkv"], dma=True)
        S.add("pool", lambda e: e.dma_start(out=wupb, in_=wup_d[l]), writes=["wupb"], dma=True)
        winb = wbf[(l, "win")]
        wkeys = [("wbf", l, "win", p) for p in range(4)]
        nq = 0
        for k in range(8):
            c = 0
            for (a, b) in FM_COLS:
                S.add("sp" if nq % 2 == 0 else "act", lambda e, k=k, a=a, b=b, c=c: e.dma_start(
                    out=wfm[:, k, c:c + (b - a)], in_=winb[k * 128:(k + 1) * 128, a:b]),
                    reads=wkeys, writes=[("wfm", k)], dma=True)
                nq += 1
                c += b - a
            for (a, b, c) in [(640, 768, 0), (1280, 1536, 128), (1792, 1920, 384), (2180, 2244, 512)]:
                S.add("sp" if nq % 2 == 0 else "act", lambda e, k=k, a=a, b=b, c=c: e.dma_start(
                    out=wtm[:, k, c:c + (b - a)], in_=winb[k * 128:(k + 1) * 128, a:b]),
                    reads=wkeys, writes=[("wtm", k)], dma=True)
                nq += 1
        nev = 0
        PSTOP = int(os.environ.get("PROJ_STOP", "99"))
        for tt in range(NTT if PSTOP > 1 else 0):
            for sub in range(4):
                u = tt * 4 + sub
                sl = u % 2
                S.add("sp", lambda e, u=u, sl=sl: e.dma_start(out=xs[sl], in_=xres[u * 128:(u + 1) * 128, :]),
                      reads=[("xres", u)], writes=[("xs", sl)], dma=True)
                norm_sub(xs[sl], ("xs", sl), gb, "gb", stat[:, sl, :], ("stat", sl), hb[sl], ("hb", sl), junk)
                for k in range(8):
                    S.add("pe", lambda e, sl=sl, k=k: e.transpose(out=pbh[0][:, k * 128:(k + 1) * 128],
                                                                  in_=hb[sl][:, k * 128:(k + 1) * 128], identity=ident),
                          reads=[("hb", sl), "ident"], writes=["pb0"])
                S.add("act", lambda e, sub=sub: e.activation(
                    out=hT[:, :, sub * 128:(sub + 1) * 128],
                    in_=pbh[0].rearrange("p (k t) -> p k t", t=128), func=AF.Copy),
                    reads=["pb0"], writes=[("hT", sub)])
            hkeys = [("hT", s) for s in range(4)]
            for sub in range(4):
                u = tt * 4 + sub
                for k in range(8):
                    S.add("pe", lambda e, sub=sub, k=k: e.matmul(pb[4], lhsT=hT[:, k, sub * 128:(sub + 1) * 128],
                                                                 rhs=wtm[:, k, 0:512], start=(k == 0), stop=(k == 7)),
                          reads=[("wtm", k), ("hT", sub)], writes=[("pb", 4)])
                for k in range(8):
                    S.add("pe", lambda e, sub=sub, k=k: e.matmul(pb[5][:, 0:64], lhsT=hT[:, k, sub * 128:(sub + 1) * 128],
                                                                 rhs=wtm[:, k, 512:576], start=(k == 0), stop=(k == 7)),
                          reads=[("wtm", k), ("hT", sub)], writes=[("pb", 5)])
                S.add("act", lambda e, sub=sub: e.activation(out=vtm[:, sub, 0:384], in_=pb[4][:, 0:384], func=AF.Copy),
                      reads=[("pb", 4)], writes=[("vtm", sub)])
                S.add("act", lambda e: e.activation(out=ck, in_=pb[4][:, 384:512], func=AF.Copy), reads=[("pb", 4)], writes=["ck"])
                S.add("dve", lambda e, sub=sub: e.tensor_scalar(out=wis[:, sub, :], in0=pb[5][:, 48:64], scalar1=0.5,
                                                                scalar2=None, op0=ALU.mult),
                      reads=[("pb", 5)], writes=["wis"])
                norm_sub(ck, "ck", gkv, "gkv", stat2, "stat2", ckn, "ckn", junk, width=128)
                S.add("pe", lambda e: e.transpose(out=pbh[6][:, 0:128], in_=ckn, identity=ident),
                      reads=["ckn", "ident"], writes=[("pb", 6)])
                S.add("act", lambda e, sub=sub: e.activation(out=cknT[:, sub * 128:(sub + 1) * 128], in_=pbh[6][:, 0:128],
                                                             func=AF.Copy),
                      reads=[("pb", 6)], writes=[("cknT", sub)])
                S.add("pe", lambda e, sub=sub: e.matmul(pb[7][:, 0:64], lhsT=cknT[:, sub * 128:(sub + 1) * 128],
                                                        rhs=wupb[:, 64:128], start=True, stop=True),
                      reads=[("cknT", sub), "wupb"], writes=[("pb", 7)])
                S.add("dve", lambda e, sub=sub: e.tensor_copy(out=vtm[:, sub, 384:448], in_=pb[7][:, 0:64]),
                      reads=[("pb", 7)], writes=[("vtm", sub)])
            for f in range(27 if PSTOP > 2 else 0):
                pf = 1 + f % 3
                for k in range(8):
                    S.add("pe", lambda e, f=f, k=k, pf=pf: e.matmul(pb[pf][0:64, :], lhsT=wfm[:, k, f * 64:(f + 1) * 64],
                                                                    rhs=hT[:, k, :], start=(k == 0), stop=(k == 7)),
                          reads=[("wfm", k)] + hkeys, writes=[("pb", pf)])
                sc = 0.125 if QSCALE[f] else 1.0
                if nev % 2 == 0:
                    S.add("act", lambda e, f=f, pf=pf, sc=sc: e.activation(out=hd[:, f, :], in_=pb[pf][0:64, :],
                                                                           func=AF.Copy, scale=sc),
                          reads=[("pb", pf)], writes=[("hd", f)])
                else:
                    S.add("dve", lambda e, f=f, pf=pf, sc=sc: e.tensor_scalar(out=hd[:, f, :], in0=pb[pf][0:64, :],
                                                                              scalar1=sc, scalar2=None, op0=ALU.mult),
                          reads=[("pb", pf)], writes=[("hd", f)])
                nev += 1
            S.add("pe", lambda e: e.matmul(pb[1][0:64, :], lhsT=wupb[:, 0:64], rhs=cknT, start=True, stop=True),
                  reads=[("cknT", s) for s in range(4)] + ["wupb"], writes=[("pb", 1)])
            S.add("act", lambda e: e.activation(out=hd[:, 27, :], in_=pb[1][0:64, :], func=AF.Copy),
                  reads=[("pb", 1)], writes=[("hd", 27)])
            t0 = tt * 512
            if PSTOP <= 4:
                continue

            def fm_out(dst3, f0, nf, key):
                S.add("sp", lambda e: e.dma_start(out=dst3, in_=hd[:, f0:f0 + nf, :]),
                      reads=[("hd", f) for f in range(f0, f0 + nf)], writes=[key], dma=True)
            fm_out(qaT_d[:, :, t0:t0 + 512].rearrange("h d t -> d h t"), 0, 8, ("qaT", tt))
            fm_out(KT_t[tt][0:128, :].rearrange("(h d) t -> d h t", d=64), 8, 2, ("KTa", tt))
            fm_out(qbT_d[:, :, t0:t0 + 512].rearrange("h d t -> d h t"), 10, 4, ("qbT", tt))
            fm_out(KT_t[tt][128:384, :].rearrange("(h d) t -> d h t", d=64), 14, 4, ("KTb", tt))
            fm_out(qcT_d[:, :, t0:t0 + 512].rearrange("h d t -> d h t"), 18, 4, ("qcT", tt))
            fm_out(qiT_d[:, :, t0:t0 + 512].rearrange("h d t -> d h t"), 22, 4, ("qiT", tt))
            fm_out(KT_t[tt][448:512, :].rearrange("(h d) t -> d h t", d=64), 26, 1, ("KTi", tt))
            fm_out(KT_t[tt][384:448, :].rearrange("(h d) t -> d h t", d=64), 27, 1, ("KTc", tt))
            S.add("sp", lambda e, tt=tt: e.dma_start(out=VT_t[tt].rearrange("(s p) c -> p s c", p=128), in_=vtm),
                  reads=[("vtm", s) for s in range(4)], writes=[("VT", tt)], dma=True)
            S.add("sp", lambda e, t0=t0: e.dma_start(out=wi_d[t0:t0 + 512, :].rearrange("(s p) c -> p s c", p=128), in_=wis),
                  reads=["wis"], writes=[("wi", tt)], dma=True)
        S.barrier()
        A.release(m)

    def exchange():
        if os.environ.get("SKIP_XCHG"):
            return
        groups = [[2 * g, 2 * g + 1] for g in range(4)]
        for t in range(NTT):
            S.add("pool", lambda e, t=t: e.collective_compute("AllGather", ALU.bypass, replica_groups=groups,
                                                              ins=[KT_t[t]], outs=[gKT_t[t]]),
                  reads=["ccorder"], writes=[("gKT", t), "ccorder"], cc=True)
            S.add("pool", lambda e, t=t: e.collective_compute("AllGather", ALU.bypass, replica_groups=groups,
                                                              ins=[VT_t[t]], outs=[gVT_t[t]]),
                  reads=["ccorder"], writes=[("gVT", t), "ccorder"], cc=True)
        S.barrier()

    def finalize(pO, pD, okey, dkey, nh, dst3, dkey_out, extra=None, tmp=None):
        dn, oc, osb = tmp
        W = nh * 128
        S.add("act", lambda e: e.activation(out=osb[:, 0:W], in_=pO[0:65, 0:W], func=AF.Copy), reads=[okey], writes=["osb"])
        S.add("pe", lambda e: e.matmul(pD[0:64, 0:W], lhsT=selT, rhs=osb[:, 0:W], start=True, stop=True),
              reads=["selT", "osb"], writes=[dkey])
        if extra is not None:
            S.add("dve", lambda e: e.tensor_tensor(out=dn[:, 0:W], in0=pD[0:64, 0:W], in1=extra, op=ALU.add),
                  reads=[dkey, "exps"], writes=["dn"])
            S.add("dve", lambda e: e.reciprocal(out=dn[:, 0:W], in_=dn[:, 0:W]), reads=["dn"], writes=["dn"])
        else:
            S.add("dve", lambda e: e.reciprocal(out=dn[:, 0:W], in_=pD[0:64, 0:W]), reads=[dkey], writes=["dn"])
        S.add("dve", lambda e: e.tensor_tensor(out=oc[:, 0:W], in0=osb[0:64, 0:W], in1=dn[:, 0:W], op=ALU.mult),
              reads=["osb", "dn"], writes=["oc"])
        S.add("sp", lambda e: e.dma_start(out=dst3, in_=oc[:, 0:W].rearrange("p (h t) -> p h t", t=128)),
              reads=["oc"], writes=[dkey_out], dma=True)

    def swa_phase(l):
        m = A.mark()
        kaT = A.alloc("kaT", 64, [2, TC], BF16)
        vaO = A.alloc("vaO", 128, [NT, 2, 65], BF16)
        kaH = A.alloc("kaH", 65, [2, 128], BF16)
        vaH = A.alloc("vaH", 128, [2, 65], BF16)
        exps8 = A.alloc("exps8", 64, 8, F32)
        exps = A.alloc("exps", 64, [8, 128], F32)
        qa = [A.alloc("qa%d" % s, 65, [8, 128], BF16) for s in range(2)]
        PT = [A.alloc("PT%d" % s, 128, 512, BF16) for s in range(2)]
        dn = A.alloc("dn", 64, 512, F32)
        oc = A.alloc("oc", 64, 512, BF16)
        osb = A.alloc("osb", 65, 512, F32)
        S.add("dve", lambda e: e.memset(vaO[:, :, :, 64:65], 1.0), writes=["vaO"])
        S.add("dve", lambda e: e.memset(vaH[:, :, 64:65], 1.0), writes=["vaH"])
        for t in range(NTT):
            S.add("sp", lambda e, t=t: e.dma_start(out=kaT[:, :, t * 512:(t + 1) * 512],
                                                   in_=KT_t[t][0:128, :].rearrange("(g d) t -> d g t", d=64)),
                  reads=[("KTa", t)], writes=["kaT"], dma=True)
            for g in range(2):
                S.add("sp", lambda e, t=t, g=g: e.dma_start(out=vaO[:, 4 * t:4 * t + 4, g, 0:64],
                                                            in_=VT_t[t][:, g * 64:(g + 1) * 64].rearrange("(n p) d -> p n d", p=128)),
                      reads=[("VT", t)], writes=["vaO"], dma=True)
        S.add("sp", lambda e: e.dma_start(out=kaH[0:64, :, :],
                                          in_=gKT_t[NTT - 1][0:128, 384:512].rearrange("(g d) t -> d g t", d=64)),
              reads=[("gKT", NTT - 1)], writes=["kaH"], dma=True)
        S.add("dve", lambda e: e.memset(kaH[64:65, :, :], 1.0), writes=["kaH"])
        S.add("sp", lambda e: e.dma_start(out=vaH[:, :, 0:64], in_=gVT_t[NTT - 1][384:512, 0:128].rearrange("p (g d) -> p g d", d=64)),
              reads=[("gVT", NTT - 1)], writes=["vaH"], dma=True)
        S.add("sp", lambda e: e.dma_start(out=exps8, in_=sink_d[l:l + 1, :].to_broadcast([64, 8])), writes=["exps8"], dma=True)
        S.add("act", lambda e: e.activation(out=exps8, in_=exps8, func=AF.Exp), reads=["exps8"], writes=["exps8"])
        S.add("dve", lambda e: e.tensor_copy(out=exps, in_=bc_last(exps8, 128)), reads=["exps8"], writes=["exps"])
        for s in range(2):
            S.add("dve", lambda e, s=s: e.tensor_copy(out=flat2(qa[s][64:65, :, :]), in_=bc_cols(pfxc[64:65, 0:1], 1024)),
                  reads=["pfxc"], writes=[("qa", s)])
        for i in range(NT):
            s = i % 2
            S.add("sp", lambda e, i=i, s=s: e.dma_start(out=qa[s][0:64, :, :],
                                                        in_=qaT_d[:, :, i * 128:(i + 1) * 128].rearrange("h d t -> d h t")),
                  reads=[("qaT", i // 4)], writes=[("qa", s)], dma=True)
            for g in range(2):
                chunks = []
                if i == 0:
                    chunks.append((kaH[0:65, g, :], 65, vaH[:, g, :], T1all, ["kaH", "vaH"]))
                else:
                    chunks.append((kaT[:, g, (i - 1) * 128:i * 128], 64, vaO[:, i - 1, g, :], T1all,
                                   ["kaT", "vaO"]))
                chunks.append((kaT[:, g, i * 128:(i + 1) * 128], 64, vaO[:, i, g, :], T0all, ["kaT", "vaO"]))
                pO, pD = pb[4 + 2 * (g % 2)], pb[5 + 2 * (g % 2)]
                okey, dkey = ("pb", 4 + 2 * (g % 2)), ("pb", 5 + 2 * (g % 2))
                for ci, (kap, r, vap, Tt, kk) in enumerate(chunks):
                    ps = (2 * g + ci) % 4
                    S.add("pe", lambda e, kap=kap, r=r, ps=ps, s=s, g=g: e.matmul(
                        pb[ps], lhsT=kap, rhs=flat2(qa[s][0:r, 4 * g:4 * g + 4, :]), start=True, stop=False),
                        reads=kk + [("qa", s)], writes=[("pb", ps)])
                    S.add("pe", lambda e, Tt=Tt, ps=ps, g=g: e.matmul(
                        pb[ps], lhsT=ident, rhs=flat2(Tt[:, 4 * g:4 * g + 4, :]), start=False, stop=True),
                        reads=["ident", "T0all", "T1all"], writes=[("pb", ps)])
                    S.add("act", lambda e, ps=ps, ci=ci: e.activation(out=PT[ci], in_=pb[ps], func=AF.Exp),
                          reads=[("pb", ps)], writes=[("PT", ci)])
                for ci, (kap, r, vap, Tt, kk) in enumerate(chunks):
                    S.add("pe", lambda e, vap=vap, ci=ci, pO=pO: e.matmul(pO[0:65, :], lhsT=vap, rhs=PT[ci],
                                                                          start=(ci == 0), stop=(ci == 1)),
                          reads=kk + [("PT", ci)], writes=[okey])
                finalize(pO, pD, okey, dkey, 4,
                         catT_d[4 * g:4 * g + 4, :, i * 128:(i + 1) * 128].rearrange("h d t -> d h t"),
                         ("catT", i), extra=flat2(exps[:, 4 * g:4 * g + 4, :]), tmp=(dn, oc, osb))
        S.barrier()
        A.release(m)

    def moba_phase(l):
        m = A.mark()
        kb = A.alloc("kb", 97, [4, T2], BF16)
        vb = A.alloc("vb", 128, [NC2, 4, 65], BF16)
        kmf = A.alloc("kmf", 64, [4, GW], F32)
        kmT = A.alloc("kmT", 64, [4, GW], BF16)
        qb = [A.alloc("qb%d" % s, 97, [4, 128], BF16) for s in range(2)]
        bsel = [A.alloc("bsel%d" % s, 128, [4, 96], BF16) for s in range(2)]
        gt = A.alloc("gt", 128, [4, GW], F32)
        mx8 = A.alloc("mx8", 128, [4, 8], F32)
        thr = A.alloc("thr", 128, 4, F32)
        PT = [A.alloc("PT%d" % s, 128, 512, BF16) for s in range(4)]
        SBANK = [2, 3, 5, 7]
        dn = A.alloc("dn", 64, 512, F32)
        oc = A.alloc("oc", 64, 512, BF16)
        osb = A.alloc("osb", 65, 512, F32)
        S.add("dve", lambda e: e.memset(vb[:, :, :, 64:65], 1.0), writes=["vb"])
        for t in range(NTT):
            S.add("sp", lambda e, t=t: e.dma_start(out=kb[0:64, :, t * 512:(t + 1) * 512],
                                                   in_=gKT_t[t][128:384, :].rearrange("(h d) t -> d h t", d=64)),
                  reads=[("gKT", t)], writes=["kbk"], dma=True)
            S.add("sp", lambda e, t=t: e.dma_start(out=kb[0:64, :, TC + t * 512:TC + (t + 1) * 512],
                                                   in_=KT_t[t][128:384, :].rearrange("(h d) t -> d h t", d=64)),
                  reads=[("KTb", t)], writes=["kbk"], dma=True)
            for h in range(4):
                S.add("sp", lambda e, t=t, h=h: e.dma_start(
                    out=vb[:, 4 * t:4 * t + 4, h, 0:64],
                    in_=gVT_t[t][0:512, 128 + h * 64:128 + (h + 1) * 64].rearrange("(n p) d -> p n d", p=128)),
                    reads=[("gVT", t)], writes=["vb"], dma=True)
            for h in range(4):
                S.add("sp", lambda e, t=t, h=h: e.dma_start(
                    out=vb[:, NT + 4 * t:NT + 4 * t + 4, h, 0:64],
                    in_=VT_t[t][:, 128 + h * 64:128 + (h + 1) * 64].rearrange("(n p) d -> p n d", p=128)),
                    reads=[("VT", t)], writes=["vb"], dma=True)
        for h in range(4):
            S.add("pool", lambda e, h=h: e.dma_start(out=kb[64:97, h, :], in_=er_d), writes=["kbk"], dma=True)
        S.add("dve", lambda e: e.memset(kmf, 0.0), writes=["kmf"])
        for h in range(4):
            S.add("dve", lambda e, h=h: e.reduce_sum(out=kmf[:, h, 0:NB],
                                                     in_=kb[0:64, h, :].rearrange("p (n s) -> p n s", s=256), axis=AX.X),
                  reads=["kbk", "kmf"], writes=["kmf"])
        S.add("dve", lambda e: e.tensor_scalar(out=kmT, in0=kmf, scalar1=1.0 / 256, scalar2=None, op0=ALU.mult),
              reads=["kmf"], writes=["kmT"])
        for s in range(2):
            S.add("sp", lambda e, s=s: e.dma_start(out=qb[s][96:97, :, :], in_=b31s[0:1, 0:4, :]),
                  reads=["b31s"], writes=[("qb", s)], dma=True)
            S.add("dve", lambda e, s=s: e.memset(bsel[s][:, :, 0:64], 0.0), writes=[("bsel", s)])
            S.add("dve", lambda e, s=s: e.memset(bsel[s][:, :, 64:96], -1.0), writes=[("bsel", s)])
        for i in range(NT):
            s = i % 2
            nb = NBH + i // 2
            cq = NT + i
            ob0 = cq - (i % 2)
            S.add("sp", lambda e, i=i, s=s: e.dma_start(out=qb[s][0:64, :, :],
                                                        in_=qbT_d[:, :, i * 128:(i + 1) * 128].rearrange("h d t -> d h t")),
                  reads=[("qbT", i // 4)], writes=[("qb", s)], dma=True)
            for h in range(4):
                S.add("pe", lambda e, s=s, h=h: e.matmul(pb[0][:, h * GW:(h + 1) * GW], lhsT=qb[s][0:64, h, :],
                                                         rhs=kmT[:, h, :], start=(h == 0), stop=(h == 3)),
                      reads=[("qb", s), "kmT"], writes=[("pb", 0)])
            S.add("dve", lambda e: e.tensor_tensor(out=gt, in0=pb[0][:, 0:4 * GW].rearrange("p (h n) -> p h n", n=GW),
                                                   in1=bc_mid(pfxrow, 4), op=ALU.add),
                  reads=[("pb", 0), "pfxrow"], writes=["gt"])
            S.add("dve", lambda e, nb=nb: e.tensor_tensor(out=gt, in0=gt, in1=bc_mid(stepc[:, GW - nb:2 * GW - nb], 4), op=ALU.add),
                  reads=["gt", "stepc"], writes=["gt"])
            for h in range(4):
                S.add("dve", lambda e, h=h: e.max(out=mx8[:, h, :], in_=gt[:, h, :]), reads=["gt"], writes=["mx8"])
            S.add("dve", lambda e: e.tensor_scalar(out=thr, in0=mx8[:, :, 2], scalar1=-16000.0, scalar2=None, op0=ALU.max),
                  reads=["mx8"], writes=["thr"])
            for h in range(4):
                S.add("dve", lambda e, s=s, h=h, nb=nb: e.tensor_scalar(out=bsel[s][:, h, 64:64 + nb], in0=gt[:, h, 0:nb],
                                                                        scalar1=thr[:, h:h + 1], scalar2=-1.0,
                                                                        op0=ALU.is_ge, op1=ALU.add),
                      reads=["gt", "thr"], writes=[("bsel", s)])
            for h in range(4):
                S.add("pe", lambda e, s=s, h=h: e.transpose(out=pbh[1][0:96, h * 128:(h + 1) * 128], in_=bsel[s][:, h, :],
                                                            identity=ident),
                      reads=[("bsel", s), "ident"], writes=[("pb", 1)])
            S.add("act", lambda e, s=s: e.activation(out=flat2(qb[s][64:96, :, :]), in_=pbh[1][64:96, 0:512], func=AF.Copy),
                  reads=[("pb", 1)], writes=[("qb", s)])
            pO, pD = pb[4 + 2 * s], pb[1]
            okey, dkey = ("pb", 4 + 2 * s), ("pb", 1)
            def qk(c, i=i, s=s, cq=cq, ob0=ob0):
                ps = SBANK[c % 4]
                Tt = None
                if c == cq:
                    r, Tt = 64, T0all
                elif c == cq - 1 and c >= ob0:
                    r, Tt = 64, T1all
                elif c == cq - 1:
                    r, Tt = 96, T1all
                else:
                    r = 97
                for h in range(4):
                    S.add("pe", lambda e, h=h, r=r, ps=ps, Tt=Tt: e.matmul(
                        pb[ps][:, h * 128:(h + 1) * 128], lhsT=kb[0:r, h, c * 128:(c + 1) * 128], rhs=qb[s][0:r, h, :],
                        start=(h == 0), stop=(Tt is None and h == 3)),
                        reads=["kbk", ("qb", s)], writes=[("pb", ps)])
                if Tt is not None:
                    S.add("pe", lambda e, ps=ps, Tt=Tt: e.matmul(pb[ps], lhsT=ident, rhs=flat2(Tt[:, 8:12, :]),
                                                                 start=False, stop=True),
                          reads=["ident", "T0all", "T1all"], writes=[("pb", ps)])
                S.add("act", lambda e, ps=ps: e.activation(out=PT[c % 4], in_=pb[ps], func=AF.Exp),
                      reads=[("pb", ps)], writes=[("PT", c % 4)])

            def pv(c, pO=pO, cq=cq, okey=okey):
                for h in range(4):
                    S.add("pe", lambda e, h=h: e.matmul(
                        pO[0:65, h * 128:(h + 1) * 128], lhsT=vb[:, c, h, :], rhs=PT[c % 4][:, h * 128:(h + 1) * 128],
                        start=(c == 0 and h == 0), stop=(c == cq and h == 3)),
                        reads=["vb", ("PT", c % 4)], writes=[okey])

            qk(0)
            if cq >= 1:
                qk(1)
            for c in range(cq + 1):
                if c + 2 <= cq:
                    qk(c + 2)
                pv(c)
            finalize(pO, pD, okey, dkey, 4, catT_d[8:12, :, i * 128:(i + 1) * 128].rearrange("h d t -> d h t"),
                     ("catT", i), tmp=(dn, oc, osb))
        S.barrier()
        A.release(m)

    def dsa_phase(l):
        m = A.mark()
        ki = A.alloc("ki", 64, T2, BF16)
        kc = A.alloc("kc", 65, T2, BF16)
        vc = A.alloc("vc", 128, [NC2, 65], BF16)
        Ib = A.alloc("Ib", 128, T2, F32)
        jk = A.alloc("jk", 128, T2, BF16)
        jk2 = A.alloc("jk2", 128, T2 // 2, BF16)
        mb = [A.alloc("mb%d" % s, 128, T2, BF16) for s in range(2)]
        rl = [A.alloc("rl%d" % h, 128, 512, F32) for h in range(4)]
        qi = [A.alloc("qi%d" % s, 64, [4, 128], BF16) for s in range(2)]
        qc = [A.alloc("qc%d" % s, 65, [4, 128], BF16) for s in range(2)]
        wt = [A.alloc("wt%d" % s, 128, 16, F32) for s in range(2)]
        bs = A.alloc("bs", 128, 8, F32)
        PT = [A.alloc("PT%d" % s, 128, 512, BF16) for s in range(2)]
        dn = A.alloc("dn", 64, 512, F32)
        oc = A.alloc("oc", 64, 512, BF16)
        osb = A.alloc("osb", 65, 512, F32)
        lo, mid, cnt, dl = bs[:, 0:1], bs[:, 1:2], bs[:, 2:3], bs[:, 3:4]
        sgn, tt_ = bs[:, 4:5], bs[:, 5:6]
        if l + 1 < L:
            precast([(l + 1, n_) for n_ in ("w1g", "w1u", "w1d", "win", "w2g", "w2u", "w2d")])
        S.add("dve", lambda e: e.memset(vc[:, :, 64:65], 1.0), writes=["vc"])
        for t in range(NTT):
            S.add("sp", lambda e, t=t: e.dma_start(out=ki[:, t * 512:(t + 1) * 512], in_=gKT_t[t][448:512, :]),
                  reads=[("gKT", t)], writes=["ki"], dma=True)
            S.add("sp", lambda e, t=t: e.dma_start(out=ki[:, TC + t * 512:TC + (t + 1) * 512], in_=KT_t[t][448:512, :]),
                  reads=[("KTi", t)], writes=["ki"], dma=True)
            S.add("sp", lambda e, t=t: e.dma_start(out=kc[0:64, t * 512:(t + 1) * 512], in_=gKT_t[t][384:448, :]),
                  reads=[("gKT", t)], writes=["kc"], dma=True)
            S.add("sp", lambda e, t=t: e.dma_start(out=kc[0:64, TC + t * 512:TC + (t + 1) * 512], in_=KT_t[t][384:448, :]),
                  reads=[("KTc", t)], writes=["kc"], dma=True)
            S.add("sp", lambda e, t=t: e.dma_start(out=vc[:, 4 * t:4 * t + 4, 0:64],
                                                   in_=gVT_t[t][0:512, 384:448].rearrange("(n p) c -> p n c", p=128)),
                  reads=[("gVT", t)], writes=["vc"], dma=True)
            S.add("sp", lambda e, t=t: e.dma_start(out=vc[:, NT + 4 * t:NT + 4 * t + 4, 0:64],
                                                   in_=VT_t[t][:, 384:448].rearrange("(n p) c -> p n c", p=128)),
                  reads=[("VT", t)], writes=["vc"], dma=True)
        S.add("dve", lambda e: e.memset(kc[64:65, :], 1.0), writes=["kc"])
        for s in range(2):
            S.add("sp", lambda e, s=s: e.dma_start(out=qc[s][64:65, :, :], in_=b31s[0:1, 4:8, :]),
                  reads=["b31s"], writes=[("qc", s)], dma=True)

        def stage_a(i, units=None):
            s = i % 2
            nkc = NT + i + 1
            nk = nkc * 128
            nblk0 = (nk + 511) // 512
            per_hook = 0 if not units else -(-len(units) // (nblk0 + NIT))

            def pump():
                for _ in range(per_hook):
                    if units:
                        units.pop(0)()
            na = min(T2 // 2, int(round(ACT_FRAC * nk / 128.0)) * 128)
            nd = nk - na
            S.add("sp", lambda e: e.dma_start(out=qi[s], in_=qiT_d[:, :, i * 128:(i + 1) * 128].rearrange("h d t -> d h t")),
                  reads=[("qiT", i // 4)], writes=[("qi", s)], dma=True)
            S.add("sp", lambda e: e.dma_start(out=qc[s][0:64, :, :],
                                              in_=qcT_d[:, :, i * 128:(i + 1) * 128].rearrange("h d t -> d h t")),
                  reads=[("qcT", i // 4)], writes=[("qc", s)], dma=True)
            S.add("sp", lambda e: e.dma_start(out=wt[s], in_=wi_d[i * 128:(i + 1) * 128, :]),
                  reads=[("wi", i // 4)], writes=[("wt", s)], dma=True)
            nblk = (nk + 511) // 512
            for b in range(nblk):
                w = min(512, nk - b * 512)
                c0 = b * 512
                pfx_blk = (c0 + w) <= TC
                assert pfx_blk or c0 >= TC
                for h in range(4):
                    S.add("pe", lambda e, h=h, c0=c0, w=w: e.matmul(pb[h][:, 0:w], lhsT=qi[s][:, h, :], rhs=ki[:, c0:c0 + w],
                                                                    start=True, stop=True),
                          reads=[("qi", s), "ki"], writes=[("pb", h)])
                    S.add("act", lambda e, h=h, w=w: e.activation(out=rl[h][:, 0:w], in_=pb[h][:, 0:w], func=AF.Relu),
                          reads=[("pb", h)], writes=[("rl", h)])
                sc2 = pfxc[:, 0:1] if pfx_blk else zero1[:, 0:1]
                S.add("dve", lambda e, c0=c0, w=w, sc2=sc2: e.tensor_scalar(
                    out=Ib[:, c0:c0 + w], in0=rl[0][:, 0:w], scalar1=wt[s][:, 12:13], scalar2=sc2, op0=ALU.mult, op1=ALU.add),
                    reads=[("rl", 0), ("wt", s), "pfxc", "zero1"], writes=["Ib"])
                for h in range(1, 4):
                    S.add("dve", lambda e, h=h, c0=c0, w=w: e.scalar_tensor_tensor(
                        out=Ib[:, c0:c0 + w], in0=rl[h][:, 0:w], scalar=wt[s][:, 12 + h:13 + h], in1=Ib[:, c0:c0 + w],
                        op0=ALU.mult, op1=ALU.add),
                        reads=[("rl", h), ("wt", s), "Ib"], writes=["Ib"])
                pump()
            S.add("dve", lambda e: e.tensor_tensor(out=Ib[:, nk - 128:nk], in0=Ib[:, nk - 128:nk], in1=cmt, op=ALU.add),
                  reads=["Ib", "cmt"], writes=["Ib"])
            S.add("dve", lambda e: e.memset(mid, LO0 + BRW / 2), writes=["mid"])
            for it in range(NIT):
                step = BRW / (2 ** (it + 1))
                S.add("act", lambda e: e.activation(out=jk2[:, 0:na], in_=Ib[:, nd:nk], func=AF.Sign, scale=-1.0, bias=mid,
                                                    accum_out=sgn),
                      reads=["Ib", "mid"], writes=["jk2", "sgn"])
                S.add("dve", lambda e: e.tensor_scalar(out=jk[:, 0:nd], in0=Ib[:, 0:nd], scalar1=mid, scalar2=None,
                                                       op0=ALU.is_ge, op1=ALU.add, accum_out=cnt),
                      reads=["Ib", "mid"], writes=["jk", "cnt"])
                pump()
                S.add("dve", lambda e: e.scalar_tensor_tensor(out=tt_, in0=cnt, scalar=2.0, in1=sgn,
                                                              op0=ALU.mult, op1=ALU.subtract),
                      reads=["cnt", "sgn"], writes=["tt"])
                S.add("dve", lambda e: e.tensor_scalar(out=dl, in0=tt_, scalar1=float(512 - na), scalar2=-0.5,
                                                       op0=ALU.is_ge, op1=ALU.add),
                      reads=["tt"], writes=["dl"])
                if it + 1 < NIT:
                    S.add("dve", lambda e, step=step: e.scalar_tensor_tensor(out=mid, in0=dl, scalar=step, in1=mid,
                                                                             op0=ALU.mult, op1=ALU.add),
                          reads=["dl", "mid"], writes=["mid"])
                else:
                    S.add("dve", lambda e: e.tensor_scalar(out=dl, in0=dl, scalar1=-0.5, scalar2=step,
                                                           op0=ALU.add, op1=ALU.mult),
                          reads=["dl"], writes=["dl"])
                    S.add("dve", lambda e: e.tensor_tensor(out=lo, in0=mid, in1=dl, op=ALU.add), reads=["mid", "dl"], writes=["lo"])
            S.add("dve", lambda e: e.tensor_scalar(out=mb[s][:, 0:nk], in0=Ib[:, 0:nk], scalar1=lo, scalar2=-BIG,
                                                   op0=ALU.is_lt, op1=ALU.mult),
                  reads=["Ib", "lo"], writes=[("mb", s)])

        def stage_b_units(i):
            s = i % 2
            nkc = NT + i + 1
            pO, pD = pb[6], pb[7]
            okey, dkey = ("pb", 6), ("pb", 7)
            def qk(c):
                ps = 4 + c % 2
                Tt = None
                if c == nkc - 1:
                    r, Tt = 64, T0all
                elif c == nkc - 2:
                    r, Tt = 64, T1all
                else:
                    r = 65
                S.add("pe", lambda e, r=r, ps=ps: e.matmul(pb[ps], lhsT=kc[0:r, c * 128:(c + 1) * 128],
                                                           rhs=flat2(qc[s][0:r, :, :]), start=True, stop=False),
                      reads=["kc", ("qc", s)], writes=[("pb", ps)])
                S.add("pe", lambda e, ps=ps, Tt=Tt: e.matmul(pb[ps], lhsT=mb[s][:, c * 128:(c + 1) * 128], rhs=flat2(ident4),
                                                             start=False, stop=(Tt is None)),
                      reads=[("mb", s), "ident4"], writes=[("pb", ps)])
                if Tt is not None:
                    S.add("pe", lambda e, ps=ps, Tt=Tt: e.matmul(pb[ps], lhsT=ident, rhs=flat2(Tt[:, 12:16, :]),
                                                                 start=False, stop=True),
                          reads=["ident", "T0all", "T1all"], writes=[("pb", ps)])
                S.add("act", lambda e, ps=ps: e.activation(out=PT[c % 2], in_=pb[ps], func=AF.Exp),
                      reads=[("pb", ps)], writes=[("PT", c % 2)])

            def pv(c):
                S.add("pe", lambda e: e.matmul(pO[0:65, :], lhsT=vc[:, c, :], rhs=PT[c % 2], start=(c == 0), stop=(c == nkc - 1)),
                      reads=["vc", ("PT", c % 2)], writes=[okey])

            units = [lambda: qk(0)]

            def unit(c):
                if c + 1 < nkc:
                    qk(c + 1)
                pv(c)
            for c in range(nkc):
                units.append(lambda c=c: unit(c))
            units.append(lambda: finalize(pO, pD, okey, dkey, 4,
                                          catT_d[12:16, :, i * 128:(i + 1) * 128].rearrange("h d t -> d h t"),
                                          ("catT", i), tmp=(dn, oc, osb)))
            return units

        stage_a(0)
        for i in range(NT):
            units = stage_b_units(i)
            if i + 1 < NT:
                stage_a(i + 1, units)
            while units:
                units.pop(0)()
        S.barrier()
        A.release(m)

    def wout_phase(l):
        m = A.mark()
        wo = A.alloc("wo", 64, [16, D], BF16)
        xs = [A.alloc("xs%d" % s, 128, D, F32) for s in range(3)]
        ct = [A.alloc("ct%d" % s, 64, [16, 128], BF16) for s in range(2)]
        S.add("pool", lambda e: e.dma_start(out=wo[:, 0:8, :], in_=wo_d[l, 0:512, :].rearrange("(h d) m -> d h m", d=64)),
              writes=["wo"], dma=True)
        S.add("pool", lambda e: e.dma_start(out=wo[:, 8:16, :], in_=wo_d[l, 512:1024, :].rearrange("(h d) m -> d h m", d=64)),
              writes=["wo"], dma=True)
        for u in range(NT):
            sl = u % 3
            s = u % 2
            S.add("sp", lambda e, u=u, sl=sl: e.dma_start(out=xs[sl], in_=xres[u * 128:(u + 1) * 128, :]),
                  reads=[("xres", u)], writes=[("xs", sl)], dma=True)
            S.add("sp", lambda e, u=u, s=s: e.dma_start(out=ct[s], in_=catT_d[:, :, u * 128:(u + 1) * 128].rearrange("h d t -> d h t")),
                  reads=[("catT", u)], writes=[("ct", s)], dma=True)
            for half in range(2):
                py = (2 * u + half) % 4
                for h in range(16):
                    S.add("pe", lambda e, s=s, h=h, half=half, py=py: e.matmul(
                        pb[py], lhsT=ct[s][:, h, :], rhs=wo[:, h, half * 512:(half + 1) * 512], start=(h == 0), stop=(h == 15)),
                        reads=[("ct", s), "wo"], writes=[("pb", py)])
                S.add("dve", lambda e, sl=sl, half=half, py=py: e.tensor_tensor(
                    out=xs[sl][:, half * 512:(half + 1) * 512], in0=pb[py], in1=xs[sl][:, half * 512:(half + 1) * 512], op=ALU.add),
                    reads=[("pb", py), ("xs", sl)], writes=[("xs", sl)])
            S.add("sp", lambda e, u=u, sl=sl: e.dma_start(out=xres[u * 128:(u + 1) * 128, :], in_=xs[sl]),
                  reads=[("xs", sl)], writes=[("xres", u)], dma=True)
        S.barrier()
        A.release(m)

    def final_phase():
        m = A.mark()
        gb = A.alloc("gb", 128, D, F32)
        xs = [A.alloc("xs%d" % s, 128, D, F32) for s in range(3)]
        ob = [A.alloc("ob%d" % s, 128, D, F32) for s in range(2)]
        stat = A.alloc("stat", 128, [3, 4], F32)
        junk = A.alloc("junk", 128, D, BF16)
        S.add("sp", lambda e: e.dma_start(out=gb, in_=nf_d.to_broadcast([128, D])), writes=["gb"], dma=True)
        for u in range(NT):
            sl = u % 3
            S.add("sp", lambda e, u=u, sl=sl: e.dma_start(out=xs[sl], in_=xres[u * 128:(u + 1) * 128, :]),
                  reads=[("xres", u)], writes=[("xs", sl)], dma=True)
            norm_sub(xs[sl], ("xs", sl), gb, "gb", stat[:, sl, :], ("stat", sl), ob[u % 2], ("ob", u % 2), junk)
            S.add("sp", lambda e, u=u: e.dma_start(out=out_d[u * 128:(u + 1) * 128, :], in_=ob[u % 2]),
                  reads=[("ob", u % 2)], writes=[("out", u)], dma=True)
        S.barrier()
        A.release(m)

    def dump(name, src):
        S.add("sp", lambda e: e.dma_start(out=dbg[name], in_=src), writes=[("dbg", name)], dma=True)
        S.barrier()

    plist = [("phase0", phase0)]
    for l in range(L):
        plist.append(("ffn1_%d" % l, lambda l=l: ffn_phase(l, w1g_d, w1u_d, w1d_d, n1_d[l:l + 1, :], x_in if l == 0 else xres, xres,
                                                          pre=(None if l == 0 else ("w1g", "w1u", "w1d")))))
        plist.append(("proj_%d" % l, lambda l=l: proj_phase(l)))
        plist.append(("xchg_%d" % l, exchange))
        plist.append(("swa_%d" % l, lambda l=l: swa_phase(l)))
        plist.append(("moba_%d" % l, lambda l=l: moba_phase(l)))
        plist.append(("dsa_%d" % l, lambda l=l: dsa_phase(l)))
        plist.append(("wout_%d" % l, lambda l=l: wout_phase(l)))
        plist.append(("ffn2_%d" % l, lambda l=l: ffn_phase(l, w2g_d, w2u_d, w2d_d, n2_d[l:l + 1, :], xres, xres,
                                                          pre=("w2g", "w2u", "w2d"))))
    plist.append(("final", final_phase))
    for name, fn in plist:
        S.tag = name
        fn()
        if upto is not None and name == upto:
            break
    if debug:
        S.tag = "dump"
        dump("xres", xres)
        dump("catT", catT_d)
        dump("qaT", qaT_d)
    S.add("act", lambda e: e.activation(out=zero1, in_=zero1, func=AF.Copy), reads=["zero1"], writes=["zero1"])
    S.add("dve", lambda e: e.memset(zero1, 0.0), writes=["zero1"])
    S.add("pool", lambda e: e.memset(zero1, 0.0), writes=["zero1"])
    S.add("sp", lambda e: e.dma_start(out=pfxc, in_=pfx_d.to_broadcast([128, 2])), writes=["pfxc"], dma=True)
    S.emit(stack)
    stack.close()
    return nc, S


_CACHE = {}


def make_in_maps(inputs, SEQ, DEPTH):
    f = lambda a: np.ascontiguousarray(np.asarray(a, dtype=np.float32))
    x = f(inputs["x"])
    B = x.shape[0]
    TC = SEQ // 2
    oh, er, cm = host_consts(SEQ)
    shared = {
        "tab": f(inputs["rel_bias_table"]),
        "n1": f(inputs["ffn1_norm"]), "w1g": f(inputs["ffn1_w_gate"]), "w1u": f(inputs["ffn1_w_up"]),
        "w1d": f(inputs["ffn1_w_down"]), "nm": f(inputs["mix_norm"]), "win": f(inputs["w_in"]),
        "sinks": f(inputs["attn_sinks"]), "kvn": f(inputs["kv_norm_c"]), "wup": f(inputs["w_kv_up_c"]),
        "wo": f(inputs["w_out"]), "n2": f(inputs["ffn2_norm"]), "w2g": f(inputs["ffn2_w_gate"]),
        "w2u": f(inputs["ffn2_w_up"]), "w2d": f(inputs["ffn2_w_down"]),
        "nf": f(inputs["final_norm"]).reshape(1, -1),
        "oh": oh, "er": er, "cm": cm,
    }
    maps = []
    for c in range(2 * B):
        b, half = c // 2, c % 2
        mp = dict(shared)
        mp["x"] = np.ascontiguousarray(x[b, half * TC:(half + 1) * TC, :])
        mp["pfx"] = np.array([[-BIG if half == 0 else 0.0, 0.0]], np.float32)
        maps.append(mp)
    return maps


def kernel(**inputs):
    x = np.asarray(inputs["x"])
    B, SEQ, _ = x.shape
    DEPTH = np.asarray(inputs["ffn1_norm"]).shape[0]
    assert B == 4
    key = (SEQ, DEPTH)
    if key not in _CACHE:
        _CACHE[key] = build_program(SEQ, DEPTH)[0]
    nc = _CACHE[key]
    maps = make_in_maps(inputs, SEQ, DEPTH)
    res = run_bass_kernel_spmd(nc, maps, core_ids=list(range(8)))
    TC = SEQ // 2
    out = np.empty((B, SEQ, D), np.float32)
    for c in range(8):
        b, half = c // 2, c % 2
        out[b, half * TC:(half + 1) * TC, :] = np.asarray(res.results[c]["out"], dtype=np.float32)
    return out
```

```python
import math
import os
from contextlib import ExitStack

import numpy as np
import concourse.bass as bass
import concourse.mybir as mybir
from concourse.bass_utils import run_bass_kernel_spmd

F32 = mybir.dt.float32
BF16 = mybir.dt.bfloat16
U8 = mybir.dt.uint8
AF = mybir.ActivationFunctionType
ALU = mybir.AluOpType
AX = mybir.AxisListType

DT_SIZE = {F32: 4, BF16: 2, U8: 1}
ENGS = ["pe", "act", "dve", "pool", "sp"]
EPOCH = 20000
NSLOT = {"sp": 8, "act": 4, "pool": 8}

D = 1024
FF = 2816
NJ = FF // 128
DIN = 2244
BIG = 32768.0
EPS = 1e-6
NIT = 24
LO0 = -64.0 - 1.0 / 3.0
BRW = 128.0
ACT_FRAC = 0.45


class Op:
    __slots__ = ("eng", "fn", "dma", "deps", "signal", "sig", "slotprev", "cc", "tag")

    def __init__(self, eng, fn, dma):
        self.eng = eng
        self.fn = fn
        self.dma = dma
        self.deps = []
        self.signal = False
        self.sig = None
        self.slotprev = None
        self.cc = False


class Sched:
    def __init__(self, nc):
        self.nc = nc
        self.ops = {e: [] for e in ENGS}
        self.lastw = {}
        self.readers = {}
        self.pending = {e: [] for e in ENGS}
        self.dma_hist = {e: [] for e in ENGS}
        self.last_comp = {e: None for e in ENGS}
        self.tag = ""
        self.names = {}

    def add(self, eng, fn, reads=(), writes=(), dma=False, cc=False, bg=False):
        op = Op(eng, fn, dma)
        op.cc = cc
        op.tag = (self.tag, tuple(reads), tuple(writes))
        deps = []
        seen = set()

        def push(d):
            if d is None or id(d) in seen:
                return
            seen.add(id(d))
            if d.eng == "pe" and eng == "pe" and not d.dma and not dma:
                return
            deps.append(d)

        psum_reads = [k for k in reads if (k == "pb0" or (isinstance(k, tuple) and k[0] == "pb"))]
        if psum_reads:
            reads = [k for k in reads if k not in psum_reads]
            writes = list(writes) + psum_reads
        for k in reads:
            push(self.lastw.get(k))
        for k in writes:
            push(self.lastw.get(k))
            for r in self.readers.get(k, ()):
                push(r)
        for d in self.pending[eng]:
            push(d)
        self.pending[eng] = []
        op.deps = deps
        for d in deps:
            d.signal = True
        for k in writes:
            self.lastw[k] = op
            self.readers[k] = []
        for k in reads:
            lst = self.readers.setdefault(k, [])
            if not dma:
                lst[:] = [r for r in lst if not (r.eng == eng and not r.dma)]
            lst.append(op)
        self.ops[eng].append(op)
        if bg:
            pass
        elif dma or cc:
            h = self.dma_hist[eng]
            h.append(op)
            if len(h) > NSLOT[eng] + 2:
                h.pop(0)
        else:
            self.last_comp[eng] = op
        return op

    def barrier(self):
        tails = []
        for e in ENGS:
            if self.last_comp[e] is not None:
                tails.append(self.last_comp[e])
            tails.extend(self.dma_hist[e])
        for e in ENGS:
            self.pending[e] = list(tails)

    def emit(self, stack):
        nc = self.nc
        names = set()
        for e in ENGS:
            cnt = 0
            ndma = 0
            slot_last = {}
            ncc = 0
            for op in self.ops[e]:
                if op.dma:
                    R = NSLOT[e]
                    slot = ndma % R
                    val = 16 * (ndma // R + 1)
                    op.sig = ("d_%s_%d" % (e, slot), val, 16)
                    op.slotprev = slot_last.get(slot)
                    slot_last[slot] = op
                    ndma += 1
                elif op.cc:
                    ncc += 1
                    op.sig = ("cc_%s" % e, ncc, 1)
                elif op.signal:
                    ep = cnt // EPOCH
                    op.sig = ("c_%s_%d" % (e, ep), cnt - ep * EPOCH + 1, 1)
                    cnt += 1
                if op.sig is not None:
                    names.add(op.sig[0])
        sems = {}
        for name in sorted(names):
            sems[name] = stack.enter_context(nc.semaphore(name))
        block = stack.enter_context(nc.Block())
        stats = {"wait": 0, "ins": 0}

        def run(e):
            def body(engine):
                seenv = {}
                for op in self.ops[e]:
                    dl = list(op.deps)
                    if op.slotprev is not None:
                        dl.append(op.slotprev)
                    for d in dl:
                        name, val, _ = d.sig
                        if seenv.get(name, 0) >= val:
                            continue
                        engine.wait_ge(sems[name], val)
                        stats["wait"] += 1
                        seenv[name] = val
                    ins = op.fn(engine)
                    try:
                        self.names[str(ins.ins.name)] = op.tag
                    except Exception:
                        pass
                    stats["ins"] += 1
                    if op.sig is not None:
                        name, val, inc = op.sig
                        ins.then_inc(sems[name], inc)
            return body

        block.tensor(run("pe"))
        block.scalar(run("act"))
        block.vector(run("dve"))
        block.gpsimd(run("pool"))
        block.sync(run("sp"))
        self.stats = stats


class Arena:
    def __init__(self, nc, nbytes):
        self.h = nc.alloc_sbuf_tensor("arena", [128, nbytes], U8)
        self.ap = self.h.ap()
        self.nbytes = nbytes
        self.off = 0

    def alloc(self, name, parts, free, dt):
        if isinstance(free, int):
            free = [free]
        n = 1
        for f in free:
            n *= f
        nb = n * DT_SIZE[dt]
        off = (self.off + 63) // 64 * 64
        assert off + nb <= self.nbytes, "SBUF arena overflow %s: %d + %d > %d" % (name, off, nb, self.nbytes)
        self.off = off + nb
        ap = self.ap[0:parts, off:off + nb].bitcast(dt)
        if len(free) == 2:
            ap = ap.rearrange("p (a b) -> p a b", b=free[1])
        elif len(free) == 3:
            ap = ap.rearrange("p (a b c) -> p a b c", b=free[1], c=free[2])
        return ap

    def mark(self):
        return self.off

    def release(self, m):
        self.off = m


def bc_mid(a, n):
    return bass.AP(a.tensor, a.offset, [list(a.ap[0]), [0, n], list(a.ap[1])])


def bc_last(a, n):
    return bass.AP(a.tensor, a.offset, [list(a.ap[0]), list(a.ap[1]), [0, n]])


def bc_cols(a, n):
    return bass.AP(a.tensor, a.offset, [list(a.ap[0]), [0, n]])


def flat2(a):
    if len(a.shape) == 3:
        return a.rearrange("p a b -> p (a b)")
    return a


def _t5_bucket_np(n):
    n = np.maximum(n, 0)
    nf = np.maximum(n, 1).astype(np.float32)
    large = 16 + (np.log(nf / np.float32(16)) / np.float32(math.log(128 / 16)) * np.float32(16)).astype(np.int32)
    large = np.minimum(large, 31)
    return np.where(n < 16, n, large)


def host_consts(SEQ):
    oh = np.zeros((2, 33, 384), np.float32)
    for m in range(384):
        d = m - 128
        b = int(_t5_bucket_np(np.array([max(d, 0)]))[0])
        oh[0, 32 if d < 0 else b, m] = 1.0
        oh[1, 32 if (d < 0 or d >= 128) else b, m] = 1.0
    er = np.zeros((33, SEQ), np.float32)
    for n in range(min(32, SEQ // 256)):
        er[n, n * 256:(n + 1) * 256] = BIG
    er[32, :] = 1.0
    q = np.arange(128)[:, None]
    j = np.arange(128)[None, :]
    cm = np.where(j <= q, 0.0, -BIG).astype(np.float32)
    return oh, er, cm


def build_program(SEQ, DEPTH, debug=False, upto=None):
    TC = SEQ // 2
    NT = TC // 128
    NTT = TC // 512
    T2 = SEQ
    NC2 = T2 // 128
    NB = SEQ // 256
    NBH = NB // 2
    GW = max(NB, 8)

    nc = bass.Bass("TRN2", target_bir_lowering=False)
    L = DEPTH

    def din(name, shape, dt=F32):
        return nc.dram_tensor(name, list(shape), dt, kind="ExternalInput").ap()

    def dscr(name, shape, dt=BF16):
        return nc.dram_tensor(name, list(shape), dt, kind="Internal").ap()

    x_in = din("x", [TC, D])
    tab_d = din("tab", [32, 16])
    n1_d = din("n1", [L, D])
    w1g_d = din("w1g", [L, D, FF])
    w1u_d = din("w1u", [L, D, FF])
    w1d_d = din("w1d", [L, FF, D])
    nm_d = din("nm", [L, D])
    win_d = din("win", [L, D, DIN])
    sink_d = din("sinks", [L, 8])
    kvn_d = din("kvn", [L, 128])
    wup_d = din("wup", [L, 128, 128])
    wo_d = din("wo", [L, D, D])
    n2_d = din("n2", [L, D])
    w2g_d = din("w2g", [L, D, FF])
    w2u_d = din("w2u", [L, D, FF])
    w2d_d = din("w2d", [L, FF, D])
    nf_d = din("nf", [1, D])
    oh_d = din("oh", [2, 33, 384])
    er_d = din("er", [33, T2])
    cm_d = din("cm", [128, 128])
    pfx_d = din("pfx", [1, 2])
    out_d = nc.dram_tensor("out", [TC, D], F32, kind="ExternalOutput").ap()

    xres = dscr("xres", [TC, D], F32)
    qaT_d = dscr("qaT", [8, 64, TC])
    qbT_d = dscr("qbT", [4, 64, TC])
    qcT_d = dscr("qcT", [4, 64, TC])
    qiT_d = dscr("qiT", [4, 64, TC])
    wi_d = dscr("wi", [TC, 16], F32)
    KT_t = [dscr("KT%d" % t, [512, 512]) for t in range(NTT)]
    VT_t = [dscr("VT%d" % t, [512, 448]) for t in range(NTT)]
    gKT_t = [dscr("gKT%d" % t, [1024, 512]) for t in range(NTT)]
    gVT_t = [dscr("gVT%d" % t, [1024, 448]) for t in range(NTT)]
    catT_d = dscr("catT", [16, 64, TC])
    Zf_d = dscr("Zf", [16, 128, 384], F32)
    ZA_d = dscr("ZA", [8, 128, 384], F32)
    wbf = {}
    for l in range(L):
        for nm_, shp in (("w1g", [D, FF]), ("w1u", [D, FF]), ("w1d", [FF, D]), ("w2g", [D, FF]), ("w2u", [D, FF]),
                         ("w2d", [FF, D]), ("win", [D, DIN])):
            if l == 0 and nm_ in ("w1g", "w1u", "w1d"):
                continue
            wbf[(l, nm_)] = dscr("wbf_%s_%d" % (nm_, l), shp)
    dbg = {}
    if debug:
        dbg["xres"] = nc.dram_tensor("dbg_xres", [TC, D], F32, kind="ExternalOutput").ap()
        dbg["catT"] = nc.dram_tensor("dbg_catT", [16, 64, TC], BF16, kind="ExternalOutput").ap()
        dbg["qaT"] = nc.dram_tensor("dbg_qaT", [8, 64, TC], BF16, kind="ExternalOutput").ap()

    stack = ExitStack()
    S = Sched(nc)
    A = Arena(nc, 210000)
    pb = [nc.alloc_psum_tensor("pb%d" % i, [128, 512], F32).ap() for i in range(8)]
    pbh = [p.bitcast(BF16) for p in pb]

    ident = A.alloc("ident", 128, 128, BF16)
    ident4 = A.alloc("ident4", 128, [4, 128], BF16)
    ones64 = A.alloc("ones64", 128, 64, BF16)
    T0all = A.alloc("T0all", 128, [16, 128], BF16)
    T1all = A.alloc("T1all", 128, [16, 128], BF16)
    cmt = A.alloc("cmt", 128, 128, F32)
    pfxc = A.alloc("pfxc", 128, 2, F32)
    pfxrow = A.alloc("pfxrow", 128, GW, F32)
    stepc = A.alloc("stepc", 128, 2 * GW, F32)
    zero1 = A.alloc("zero1", 128, 1, F32)
    selT = A.alloc("selT", 65, 64, F32)
    tab31 = A.alloc("tab31", 128, 16, F32)
    b31s = A.alloc("b31s", 1, [8, 128], BF16)

    def phase0():
        m = A.mark()
        identf = A.alloc("identf", 128, 128, F32)
        tabf = A.alloc("tabf", 33, 16, F32)
        ohb = A.alloc("ohb", 33, [2, 384], BF16)
        lh = [A.alloc("lh%d" % i, 33, 128, BF16) for i in range(2)]
        zsb = [A.alloc("zsb%d" % i, 128, 384, F32) for i in range(2)]
        S.add("pool", lambda e: e.memset(identf, 0.0), writes=["identf"])
        S.add("pool", lambda e: e.affine_select(out=identf, in_=identf, pattern=[[-1, 128]],
                                                compare_op=ALU.not_equal, fill=1.0, base=0,
                                                channel_multiplier=1),
              reads=["identf"], writes=["identf"])
        S.add("dve", lambda e: e.tensor_copy(out=ident, in_=identf), reads=["identf"], writes=["ident"])
        S.add("dve", lambda e: e.tensor_copy(out=ident4, in_=bc_mid(identf, 4)), reads=["identf"], writes=["ident4"])
        S.add("dve", lambda e: e.memset(ones64, 1.0), writes=["ones64"])
        S.add("dve", lambda e: e.memset(selT[0:64, :], 0.0), writes=["selT"])
        S.add("dve", lambda e: e.memset(selT[64:65, :], 1.0), writes=["selT"])
        S.add("dve", lambda e: e.memset(zero1, 0.0), writes=["zero1"])
        S.add("sp", lambda e: e.dma_start(out=cmt, in_=cm_d), writes=["cmt"], dma=True)
        S.add("sp", lambda e: e.dma_start(out=pfxc, in_=pfx_d.to_broadcast([128, 2])), writes=["pfxc"], dma=True)
        S.add("sp", lambda e: e.dma_start(out=tab31, in_=tab_d[31:32, :].to_broadcast([128, 16])), writes=["tab31"], dma=True)
        for j in range(8):
            S.add("dve", lambda e, j=j: e.tensor_copy(out=b31s[0:1, j, :], in_=bc_cols(tab31[0:1, 8 + j:9 + j], 128)),
                  reads=["tab31"], writes=["b31s"])
        S.add("dve", lambda e: e.memset(pfxrow, 0.0), writes=["pfxrow"])
        S.add("dve", lambda e: e.tensor_scalar(out=pfxrow[:, 0:NBH], in0=pfxrow[:, 0:NBH], scalar1=pfxc[:, 0:1],
                                               scalar2=None, op0=ALU.add),
              reads=["pfxrow", "pfxc"], writes=["pfxrow"])
        S.add("dve", lambda e: e.memset(stepc[:, 0:GW], 0.0), writes=["stepc"])
        S.add("dve", lambda e: e.memset(stepc[:, GW:2 * GW], -BIG), writes=["stepc"])
        S.add("sp", lambda e: e.dma_start(out=tabf[0:32, :], in_=tab_d), writes=["tabf"], dma=True)
        S.add("dve", lambda e: e.memset(tabf[32:33, :], -BIG), writes=["tabf"])
        S.add("pool", lambda e: e.dma_start(out=ohb, in_=oh_d.rearrange("w r m -> r w m")), writes=["ohb"], dma=True)
        n = 0
        for h in range(16):
            for which in range(2):
                if which == 1 and h >= 8:
                    continue
                s = n % 2
                n += 1
                S.add("dve", lambda e, s=s, h=h: e.tensor_copy(out=lh[s], in_=bc_cols(tabf[0:33, h:h + 1], 128)),
                      reads=["tabf"], writes=[("lh", s)])
                S.add("pe", lambda e, s=s, which=which: e.matmul(pb[s][:, 0:384], lhsT=lh[s], rhs=ohb[:, which, :],
                                                                  start=True, stop=True),
                      reads=[("lh", s), "ohb"], writes=[("pb", s)])
                S.add("act", lambda e, s=s: e.activation(out=zsb[s], in_=pb[s][:, 0:384], func=AF.Copy),
                      reads=[("pb", s)], writes=[("zsb", s)])
                dst = (ZA_d if which == 1 else Zf_d)[h]
                zkey = ("Z", which, h)
                S.add("sp", lambda e, s=s, dst=dst: e.dma_start(out=dst, in_=zsb[s]),
                      reads=[("zsb", s)], writes=[zkey], dma=True)
        for h in range(16):
            zt = Zf_d.tensor
            base = h * 128 * 384
            S.add("pool", lambda e, h=h, base=base: e.dma_start(
                out=T0all[:, h, :], in_=bass.AP(Zf_d.tensor, base + 128, [[383, 128], [1, 128]])),
                reads=[("Z", 0, h)], writes=["T0all"], dma=True)
            if h < 8:
                S.add("pool", lambda e, h=h, base=base: e.dma_start(
                    out=T1all[:, h, :], in_=bass.AP(ZA_d.tensor, base + 256, [[383, 128], [1, 128]])),
                    reads=[("Z", 1, h)], writes=["T1all"], dma=True)
            else:
                S.add("pool", lambda e, h=h, base=base: e.dma_start(
                    out=T1all[:, h, :], in_=bass.AP(Zf_d.tensor, base + 256, [[383, 128], [1, 128]])),
                    reads=[("Z", 0, h)], writes=["T1all"], dma=True)
        S.barrier()
        A.release(m)

    def norm_sub(xs_ap, xkey, gtile, gkey, stat, skey, hb_ap, hkey, junk, width=D):
        if junk is None:
            jout, jkey = hb_ap, hkey
        else:
            jout, jkey = junk[:, 0:width], "junk"
        S.add("act", lambda e: e.activation(out=jout, in_=xs_ap, func=AF.Square, accum_out=stat[:, 0:1]),
              reads=[xkey], writes=[jkey, skey])
        S.add("act", lambda e: e.activation(out=stat[:, 1:2], in_=stat[:, 0:1], func=AF.Sqrt, scale=1.0 / width, bias=EPS),
              reads=[skey], writes=[skey])
        S.add("dve", lambda e: e.reciprocal(out=stat[:, 2:3], in_=stat[:, 1:2]), reads=[skey], writes=[skey])
        S.add("dve", lambda e: e.scalar_tensor_tensor(out=hb_ap, in0=xs_ap, scalar=stat[:, 2:3], in1=gtile,
                                                      op0=ALU.mult, op1=ALU.mult),
              reads=[xkey, skey, gkey], writes=[hkey])

    def precast(order):
        srcs = {"w1g": w1g_d, "w1u": w1u_d, "w1d": w1d_d, "w2g": w2g_d, "w2u": w2u_d, "w2d": w2d_d, "win": win_d}
        for (l, nm_) in order:
            dstt = wbf[(l, nm_)]
            rows = dstt.shape[0]
            nparts = 4
            step = rows // nparts
            for p in range(nparts):
                r0, r1 = p * step, (rows if p == nparts - 1 else (p + 1) * step)
                S.add("pool", lambda e, l=l, nm_=nm_, r0=r0, r1=r1, dstt=dstt: e.dma_start(out=dstt[r0:r1, :], in_=srcs[nm_][l, r0:r1, :]),
                      writes=[("wbf", l, nm_, p)], dma=True, bg=True)

    def ffn_phase(l, wg_d, wu_d, wd_d, norm_row, src, dst, pre=None):
        m = A.mark()
        wg = A.alloc("wg", 128, [8, FF], BF16)
        wu = A.alloc("wu", 128, [8, FF], BF16)
        wd = A.alloc("wd", 128, [NJ, D], BF16)
        gb = A.alloc("gb", 128, D, F32)
        NXS = 5
        xs = [A.alloc("xs%d" % s, 128, D, F32) for s in range(NXS)]
        hb1 = A.alloc("hb0", 128, D, BF16)
        hb = [hb1, hb1]
        hT = A.alloc("hT", 128, [8, 512], BF16)
        actT = A.alloc("actT", 128, [NJ, 512], BF16)
        sg = [A.alloc("sg%d" % s, 128, 512, F32) for s in range(2)]
        stat = A.alloc("stat", 128, [NXS, 4], F32)
        junk = None
        S.add("sp", lambda e: e.dma_start(out=gb, in_=norm_row.to_broadcast([128, D])), writes=["gb"], dma=True)
        if pre is None:
            for k in range(8):
                S.add("pool", lambda e, k=k: e.dma_start(out=wg[:, k, :], in_=wg_d[l, k * 128:(k + 1) * 128, :]),
                      writes=[("wg", k)], dma=True)
                S.add("pool", lambda e, k=k: e.dma_start(out=wu[:, k, :], in_=wu_d[l, k * 128:(k + 1) * 128, :]),
                      writes=[("wu", k)], dma=True)
            for j in range(NJ):
                S.add("pool", lambda e, j=j: e.dma_start(out=wd[:, j, :], in_=wd_d[l, j * 128:(j + 1) * 128, :]),
                      writes=[("wd", j)], dma=True)
            precast([(0, "win"), (0, "w2g"), (0, "w2u"), (0, "w2d")])
        else:
            gname, uname, dname = pre
            gb_, ub_, db_ = wbf[(l, gname)], wbf[(l, uname)], wbf[(l, dname)]
            allk = lambda nm_: [("wbf", l, nm_, p) for p in range(4)]
            nq = 0
            for k in range(8):
                S.add("sp" if nq % 2 == 0 else "act", lambda e, k=k: e.dma_start(out=wg[:, k, :], in_=gb_[k * 128:(k + 1) * 128, :]),
                      reads=allk(gname), writes=[("wg", k)], dma=True)
                nq += 1
                S.add("sp" if nq % 2 == 0 else "act", lambda e, k=k: e.dma_start(out=wu[:, k, :], in_=ub_[k * 128:(k + 1) * 128, :]),
                      reads=allk(uname), writes=[("wu", k)], dma=True)
                nq += 1
            for j in range(NJ):
                S.add("sp" if nq % 2 == 0 else "act", lambda e, j=j: e.dma_start(out=wd[:, j, :], in_=db_[j * 128:(j + 1) * 128, :]),
                      reads=allk(dname), writes=[("wd", j)], dma=True)
                nq += 1
        for tt in range(NTT):
            for sub in range(4):
                u = tt * 4 + sub
                sl = u % NXS
                S.add("sp", lambda e, u=u, sl=sl: e.dma_start(out=xs[sl], in_=src[u * 128:(u + 1) * 128, :]),
                      reads=[("xres", u)], writes=[("xs", sl)], dma=True)
                norm_sub(xs[sl], ("xs", sl), gb, "gb", stat[:, sl, :], ("stat", sl), hb[0], ("hb", 0), junk)
                for k in range(8):
                    S.add("pe", lambda e, u=u, k=k: e.transpose(out=pbh[0][:, k * 128:(k + 1) * 128],
                                                                in_=hb[0][:, k * 128:(k + 1) * 128], identity=ident),
                          reads=[("hb", 0), "ident"], writes=["pb0"])
                S.add("act", lambda e, sub=sub: e.activation(
                    out=hT[:, :, sub * 128:(sub + 1) * 128],
                    in_=pbh[0].rearrange("p (k t) -> p k t", t=128), func=AF.Copy),
                    reads=["pb0"], writes=[("hT", sub)])
            hkeys = [("hT", s) for s in range(4)]
            for j in range(NJ):
                pg = 1 + j % 2
                pu = 3 + j % 2
                for k in range(8):
                    S.add("pe", lambda e, j=j, k=k, pg=pg: e.matmul(pb[pg], lhsT=wg[:, k, j * 128:(j + 1) * 128],
                                                                    rhs=hT[:, k, :], start=(k == 0), stop=(k == 7)),
                          reads=[("wg", k)] + hkeys, writes=[("pb", pg)])
                for k in range(8):
                    S.add("pe", lambda e, j=j, k=k, pu=pu: e.matmul(pb[pu], lhsT=wu[:, k, j * 128:(j + 1) * 128],
                                                                    rhs=hT[:, k, :], start=(k == 0), stop=(k == 7)),
                          reads=[("wu", k)] + hkeys, writes=[("pb", pu)])
                S.add("act", lambda e, j=j, pg=pg: e.activation(out=sg[j % 2], in_=pb[pg], func=AF.Silu),
                      reads=[("pb", pg)], writes=[("sg", j % 2)])
                S.add("dve", lambda e, j=j, pu=pu: e.tensor_tensor(out=actT[:, j, :], in0=sg[j % 2], in1=pb[pu], op=ALU.mult),
                      reads=[("sg", j % 2), ("pb", pu)], writes=[("actT", j)])
            n = 0
            for sub in range(4):
                u = tt * 4 + sub
                sl = u % NXS
                for half in range(2):
                    py = 5 + n % 2
                    n += 1
                    for j in range(NJ):
                        S.add("pe", lambda e, j=j, sub=sub, half=half, py=py: e.matmul(
                            pb[py], lhsT=actT[:, j, sub * 128:(sub + 1) * 128],
                            rhs=wd[:, j, half * 512:(half + 1) * 512], start=(j == 0), stop=(j == NJ - 1)),
                            reads=[("actT", j), ("wd", j)], writes=[("pb", py)])
                    S.add("dve", lambda e, sl=sl, half=half, py=py: e.scalar_tensor_tensor(
                        out=xs[sl][:, half * 512:(half + 1) * 512], in0=pb[py], scalar=0.5,
                        in1=xs[sl][:, half * 512:(half + 1) * 512], op0=ALU.mult, op1=ALU.add),
                        reads=[("pb", py), ("xs", sl)], writes=[("xs", sl)])
                S.add("sp", lambda e, u=u, sl=sl: e.dma_start(out=dst[u * 128:(u + 1) * 128, :], in_=xs[sl]),
                      reads=[("xs", sl)], writes=[("xres", u)], dma=True)
        S.barrier()
        A.release(m)

    FM_COLS = [(0, 512), (512, 640), (768, 1024), (1024, 1280), (1536, 1792), (1920, 2176), (2176, 2240)]
    QSCALE = [True] * 8 + [False] * 2 + [True] * 4 + [False] * 4 + [True] * 4 + [True] * 4 + [False]

    def proj_phase(l):
        m = A.mark()
        wfm = A.alloc("wfm", 128, [8, 1728], BF16)
        wtm = A.alloc("wtm", 128, [8, 576], BF16)
        wupb = A.alloc("wupb", 128, 128, BF16)
        gb = A.alloc("gb", 128, D, F32)
        gkv = A.alloc("gkv", 128, 128, F32)
        xs = [A.alloc("xs%d" % s, 128, D, F32) for s in range(2)]
        hb = [A.alloc("hb%d" % s, 128, D, BF16) for s in range(2)]
        hT = A.alloc("hT", 128, [8, 512], BF16)
        hd = A.alloc("hd", 64, [28, 512], BF16)
        vtm = A.alloc("vtm", 128, [4, 448], BF16)
        ck = A.alloc("ck", 128, 128, F32)
        ckn = A.alloc("ckn", 128, 128, BF16)
        cknT = A.alloc("cknT", 128, 512, BF16)
        wis = A.alloc("wis", 128, [4, 16], F32)
        stat = A.alloc("stat", 128, [2, 4], F32)
        stat2 = A.alloc("stat2", 128, 4, F32)
        junk = A.alloc("junk", 128, D, BF16)
        S.add("sp", lambda e: e.dma_start(out=gb, in_=nm_d[l:l + 1, :].to_broadcast([128, D])), writes=["gb"], dma=True)
        S.add("sp", lambda e: e.dma_start(out=gkv, in_=kvn_d[l:l + 1, :].to_broadcast([128, 128])), writes=["gkv"], dma=True)
        S.add("pool", lambda e: e.dma_start(out=wupb, in_=wup_d[l]), writes=["wupb"], dma=True)
        winb = wbf[(l, "win")]
        wkeys = [("wbf", l, "win", p) for p in range(4)]
        nq = 0
        for k in range(8):
            c = 0
            for (a, b) in FM_COLS:
                S.add("sp" if nq % 2 == 0 else "act", lambda e, k=k, a=a, b=b, c=c: e.dma_start(
                    out=wfm[:, k, c:c + (b - a)], in_=winb[k * 128:(k + 1) * 128, a:b]),
                    reads=wkeys, writes=[("wfm", k)], dma=True)
                nq += 1
                c += b - a
            for (a, b, c) in [(640, 768, 0), (1280, 1536, 128), (1792, 1920, 384), (2180, 2244, 512)]:
                S.add("sp" if nq % 2 == 0 else "act", lambda e, k=k, a=a, b=b, c=c: e.dma_start(
                    out=wtm[:, k, c:c + (b - a)], in_=winb[k * 128:(k + 1) * 128, a:b]),
                    reads=wkeys, writes=[("wtm", k)], dma=True)
                nq += 1
        nev = 0
        PSTOP = int(os.environ.get("PROJ_STOP", "99"))
        for tt in range(NTT if PSTOP > 1 else 0):
            for sub in range(4):
                u = tt * 4 + sub
                sl = u % 2
                S.add("sp", lambda e, u=u, sl=sl: e.dma_start(out=xs[sl], in_=xres[u * 128:(u + 1) * 128, :]),
                      reads=[("xres", u)], writes=[("xs", sl)], dma=True)
                norm_sub(xs[sl], ("xs", sl), gb, "gb", stat[:, sl, :], ("stat", sl), hb[sl], ("hb", sl), junk)
                for k in range(8):
                    S.add("pe", lambda e, sl=sl, k=k: e.transpose(out=pbh[0][:, k * 128:(k + 1) * 128],
                                                                  in_=hb[sl][:, k * 128:(k + 1) * 128], identity=ident),
                          reads=[("hb", sl), "ident"], writes=["pb0"])
                S.add("act", lambda e, sub=sub: e.activation(
                    out=hT[:, :, sub * 128:(sub + 1) * 128],
                    in_=pbh[0].rearrange("p (k t) -> p k t", t=128), func=AF.Copy),
                    reads=["pb0"], writes=[("hT", sub)])
            hkeys = [("hT", s) for s in range(4)]
            for sub in range(4):
                u = tt * 4 + sub
                for k in range(8):
                    S.add("pe", lambda e, sub=sub, k=k: e.matmul(pb[4], lhsT=hT[:, k, sub * 128:(sub + 1) * 128],
                                                                 rhs=wtm[:, k, 0:512], start=(k == 0), stop=(k == 7)),
                          reads=[("wtm", k), ("hT", sub)], writes=[("pb", 4)])
                for k in range(8):
                    S.add("pe", lambda e, sub=sub, k=k: e.matmul(pb[5][:, 0:64], lhsT=hT[:, k, sub * 128:(sub + 1) * 128],
                                                                 rhs=wtm[:, k, 512:576], start=(k == 0), stop=(k == 7)),
                          reads=[("wtm", k), ("hT", sub)], writes=[("pb", 5)])
                S.add("act", lambda e, sub=sub: e.activation(out=vtm[:, sub, 0:384], in_=pb[4][:, 0:384], func=AF.Copy),
                      reads=[("pb", 4)], writes=[("vtm", sub)])
                S.add("act", lambda e: e.activation(out=ck, in_=pb[4][:, 384:512], func=AF.Copy), reads=[("pb", 4)], writes=["ck"])
                S.add("dve", lambda e, sub=sub: e.tensor_scalar(out=wis[:, sub, :], in0=pb[5][:, 48:64], scalar1=0.5,
                                                                scalar2=None, op0=ALU.mult),
                      reads=[("pb", 5)], writes=["wis"])
                norm_sub(ck, "ck", gkv, "gkv", stat2, "stat2", ckn, "ckn", junk, width=128)
                S.add("pe", lambda e: e.transpose(out=pbh[6][:, 0:128], in_=ckn, identity=ident),
                      reads=["ckn", "ident"], writes=[("pb", 6)])
                S.add("act", lambda e, sub=sub: e.activation(out=cknT[:, sub * 128:(sub + 1) * 128], in_=pbh[6][:, 0:128],
                                                             func=AF.Copy),
                      reads=[("pb", 6)], writes=[("cknT", sub)])
                S.add("pe", lambda e, sub=sub: e.matmul(pb[7][:, 0:64], lhsT=cknT[:, sub * 128:(sub + 1) * 128],
                                                        rhs=wupb[:, 64:128], start=True, stop=True),
                      reads=[("cknT", sub), "wupb"], writes=[("pb", 7)])
                S.add("dve", lambda e, sub=sub: e.tensor_copy(out=vtm[:, sub, 384:448], in_=pb[7][:, 0:64]),
                      reads=[("pb", 7)], writes=[("vtm", sub)])
            for f in range(27 if PSTOP > 2 else 0):
                pf = 1 + f % 3
                for k in range(8):
                    S.add("pe", lambda e, f=f, k=k, pf=pf: e.matmul(pb[pf][0:64, :], lhsT=wfm[:, k, f * 64:(f + 1) * 64],
                                                                    rhs=hT[:, k, :], start=(k == 0), stop=(k == 7)),
                          reads=[("wfm", k)] + hkeys, writes=[("pb", pf)])
                sc = 0.125 if QSCALE[f] else 1.0
                if nev % 2 == 0:
                    S.add("act", lambda e, f=f, pf=pf, sc=sc: e.activation(out=hd[:, f, :], in_=pb[pf][0:64, :],
                                                                           func=AF.Copy, scale=sc),
                          reads=[("pb", pf)], writes=[("hd", f)])
                else:
                    S.add("dve", lambda e, f=f, pf=pf, sc=sc: e.tensor_scalar(out=hd[:, f, :], in0=pb[pf][0:64, :],
                                                                              scalar1=sc, scalar2=None, op0=ALU.mult),
                          reads=[("pb", pf)], writes=[("hd", f)])
                nev += 1
            S.add("pe", lambda e: e.matmul(pb[1][0:64, :], lhsT=wupb[:, 0:64], rhs=cknT, start=True, stop=True),
                  reads=[("cknT", s) for s in range(4)] + ["wupb"], writes=[("pb", 1)])
            S.add("act", lambda e: e.activation(out=hd[:, 27, :], in_=pb[1][0:64, :], func=AF.Copy),
                  reads=[("pb", 1)], writes=[("hd", 27)])
            t0 = tt * 512
            if PSTOP <= 4:
                continue

            def fm_out(dst3, f0, nf, key):
                S.add("sp", lambda e: e.dma_start(out=dst3, in_=hd[:, f0:f0 + nf, :]),
                      reads=[("hd", f) for f in range(f0, f0 + nf)], writes=[key], dma=True)
            fm_out(qaT_d[:, :, t0:t0 + 512].rearrange("h d t -> d h t"), 0, 8, ("qaT", tt))
            fm_out(KT_t[tt][0:128, :].rearrange("(h d) t -> d h t", d=64), 8, 2, ("KTa", tt))
            fm_out(qbT_d[:, :, t0:t0 + 512].rearrange("h d t -> d h t"), 10, 4, ("qbT", tt))
            fm_out(KT_t[tt][128:384, :].rearrange("(h d) t -> d h t", d=64), 14, 4, ("KTb", tt))
            fm_out(qcT_d[:, :, t0:t0 + 512].rearrange("h d t -> d h t"), 18, 4, ("qcT", tt))
            fm_out(qiT_d[:, :, t0:t0 + 512].rearrange("h d t -> d h t"), 22, 4, ("qiT", tt))
            fm_out(KT_t[tt][448:512, :].rearrange("(h d) t -> d h t", d=64), 26, 1, ("KTi", tt))
            fm_out(KT_t[tt][384:448, :].rearrange("(h d) t -> d h t", d=64), 27, 1, ("KTc", tt))
            S.add("sp", lambda e, tt=tt: e.dma_start(out=VT_t[tt].rearrange("(s p) c -> p s c", p=128), in_=vtm),
                  reads=[("vtm", s) for s in range(4)], writes=[("VT", tt)], dma=True)
            S.add("sp", lambda e, t0=t0: e.dma_start(out=wi_d[t0:t0 + 512, :].rearrange("(s p) c -> p s c", p=128), in_=wis),
                  reads=["wis"], writes=[("wi", tt)], dma=True)
        S.barrier()
        A.release(m)

    def exchange():
        if os.environ.get("SKIP_XCHG"):
            return
        groups = [[2 * g, 2 * g + 1] for g in range(4)]
        for t in range(NTT):
            S.add("pool", lambda e, t=t: e.collective_compute("AllGather", ALU.bypass, replica_groups=groups,
                                                              ins=[KT_t[t]], outs=[gKT_t[t]]),
                  reads=["ccorder"], writes=[("gKT", t), "ccorder"], cc=True)
            S.add("pool", lambda e, t=t: e.collective_compute("AllGather", ALU.bypass, replica_groups=groups,
                                                              ins=[VT_t[t]], outs=[gVT_t[t]]),
                  reads=["ccorder"], writes=[("gVT", t), "ccorder"], cc=True)
        S.barrier()

    def finalize(pO, pD, okey, dkey, nh, dst3, dkey_out, extra=None, tmp=None):
        dn, oc, osb = tmp
        W = nh * 128
        S.add("act", lambda e: e.activation(out=osb[:, 0:W], in_=pO[0:65, 0:W], func=AF.Copy), reads=[okey], writes=["osb"])
        S.add("pe", lambda e: e.matmul(pD[0:64, 0:W], lhsT=selT, rhs=osb[:, 0:W], start=True, stop=True),
              reads=["selT", "osb"], writes=[dkey])
        if extra is not None:
            S.add("dve", lambda e: e.tensor_tensor(out=dn[:, 0:W], in0=pD[0:64, 0:W], in1=extra, op=ALU.add),
                  reads=[dkey, "exps"], writes=["dn"])
            S.add("dve", lambda e: e.reciprocal(out=dn[:, 0:W], in_=dn[:, 0:W]), reads=["dn"], writes=["dn"])
        else:
            S.add("dve", lambda e: e.reciprocal(out=dn[:, 0:W], in_=pD[0:64, 0:W]), reads=[dkey], writes=["dn"])
        S.add("dve", lambda e: e.tensor_tensor(out=oc[:, 0:W], in0=osb[0:64, 0:W], in1=dn[:, 0:W], op=ALU.mult),
              reads=["osb", "dn"], writes=["oc"])
        S.add("sp", lambda e: e.dma_start(out=dst3, in_=oc[:, 0:W].rearrange("p (h t) -> p h t", t=128)),
              reads=["oc"], writes=[dkey_out], dma=True)

    def swa_phase(l):
        m = A.mark()
        kaT = A.alloc("kaT", 64, [2, TC], BF16)
        vaO = A.alloc("vaO", 128, [NT, 2, 65], BF16)
        kaH = A.alloc("kaH", 65, [2, 128], BF16)
        vaH = A.alloc("vaH", 128, [2, 65], BF16)
        exps8 = A.alloc("exps8", 64, 8, F32)
        exps = A.alloc("exps", 64, [8, 128], F32)
        qa = [A.alloc("qa%d" % s, 65, [8, 128], BF16) for s in range(2)]
        PT = [A.alloc("PT%d" % s, 128, 512, BF16) for s in range(2)]
        dn = A.alloc("dn", 64, 512, F32)
        oc = A.alloc("oc", 64, 512, BF16)
        osb = A.alloc("osb", 65, 512, F32)
        S.add("dve", lambda e: e.memset(vaO[:, :, :, 64:65], 1.0), writes=["vaO"])
        S.add("dve", lambda e: e.memset(vaH[:, :, 64:65], 1.0), writes=["vaH"])
        for t in range(NTT):
            S.add("sp", lambda e, t=t: e.dma_start(out=kaT[:, :, t * 512:(t + 1) * 512],
                                                   in_=KT_t[t][0:128, :].rearrange("(g d) t -> d g t", d=64)),
                  reads=[("KTa", t)], writes=["kaT"], dma=True)
            for g in range(2):
                S.add("sp", lambda e, t=t, g=g: e.dma_start(out=vaO[:, 4 * t:4 * t + 4, g, 0:64],
                                                            in_=VT_t[t][:, g * 64:(g + 1) * 64].rearrange("(n p) d -> p n d", p=128)),
                      reads=[("VT", t)], writes=["vaO"], dma=True)
        S.add("sp", lambda e: e.dma_start(out=kaH[0:64, :, :],
                                          in_=gKT_t[NTT - 1][0:128, 384:512].rearrange("(g d) t -> d g t", d=64)),
              reads=[("gKT", NTT - 1)], writes=["kaH"], dma=True)
        S.add("dve", lambda e: e.memset(kaH[64:65, :, :], 1.0), writes=["kaH"])
        S.add("sp", lambda e: e.dma_start(out=vaH[:, :, 0:64], in_=gVT_t[NTT - 1][384:512, 0:128].rearrange("p (g d) -> p g d", d=64)),
              reads=[("gVT", NTT - 1)], writes=["vaH"], dma=True)
        S.add("sp", lambda e: e.dma_start(out=exps8, in_=sink_d[l:l + 1, :].to_broadcast([64, 8])), writes=["exps8"], dma=True)
        S.add("act", lambda e: e.activation(out=exps8, in_=exps8, func=AF.Exp), reads=["exps8"], writes=["exps8"])
        S.add("dve", lambda e: e.tensor_copy(out=exps, in_=bc_last(exps8, 128)), reads=["exps8"], writes=["exps"])
        for s in range(2):
            S.add("dve", lambda e, s=s: e.tensor_copy(out=flat2(qa[s][64:65, :, :]), in_=bc_cols(pfxc[64:65, 0:1], 1024)),
                  reads=["pfxc"], writes=[("qa", s)])
        for i in range(NT):
            s = i % 2
            S.add("sp", lambda e, i=i, s=s: e.dma_start(out=qa[s][0:64, :, :],
                                                        in_=qaT_d[:, :, i * 128:(i + 1) * 128].rearrange("h d t -> d h t")),
                  reads=[("qaT", i // 4)], writes=[("qa", s)], dma=True)
            for g in range(2):
                chunks = []
                if i == 0:
                    chunks.append((kaH[0:65, g, :], 65, vaH[:, g, :], T1all, ["kaH", "vaH"]))
                else:
                    chunks.append((kaT[:, g, (i - 1) * 128:i * 128], 64, vaO[:, i - 1, g, :], T1all,
                                   ["kaT", "vaO"]))
                chunks.append((kaT[:, g, i * 128:(i + 1) * 128], 64, vaO[:, i, g, :], T0all, ["kaT", "vaO"]))
                pO, pD = pb[4 + 2 * (g % 2)], pb[5 + 2 * (g % 2)]
                okey, dkey = ("pb", 4 + 2 * (g % 2)), ("pb", 5 + 2 * (g % 2))
                for ci, (kap, r, vap, Tt, kk) in enumerate(chunks):
                    ps = (2 * g + ci) % 4
                    S.add("pe", lambda e, kap=kap, r=r, ps=ps, s=s, g=g: e.matmul(
                        pb[ps], lhsT=kap, rhs=flat2(qa[s][0:r, 4 * g:4 * g + 4, :]), start=True, stop=False),
                        reads=kk + [("qa", s)], writes=[("pb", ps)])
                    S.add("pe", lambda e, Tt=Tt, ps=ps, g=g: e.matmul(
                        pb[ps], lhsT=ident, rhs=flat2(Tt[:, 4 * g:4 * g + 4, :]), start=False, stop=True),
                        reads=["ident", "T0all", "T1all"], writes=[("pb", ps)])
                    S.add("act", lambda e, ps=ps, ci=ci: e.activation(out=PT[ci], in_=pb[ps], func=AF.Exp),
                          reads=[("pb", ps)], writes=[("PT", ci)])
                for ci, (kap, r, vap, Tt, kk) in enumerate(chunks):
                    S.add("pe", lambda e, vap=vap, ci=ci, pO=pO: e.matmul(pO[0:65, :], lhsT=vap, rhs=PT[ci],
                                                                          start=(ci == 0), stop=(ci == 1)),
                          reads=kk + [("PT", ci)], writes=[okey])
                finalize(pO, pD, okey, dkey, 4,
                         catT_d[4 * g:4 * g + 4, :, i * 128:(i + 1) * 128].rearrange("h d t -> d h t"),
                         ("catT", i), extra=flat2(exps[:, 4 * g:4 * g + 4, :]), tmp=(dn, oc, osb))
        S.barrier()
        A.release(m)

    def moba_phase(l):
        m = A.mark()
        kb = A.alloc("kb", 97, [4, T2], BF16)
        vb = A.alloc("vb", 128, [NC2, 4, 65], BF16)
        kmf = A.alloc("kmf", 64, [4, GW], F32)
        kmT = A.alloc("kmT", 64, [4, GW], BF16)
        qb = [A.alloc("qb%d" % s, 97, [4, 128], BF16) for s in range(2)]
        bsel = [A.alloc("bsel%d" % s, 128, [4, 96], BF16) for s in range(2)]
        gt = A.alloc("gt", 128, [4, GW], F32)
        mx8 = A.alloc("mx8", 128, [4, 8], F32)
        thr = A.alloc("thr", 128, 4, F32)
        PT = [A.alloc("PT%d" % s, 128, 512, BF16) for s in range(4)]
        SBANK = [2, 3, 5, 7]
        dn = A.alloc("dn", 64, 512, F32)
        oc = A.alloc("oc", 64, 512, BF16)
        osb = A.alloc("osb", 65, 512, F32)
        S.add("dve", lambda e: e.memset(vb[:, :, :, 64:65], 1.0), writes=["vb"])
        for t in range(NTT):
            S.add("sp", lambda e, t=t: e.dma_start(out=kb[0:64, :, t * 512:(t + 1) * 512],
                                                   in_=gKT_t[t][128:384, :].rearrange("(h d) t -> d h t", d=64)),
                  reads=[("gKT", t)], writes=["kbk"], dma=True)
            S.add("sp", lambda e, t=t: e.dma_start(out=kb[0:64, :, TC + t * 512:TC + (t + 1) * 512],
                                                   in_=KT_t[t][128:384, :].rearrange("(h d) t -> d h t", d=64)),
                  reads=[("KTb", t)], writes=["kbk"], dma=True)
            for h in range(4):
                S.add("sp", lambda e, t=t, h=h: e.dma_start(
                    out=vb[:, 4 * t:4 * t + 4, h, 0:64],
                    in_=gVT_t[t][0:512, 128 + h * 64:128 + (h + 1) * 64].rearrange("(n p) d -> p n d", p=128)),
                    reads=[("gVT", t)], writes=["vb"], dma=True)
            for h in range(4):
                S.add("sp", lambda e, t=t, h=h: e.dma_start(
                    out=vb[:, NT + 4 * t:NT + 4 * t + 4, h, 0:64],
                    in_=VT_t[t][:, 128 + h * 64:128 + (h + 1) * 64].rearrange("(n p) d -> p n d", p=128)),
                    reads=[("VT", t)], writes=["vb"], dma=True)
        for h in range(4):
            S.add("pool", lambda e, h=h: e.dma_start(out=kb[64:97, h, :], in_=er_d), writes=["kbk"], dma=True)
        S.add("dve", lambda e: e.memset(kmf, 0.0), writes=["kmf"])
        for h in range(4):
            S.add("dve", lambda e, h=h: e.reduce_sum(out=kmf[:, h, 0:NB],
                                                     in_=kb[0:64, h, :].rearrange("p (n s) -> p n s", s=256), axis=AX.X),
                  reads=["kbk", "kmf"], writes=["kmf"])
        S.add("dve", lambda e: e.tensor_scalar(out=kmT, in0=kmf, scalar1=1.0 / 256, scalar2=None, op0=ALU.mult),
              reads=["kmf"], writes=["kmT"])
        for s in range(2):
            S.add("sp", lambda e, s=s: e.dma_start(out=qb[s][96:97, :, :], in_=b31s[0:1, 0:4, :]),
                  reads=["b31s"], writes=[("qb", s)], dma=True)
            S.add("dve", lambda e, s=s: e.memset(bsel[s][:, :, 0:64], 0.0), writes=[("bsel", s)])
            S.add("dve", lambda e, s=s: e.memset(bsel[s][:, :, 64:96], -1.0), writes=[("bsel", s)])
        for i in range(NT):
            s = i % 2
            nb = NBH + i // 2
            cq = NT + i
            ob0 = cq - (i % 2)
            S.add("sp", lambda e, i=i, s=s: e.dma_start(out=qb[s][0:64, :, :],
                                                        in_=qbT_d[:, :, i * 128:(i + 1) * 128].rearrange("h d t -> d h t")),
                  reads=[("qbT", i // 4)], writes=[("qb", s)], dma=True)
            for h in range(4):
                S.add("pe", lambda e, s=s, h=h: e.matmul(pb[0][:, h * GW:(h + 1) * GW], lhsT=qb[s][0:64, h, :],
                                                         rhs=kmT[:, h, :], start=(h == 0), stop=(h == 3)),
                      reads=[("qb", s), "kmT"], writes=[("pb", 0)])
            S.add("dve", lambda e: e.tensor_tensor(out=gt, in0=pb[0][:, 0:4 * GW].rearrange("p (h n) -> p h n", n=GW),
                                                   in1=bc_mid(pfxrow, 4), op=ALU.add),
                  reads=[("pb", 0), "pfxrow"], writes=["gt"])
            S.add("dve", lambda e, nb=nb: e.tensor_tensor(out=gt, in0=gt, in1=bc_mid(stepc[:, GW - nb:2 * GW - nb], 4), op=ALU.add),
                  reads=["gt", "stepc"], writes=["gt"])
            for h in range(4):
                S.add("dve", lambda e, h=h: e.max(out=mx8[:, h, :], in_=gt[:, h, :]), reads=["gt"], writes=["mx8"])
            S.add("dve", lambda e: e.tensor_scalar(out=thr, in0=mx8[:, :, 2], scalar1=-16000.0, scalar2=None, op0=ALU.max),
                  reads=["mx8"], writes=["thr"])
            for h in range(4):
                S.add("dve", lambda e, s=s, h=h, nb=nb: e.tensor_scalar(out=bsel[s][:, h, 64:64 + nb], in0=gt[:, h, 0:nb],
                                                                        scalar1=thr[:, h:h + 1], scalar2=-1.0,
                                                                        op0=ALU.is_ge, op1=ALU.add),
                      reads=["gt", "thr"], writes=[("bsel", s)])
            for h in range(4):
                S.add("pe", lambda e, s=s, h=h: e.transpose(out=pbh[1][0:96, h * 128:(h + 1) * 128], in_=bsel[s][:, h, :],
                                                            identity=ident),
                      reads=[("bsel", s), "ident"], writes=[("pb", 1)])
            S.add("act", lambda e, s=s: e.activation(out=flat2(qb[s][64:96, :, :]), in_=pbh[1][64:96, 0:512], func=AF.Copy),
                  reads=[("pb", 1)], writes=[("qb", s)])
            pO, pD = pb[4 + 2 * s], pb[1]
            okey, dkey = ("pb", 4 + 2 * s), ("pb", 1)
            def qk(c, i=i, s=s, cq=cq, ob0=ob0):
                ps = SBANK[c % 4]
                Tt = None
                if c == cq:
                    r, Tt = 64, T0all
                elif c == cq - 1 and c >= ob0:
                    r, Tt = 64, T1all
                elif c == cq - 1:
                    r, Tt = 96, T1all
                else:
                    r = 97
                for h in range(4):
                    S.add("pe", lambda e, h=h, r=r, ps=ps, Tt=Tt: e.matmul(
                        pb[ps][:, h * 128:(h + 1) * 128], lhsT=kb[0:r, h, c * 128:(c + 1) * 128], rhs=qb[s][0:r, h, :],
                        start=(h == 0), stop=(Tt is None and h == 3)),
                        reads=["kbk", ("qb", s)], writes=[("pb", ps)])
                if Tt is not None:
                    S.add("pe", lambda e, ps=ps, Tt=Tt: e.matmul(pb[ps], lhsT=ident, rhs=flat2(Tt[:, 8:12, :]),
                                                                 start=False, stop=True),
                          reads=["ident", "T0all", "T1all"], writes=[("pb", ps)])
                S.add("act", lambda e, ps=ps: e.activation(out=PT[c % 4], in_=pb[ps], func=AF.Exp),
                      reads=[("pb", ps)], writes=[("PT", c % 4)])

            def pv(c, pO=pO, cq=cq, okey=okey):
                for h in range(4):
                    S.add("pe", lambda e, h=h: e.matmul(
                        pO[0:65, h * 128:(h + 1) * 128], lhsT=vb[:, c, h, :], rhs=PT[c % 4][:, h * 128:(h + 1) * 128],
                        start=(c == 0 and h == 0), stop=(c == cq and h == 3)),
                        reads=["vb", ("PT", c % 4)], writes=[okey])

            qk(0)
            if cq >= 1:
                qk(1)
            for c in range(cq + 1):
                if c + 2 <= cq:
                    qk(c + 2)
                pv(c)
            finalize(pO, pD, okey, dkey, 4, catT_d[8:12, :, i * 128:(i + 1) * 128].rearrange("h d t -> d h t"),
                     ("catT", i), tmp=(dn, oc, osb))
        S.barrier()
        A.release(m)

    def dsa_phase(l):
        m = A.mark()
        ki = A.alloc("ki", 64, T2, BF16)
        kc = A.alloc("kc", 65, T2, BF16)
        vc = A.alloc("vc", 128, [NC2, 65], BF16)
        Ib = A.alloc("Ib", 128, T2, F32)
        jk = A.alloc("jk", 128, T2, BF16)
        jk2 = A.alloc("jk2", 128, T2 // 2, BF16)
        mb = [A.alloc("mb%d" % s, 128, T2, BF16) for s in range(2)]
        rl = [A.alloc("rl%d" % h, 128, 512, F32) for h in range(4)]
        qi = [A.alloc("qi%d" % s, 64, [4, 128], BF16) for s in range(2)]
        qc = [A.alloc("qc%d" % s, 65, [4, 128], BF16) for s in range(2)]
        wt = [A.alloc("wt%d" % s, 128, 16, F32) for s in range(2)]
        bs = A.alloc("bs", 128, 8, F32)
        PT = [A.alloc("PT%d" % s, 128, 512, BF16) for s in range(2)]
        dn = A.alloc("dn", 64, 512, F32)
        oc = A.alloc("oc", 64, 512, BF16)
        osb = A.alloc("osb", 65, 512, F32)
        lo, mid, cnt, dl = bs[:, 0:1], bs[:, 1:2], bs[:, 2:3], bs[:, 3:4]
        sgn, tt_ = bs[:, 4:5], bs[:, 5:6]
        if l + 1 < L:
            precast([(l + 1, n_) for n_ in ("w1g", "w1u", "w1d", "win", "w2g", "w2u", "w2d")])
        S.add("dve", lambda e: e.memset(vc[:, :, 64:65], 1.0), writes=["vc"])
        for t in range(NTT):
            S.add("sp", lambda e, t=t: e.dma_start(out=ki[:, t * 512:(t + 1) * 512], in_=gKT_t[t][448:512, :]),
                  reads=[("gKT", t)], writes=["ki"], dma=True)
            S.add("sp", lambda e, t=t: e.dma_start(out=ki[:, TC + t * 512:TC + (t + 1) * 512], in_=KT_t[t][448:512, :]),
                  reads=[("KTi", t)], writes=["ki"], dma=True)
            S.add("sp", lambda e, t=t: e.dma_start(out=kc[0:64, t * 512:(t + 1) * 512], in_=gKT_t[t][384:448, :]),
                  reads=[("gKT", t)], writes=["kc"], dma=True)
            S.add("sp", lambda e, t=t: e.dma_start(out=kc[0:64, TC + t * 512:TC + (t + 1) * 512], in_=KT_t[t][384:448, :]),
                  reads=[("KTc", t)], writes=["kc"], dma=True)
            S.add("sp", lambda e, t=t: e.dma_start(out=vc[:, 4 * t:4 * t + 4, 0:64],
                                                   in_=gVT_t[t][0:512, 384:448].rearrange("(n p) c -> p n c", p=128)),
                  reads=[("gVT", t)], writes=["vc"], dma=True)
            S.add("sp", lambda e, t=t: e.dma_start(out=vc[:, NT + 4 * t:NT + 4 * t + 4, 0:64],
                                                   in_=VT_t[t][:, 384:448].rearrange("(n p) c -> p n c", p=128)),
                  reads=[("VT", t)], writes=["vc"], dma=True)
        S.add("dve", lambda e: e.memset(kc[64:65, :], 1.0), writes=["kc"])
        for s in range(2):
            S.add("sp", lambda e, s=s: e.dma_start(out=qc[s][64:65, :, :], in_=b31s[0:1, 4:8, :]),
                  reads=["b31s"], writes=[("qc", s)], dma=True)

        def stage_a(i, units=None):
            s = i % 2
            nkc = NT + i + 1
            nk = nkc * 128
            nblk0 = (nk + 511) // 512
            per_hook = 0 if not units else -(-len(units) // (nblk0 + NIT))

            def pump():
                for _ in range(per_hook):
                    if units:
                        units.pop(0)()
            na = min(T2 // 2, int(round(ACT_FRAC * nk / 128.0)) * 128)
            nd = nk - na
            S.add("sp", lambda e: e.dma_start(out=qi[s], in_=qiT_d[:, :, i * 128:(i + 1) * 128].rearrange("h d t -> d h t")),
                  reads=[("qiT", i // 4)], writes=[("qi", s)], dma=True)
            S.add("sp", lambda e: e.dma_start(out=qc[s][0:64, :, :],
                                              in_=qcT_d[:, :, i * 128:(i + 1) * 128].rearrange("h d t -> d h t")),
                  reads=[("qcT", i // 4)], writes=[("qc", s)], dma=True)
            S.add("sp", lambda e: e.dma_start(out=wt[s], in_=wi_d[i * 128:(i + 1) * 128, :]),
                  reads=[("wi", i // 4)], writes=[("wt", s)], dma=True)
            nblk = (nk + 511) // 512
            for b in range(nblk):
                w = min(512, nk - b * 512)
                c0 = b * 512
                pfx_blk = (c0 + w) <= TC
                assert pfx_blk or c0 >= TC
                for h in range(4):
                    S.add("pe", lambda e, h=h, c0=c0, w=w: e.matmul(pb[h][:, 0:w], lhsT=qi[s][:, h, :], rhs=ki[:, c0:c0 + w],
                                                                    start=True, stop=True),
                          reads=[("qi", s), "ki"], writes=[("pb", h)])
                    S.add("act", lambda e, h=h, w=w: e.activation(out=rl[h][:, 0:w], in_=pb[h][:, 0:w], func=AF.Relu),
                          reads=[("pb", h)], writes=[("rl", h)])
                sc2 = pfxc[:, 0:1] if pfx_blk else zero1[:, 0:1]
                S.add("dve", lambda e, c0=c0, w=w, sc2=sc2: e.tensor_scalar(
                    out=Ib[:, c0:c0 + w], in0=rl[0][:, 0:w], scalar1=wt[s][:, 12:13], scalar2=sc2, op0=ALU.mult, op1=ALU.add),
                    reads=[("rl", 0), ("wt", s), "pfxc", "zero1"], writes=["Ib"])
                for h in range(1, 4):
                    S.add("dve", lambda e, h=h, c0=c0, w=w: e.scalar_tensor_tensor(
                        out=Ib[:, c0:c0 + w], in0=rl[h][:, 0:w], scalar=wt[s][:, 12 + h:13 + h], in1=Ib[:, c0:c0 + w],
                        op0=ALU.mult, op1=ALU.add),
                        reads=[("rl", h), ("wt", s), "Ib"], writes=["Ib"])
                pump()
            S.add("dve", lambda e: e.tensor_tensor(out=Ib[:, nk - 128:nk], in0=Ib[:, nk - 128:nk], in1=cmt, op=ALU.add),
                  reads=["Ib", "cmt"], writes=["Ib"])
            S.add("dve", lambda e: e.memset(mid, LO0 + BRW / 2), writes=["mid"])
            for it in range(NIT):
                step = BRW / (2 ** (it + 1))
                S.add("act", lambda e: e.activation(out=jk2[:, 0:na], in_=Ib[:, nd:nk], func=AF.Sign, scale=-1.0, bias=mid,
                                                    accum_out=sgn),
                      reads=["Ib", "mid"], writes=["jk2", "sgn"])
                S.add("dve", lambda e: e.tensor_scalar(out=jk[:, 0:nd], in0=Ib[:, 0:nd], scalar1=mid, scalar2=None,
                                                       op0=ALU.is_ge, op1=ALU.add, accum_out=cnt),
                      reads=["Ib", "mid"], writes=["jk", "cnt"])
                pump()
                S.add("dve", lambda e: e.scalar_tensor_tensor(out=tt_, in0=cnt, scalar=2.0, in1=sgn,
                                                              op0=ALU.mult, op1=ALU.subtract),
                      reads=["cnt", "sgn"], writes=["tt"])
                S.add("dve", lambda e: e.tensor_scalar(out=dl, in0=tt_, scalar1=float(512 - na), scalar2=-0.5,
                                                       op0=ALU.is_ge, op1=ALU.add),
                      reads=["tt"], writes=["dl"])
                if it + 1 < NIT:
                    S.add("dve", lambda e, step=step: e.scalar_tensor_tensor(out=mid, in0=dl, scalar=step, in1=mid,
                                                                             op0=ALU.mult, op1=ALU.add),
                          reads=["dl", "mid"], writes=["mid"])
                else:
                    S.add("dve", lambda e: e.tensor_scalar(out=dl, in0=dl, scalar1=-0.5, scalar2=step,
                                                           op0=ALU.add, op1=ALU.mult),
                          reads=["dl"], writes=["dl"])
                    S.add("dve", lambda e: e.tensor_tensor(out=lo, in0=mid, in1=dl, op=ALU.add), reads=["mid", "dl"], writes=["lo"])
            S.add("dve", lambda e: e.tensor_scalar(out=mb[s][:, 0:nk], in0=Ib[:, 0:nk], scalar1=lo, scalar2=-BIG,
                                                   op0=ALU.is_lt, op1=ALU.mult),
                  reads=["Ib", "lo"], writes=[("mb", s)])

        def stage_b_units(i):
            s = i % 2
            nkc = NT + i + 1
            pO, pD = pb[6], pb[7]
            okey, dkey = ("pb", 6), ("pb", 7)
            def qk(c):
                ps = 4 + c % 2
                Tt = None
                if c == nkc - 1:
                    r, Tt = 64, T0all
                elif c == nkc - 2:
                    r, Tt = 64, T1all
                else:
                    r = 65
                S.add("pe", lambda e, r=r, ps=ps: e.matmul(pb[ps], lhsT=kc[0:r, c * 128:(c + 1) * 128],
                                                           rhs=flat2(qc[s][0:r, :, :]), start=True, stop=False),
                      reads=["kc", ("qc", s)], writes=[("pb", ps)])
                S.add("pe", lambda e, ps=ps, Tt=Tt: e.matmul(pb[ps], lhsT=mb[s][:, c * 128:(c + 1) * 128], rhs=flat2(ident4),
                                                             start=False, stop=(Tt is None)),
                      reads=[("mb", s), "ident4"], writes=[("pb", ps)])
                if Tt is not None:
                    S.add("pe", lambda e, ps=ps, Tt=Tt: e.matmul(pb[ps], lhsT=ident, rhs=flat2(Tt[:, 12:16, :]),
                                                                 start=False, stop=True),
                          reads=["ident", "T0all", "T1all"], writes=[("pb", ps)])
                S.add("act", lambda e, ps=ps: e.activation(out=PT[c % 2], in_=pb[ps], func=AF.Exp),
                      reads=[("pb", ps)], writes=[("PT", c % 2)])

            def pv(c):
                S.add("pe", lambda e: e.matmul(pO[0:65, :], lhsT=vc[:, c, :], rhs=PT[c % 2], start=(c == 0), stop=(c == nkc - 1)),
                      reads=["vc", ("PT", c % 2)], writes=[okey])

            units = [lambda: qk(0)]

            def unit(c):
                if c + 1 < nkc:
                    qk(c + 1)
                pv(c)
            for c in range(nkc):
                units.append(lambda c=c: unit(c))
            units.append(lambda: finalize(pO, pD, okey, dkey, 4,
                                          catT_d[12:16, :, i * 128:(i + 1) * 128].rearrange("h d t -> d h t"),
                                          ("catT", i), tmp=(dn, oc, osb)))
            return units

        stage_a(0)
        for i in range(NT):
            units = stage_b_units(i)
            if i + 1 < NT:
                stage_a(i + 1, units)
            while units:
                units.pop(0)()
        S.barrier()
        A.release(m)

    def wout_phase(l):
        m = A.mark()
        wo = A.alloc("wo", 64, [16, D], BF16)
        xs = [A.alloc("xs%d" % s, 128, D, F32) for s in range(3)]
        ct = [A.alloc("ct%d" % s, 64, [16, 128], BF16) for s in range(2)]
        S.add("pool", lambda e: e.dma_start(out=wo[:, 0:8, :], in_=wo_d[l, 0:512, :].rearrange("(h d) m -> d h m", d=64)),
              writes=["wo"], dma=True)
        S.add("pool", lambda e: e.dma_start(out=wo[:, 8:16, :], in_=wo_d[l, 512:1024, :].rearrange("(h d) m -> d h m", d=64)),
              writes=["wo"], dma=True)
        for u in range(NT):
            sl = u % 3
            s = u % 2
            S.add("sp", lambda e, u=u, sl=sl: e.dma_start(out=xs[sl], in_=xres[u * 128:(u + 1) * 128, :]),
                  reads=[("xres", u)], writes=[("xs", sl)], dma=True)
            S.add("sp", lambda e, u=u, s=s: e.dma_start(out=ct[s], in_=catT_d[:, :, u * 128:(u + 1) * 128].rearrange("h d t -> d h t")),
                  reads=[("catT", u)], writes=[("ct", s)], dma=True)
            for half in range(2):
                py = (2 * u + half) % 4
                for h in range(16):
                    S.add("pe", lambda e, s=s, h=h, half=half, py=py: e.matmul(
                        pb[py], lhsT=ct[s][:, h, :], rhs=wo[:, h, half * 512:(half + 1) * 512], start=(h == 0), stop=(h == 15)),
                        reads=[("ct", s), "wo"], writes=[("pb", py)])
                S.add("dve", lambda e, sl=sl, half=half, py=py: e.tensor_tensor(
                    out=xs[sl][:, half * 512:(half + 1) * 512], in0=pb[py], in1=xs[sl][:, half * 512:(half + 1) * 512], op=ALU.add),
                    reads=[("pb", py), ("xs", sl)], writes=[("xs", sl)])
            S.add("sp", lambda e, u=u, sl=sl: e.dma_start(out=xres[u * 128:(u + 1) * 128, :], in_=xs[sl]),
                  reads=[("xs", sl)], writes=[("xres", u)], dma=True)
        S.barrier()
        A.release(m)

    def final_phase():
        m = A.mark()
        gb = A.alloc("gb", 128, D, F32)
        xs = [A.alloc("xs%d" % s, 128, D, F32) for s in range(3)]
        ob = [A.alloc("ob%d" % s, 128, D, F32) for s in range(2)]
        stat = A.alloc("stat", 128, [3, 4], F32)
        junk = A.alloc("junk", 128, D, BF16)
        S.add("sp", lambda e: e.dma_start(out=gb, in_=nf_d.to_broadcast([128, D])), writes=["gb"], dma=True)
        for u in range(NT):
            sl = u % 3
            S.add("sp", lambda e, u=u, sl=sl: e.dma_start(out=xs[sl], in_=xres[u * 128:(u + 1) * 128, :]),
                  reads=[("xres", u)], writes=[("xs", sl)], dma=True)
            norm_sub(xs[sl], ("xs", sl), gb, "gb", stat[:, sl, :], ("stat", sl), ob[u % 2], ("ob", u % 2), junk)
            S.add("sp", lambda e, u=u: e.dma_start(out=out_d[u * 128:(u + 1) * 128, :], in_=ob[u % 2]),
                  reads=[("ob", u % 2)], writes=[("out", u)], dma=True)
        S.barrier()
        A.release(m)

    def dump(name, src):
        S.add("sp", lambda e: e.dma_start(out=dbg[name], in_=src), writes=[("dbg", name)], dma=True)
        S.barrier()

    plist = [("phase0", phase0)]
    for l in range(L):
        plist.append(("ffn1_%d" % l, lambda l=l: ffn_phase(l, w1g_d, w1u_d, w1d_d, n1_d[l:l + 1, :], x_in if l == 0 else xres, xres,
                                                          pre=(None if l == 0 else ("w1g", "w1u", "w1d")))))
        plist.append(("proj_%d" % l, lambda l=l: proj_phase(l)))
        plist.append(("xchg_%d" % l, exchange))
        plist.append(("swa_%d" % l, lambda l=l: swa_phase(l)))
        plist.append(("moba_%d" % l, lambda l=l: moba_phase(l)))
        plist.append(("dsa_%d" % l, lambda l=l: dsa_phase(l)))
        plist.append(("wout_%d" % l, lambda l=l: wout_phase(l)))
        plist.append(("ffn2_%d" % l, lambda l=l: ffn_phase(l, w2g_d, w2u_d, w2d_d, n2_d[l:l + 1, :], xres, xres,
                                                          pre=("w2g", "w2u", "w2d"))))
    plist.append(("final", final_phase))
    for name, fn in plist:
        S.tag = name
        fn()
        if upto is not None and name == upto:
            break
    if debug:
        S.tag = "dump"
        dump("xres", xres)
        dump("catT", catT_d)
        dump("qaT", qaT_d)
    S.add("act", lambda e: e.activation(out=zero1, in_=zero1, func=AF.Copy), reads=["zero1"], writes=["zero1"])
    S.add("dve", lambda e: e.memset(zero1, 0.0), writes=["zero1"])
    S.add("pool", lambda e: e.memset(zero1, 0.0), writes=["zero1"])
    S.add("sp", lambda e: e.dma_start(out=pfxc, in_=pfx_d.to_broadcast([128, 2])), writes=["pfxc"], dma=True)
    S.emit(stack)
    stack.close()
    return nc, S


_CACHE = {}


def make_in_maps(inputs, SEQ, DEPTH):
    f = lambda a: np.ascontiguousarray(np.asarray(a, dtype=np.float32))
    x = f(inputs["x"])
    B = x.shape[0]
    TC = SEQ // 2
    oh, er, cm = host_consts(SEQ)
    shared = {
        "tab": f(inputs["rel_bias_table"]),
        "n1": f(inputs["ffn1_norm"]), "w1g": f(inputs["ffn1_w_gate"]), "w1u": f(inputs["ffn1_w_up"]),
        "w1d": f(inputs["ffn1_w_down"]), "nm": f(inputs["mix_norm"]), "win": f(inputs["w_in"]),
        "sinks": f(inputs["attn_sinks"]), "kvn": f(inputs["kv_norm_c"]), "wup": f(inputs["w_kv_up_c"]),
        "wo": f(inputs["w_out"]), "n2": f(inputs["ffn2_norm"]), "w2g": f(inputs["ffn2_w_gate"]),
        "w2u": f(inputs["ffn2_w_up"]), "w2d": f(inputs["ffn2_w_down"]),
        "nf": f(inputs["final_norm"]).reshape(1, -1),
        "oh": oh, "er": er, "cm": cm,
    }
    maps = []
    for c in range(2 * B):
        b, half = c // 2, c % 2
        mp = dict(shared)
        mp["x"] = np.ascontiguousarray(x[b, half * TC:(half + 1) * TC, :])
        mp["pfx"] = np.array([[-BIG if half == 0 else 0.0, 0.0]], np.float32)
        maps.append(mp)
    return maps


def kernel(**inputs):
    x = np.asarray(inputs["x"])
    B, SEQ, _ = x.shape
    DEPTH = np.asarray(inputs["ffn1_norm"]).shape[0]
    assert B == 4
    key = (SEQ, DEPTH)
    if key not in _CACHE:
        _CACHE[key] = build_program(SEQ, DEPTH)[0]
    nc = _CACHE[key]
    maps = make_in_maps(inputs, SEQ, DEPTH)
    res = run_bass_kernel_spmd(nc, maps, core_ids=list(range(8)))
    TC = SEQ // 2
    out = np.empty((B, SEQ, D), np.float32)
    for c in range(8):
        b, half = c // 2, c % 2
        out[b, half * TC:(half + 1) * TC, :] = np.asarray(res.results[c]["out"], dtype=np.float32)
    return out
```
